# Optimizing a Trainium2 kernel written in Bass

```python
import jax, jax.numpy as jnp
from jax import lax
import numpy as np

D_MODEL = 1024
BATCH = 8
SEQ = 4096
DEPTH = 4

CHUNK = 64
N_MIXERS = 2
EPS = 1e-6

HG_DK = 128
HG_HEADS = max(4, D_MODEL // HG_DK)
HG_DV = D_MODEL // HG_HEADS
HG_FDIM = HG_HEADS * HG_DK
HG_WIDTH = HG_HEADS * HG_DV
HG_IN = 2 * HG_FDIM + 2 * HG_WIDTH

FX_HD = 64
FX_HEADS = D_MODEL // FX_HD
FX_WIDTH = FX_HEADS * FX_HD
FX_IN = 4 * FX_WIDTH + FX_HEADS
Q_BLOCK = 128

N_HG_LAYERS = (DEPTH + N_MIXERS - 1) // N_MIXERS
N_FX_LAYERS = DEPTH // N_MIXERS

kernel_name = "hybrid_hgrn2_fox_adaln_trunk"


def _rms(x, g):
    xf = x.astype(jnp.float32)
    y = xf * lax.rsqrt(jnp.mean(xf * xf, axis=-1, keepdims=True) + EPS)
    return (y * g.astype(jnp.float32)).astype(x.dtype)


def _hgrn2_scan(q, log_f, k, v):
    B, S, H, DK = q.shape
    DV = v.shape[-1]
    N = S // CHUNK

    def to_chunks(t):
        return t.astype(jnp.float32).reshape(B, N, CHUNK, H, t.shape[-1]).transpose(1, 0, 3, 2, 4)

    qc, gc, kc, vc = to_chunks(q), to_chunks(log_f), to_chunks(k), to_chunks(v)
    causal = jnp.tril(jnp.ones((CHUNK, CHUNK), dtype=bool))

    def step(state, inp):
        qb, gb, kb, vb = inp
        bcum = jnp.cumsum(gb, axis=2)
        inter = jnp.einsum('bhtk,bhkv->bhtv', qb * jnp.exp(bcum), state)
        diff = jnp.where(causal[None, None, :, :, None],
                         bcum[:, :, :, None, :] - bcum[:, :, None, :, :], -jnp.inf)
        scores = jnp.einsum('bhtk,bhsk,bhtsk->bhts', qb, kb, jnp.exp(diff))
        intra = jnp.einsum('bhts,bhsv->bhtv', scores, vb)
        last = bcum[:, :, -1]
        k_dec = kb * jnp.exp(last[:, :, None, :] - bcum)
        new_state = state * jnp.exp(last)[..., None] + jnp.einsum('bhsk,bhsv->bhkv', k_dec, vb)
        return new_state, inter + intra

    s0 = jnp.zeros((B, H, DK, DV), jnp.float32)
    _, out = lax.scan(step, s0, (qc, gc, kc, vc))
    return out.transpose(1, 0, 3, 2, 4).reshape(B, S, H, DV)


def _hgrn2_layer(h, w_in, lb, o_g, w_out):
    B, S, _ = h.shape
    proj = h @ w_in
    q, fl, i, z = jnp.split(proj, [HG_FDIM, 2 * HG_FDIM, 2 * HG_FDIM + HG_WIDTH], axis=-1)
    fl = fl.astype(jnp.float32)
    log_f = jnp.logaddexp(jnp.log(lb), jnp.log1p(-lb) + jax.nn.log_sigmoid(fl))
    k = (1.0 - lb) * jax.nn.sigmoid(-fl)
    shp = (B, S, HG_HEADS, HG_DK)
    o = _hgrn2_scan(q.reshape(shp), log_f.reshape(shp), k.reshape(shp),
                    i.reshape(B, S, HG_HEADS, HG_DV))
    o = _rms(o, o_g.reshape(HG_HEADS, HG_DV)).astype(h.dtype)
    o = o.reshape(B, S, HG_WIDTH) * jax.nn.silu(z)
    return o @ w_out


def _fox_attn(q, k, v, log_f):
    B, S, H, D = q.shape
    cum = jnp.cumsum(log_f, axis=1).transpose(0, 2, 1)
    qh, kh, vh = (t.transpose(0, 2, 1, 3) for t in (q, k, v))
    scale = D ** -0.5
    outs = []
    for blk in range(S // Q_BLOCK):
        q0, q1 = blk * Q_BLOCK, (blk + 1) * Q_BLOCK
        s = jnp.einsum('bhtd,bhsd->bhts', qh[:, :, q0:q1], kh[:, :, :q1],
                       preferred_element_type=jnp.float32) * scale
        s = s + cum[:, :, q0:q1, None] - cum[:, :, None, :q1]
        tpos = jnp.arange(q0, q1)
        spos = jnp.arange(q1)
        s = jnp.where(spos[None, :] <= tpos[:, None], s, -jnp.inf)
        p = jax.nn.softmax(s, axis=-1)
        outs.append(jnp.einsum('bhts,bhsd->bhtd', p.astype(vh.dtype), vh[:, :, :q1]))
    return jnp.concatenate(outs, axis=2).transpose(0, 2, 1, 3)


def _fox_layer(h, w_in, b_f, q_g, k_g, w_out):
    B, S, _ = h.shape
    proj = h @ w_in
    q, k, v, z, fl = jnp.split(proj, [FX_WIDTH, 2 * FX_WIDTH, 3 * FX_WIDTH, 4 * FX_WIDTH], axis=-1)
    shp = (B, S, FX_HEADS, FX_HD)
    q = _rms(q.reshape(shp), q_g)
    k = _rms(k.reshape(shp), k_g)
    log_f = jax.nn.log_sigmoid(fl.astype(jnp.float32) + b_f.astype(jnp.float32))
    o = _fox_attn(q, k, v.reshape(shp), log_f)
    o = o.reshape(B, S, FX_WIDTH) * jax.nn.silu(z)
    return o @ w_out


def setup_inputs(seed: int = 0) -> dict:
    key = jax.random.key(seed)
    ks = jax.random.split(key, 16)
    f32 = jnp.float32
    D = D_MODEL
    return {
        "x": jax.random.normal(ks[0], (BATCH, SEQ, D), f32),
        "c": jax.random.normal(ks[1], (BATCH, D), f32),
        "norm_g": 1.0 + 0.05 * jax.random.normal(ks[2], (DEPTH, D), f32),
        "ada_w": 0.5 * D ** -0.5 * jax.random.normal(ks[3], (DEPTH, D, 3 * D), f32),
        "ada_b": 0.02 * jax.random.normal(ks[4], (DEPTH, 3 * D), f32),
        "hg_lb_logits": jax.random.normal(ks[5], (N_HG_LAYERS, HG_FDIM), f32),
        "hg_w_in": D ** -0.5 * jax.random.normal(ks[6], (N_HG_LAYERS, D, HG_IN), f32),
        "hg_o_g": 1.0 + 0.05 * jax.random.normal(ks[7], (N_HG_LAYERS, HG_WIDTH), f32),
        "hg_w_out": HG_WIDTH ** -0.5 * jax.random.normal(ks[8], (N_HG_LAYERS, HG_WIDTH, D), f32),
        "fx_w_in": D ** -0.5 * jax.random.normal(ks[9], (N_FX_LAYERS, D, FX_IN), f32),
        "fx_b_f": jax.random.uniform(ks[10], (N_FX_LAYERS, FX_HEADS), f32, 1.0, 5.0),
        "fx_q_g": 1.0 + 0.05 * jax.random.normal(ks[11], (N_FX_LAYERS, FX_HD), f32),
        "fx_k_g": 1.0 + 0.05 * jax.random.normal(ks[12], (N_FX_LAYERS, FX_HD), f32),
        "fx_w_out": FX_WIDTH ** -0.5 * jax.random.normal(ks[13], (N_FX_LAYERS, FX_WIDTH, D), f32),
    }


def reference(x, c, norm_g, ada_w, ada_b, hg_lb_logits, hg_w_in, hg_o_g, hg_w_out,
              fx_w_in, fx_b_f, fx_q_g, fx_k_g, fx_w_out):
    lb_all = jnp.cumsum(jax.nn.softmax(hg_lb_logits.astype(jnp.float32), axis=0), axis=0)
    lb_all = lb_all - lb_all[0:1]
    c_act = jax.nn.silu(c)
    for layer in range(DEPTH):
        mod = c_act @ ada_w[layer] + ada_b[layer]
        shift, scale, gate = jnp.split(mod, 3, axis=-1)
        h = _rms(x, norm_g[layer]) * (1.0 + scale[:, None, :]) + shift[:, None, :]
        j = layer // N_MIXERS
        if layer % N_MIXERS == 0:
            y = _hgrn2_layer(h, hg_w_in[j], lb_all[j], hg_o_g[j], hg_w_out[j])
        else:
            y = _fox_layer(h, fx_w_in[j], fx_b_f[j], fx_q_g[j], fx_k_g[j], fx_w_out[j])
        x = x + gate[:, None, :] * y
    return x
```

```python
import contextlib
import numpy as np
import concourse.bass as bass
import concourse.mybir as mybir
from concourse.bass_utils import run_bass_kernel_spmd

F32 = mybir.dt.float32
BF16 = mybir.dt.bfloat16
ALU = mybir.AluOpType
AF = mybir.ActivationFunctionType

ENGS = ("pe", "act", "dve", "pool", "sp")
D = 1024
KC = 8
EPS = 1e-6
SAME_ENG_SYNC = False
NEG = -30000.0


class Prog:
    def __init__(self, nc, es, n_dma_sems=40):
        self.nc = nc
        self.ops = []
        self.buf = {}
        self.n_dma_sems = n_dma_sems
        self.dma_rr = 0
        self.slot_ord = [0] * n_dma_sems
        self.slot_last = {}
        self.flushed = 0
        self.cnt = {e: 0 for e in ENGS}
        self.esem = {e: es.enter_context(nc.semaphore("s_" + e)) for e in ENGS}
        self.dsem = [es.enter_context(nc.semaphore("d_%d" % k)) for k in range(n_dma_sems)]
        self.nflush = 0

    def op(self, eng, fn, r=(), w=(), dma=False):
        pk_ = [k for k in r if isinstance(k, str) and k.startswith("bank")]
        if pk_:
            w = list(w) + [k for k in pk_ if k not in w]
            r = [k for k in r if k not in pk_]
        deps = set()
        for b in r:
            st = self.buf.get(b)
            if st is not None and st[0] is not None:
                deps.add(st[0])
        for b in w:
            st = self.buf.get(b)
            if st is not None:
                if st[0] is not None:
                    deps.add(st[0])
                deps.update(st[1])
        i = len(self.ops)
        o = dict(eng=eng, fn=fn, deps=deps, dma=dma, slot=None, ordinal=None, count=None)
        if dma:
            s = self.dma_rr % self.n_dma_sems
            self.dma_rr += 1
            prev = self.slot_last.get(s)
            if prev is not None:
                deps.add(prev)
            self.slot_ord[s] += 1
            o["ordinal"] = self.slot_ord[s]
            o["slot"] = s
            self.slot_last[s] = i
        self.ops.append(o)
        for b in r:
            st = self.buf.setdefault(b, [None, []])
            st[1].append(i)
        for b in w:
            self.buf[b] = [i, []]
        return i

    def flush(self):
        nc = self.nc
        ops = self.ops
        lo, hi = self.flushed, len(ops)
        if lo == hi:
            return
        base_cnt = dict(self.cnt)
        base_ord = list(self.slot_ord_flushed) if hasattr(self, "slot_ord_flushed") else [0] * self.n_dma_sems
        waited = {e: {f: -1 for f in ENGS} for e in ENGS}
        waited_dma = {e: {} for e in ENGS}
        needed = {}
        for i in range(lo, hi):
            o = ops[i]
            e = o["eng"]
            wl = []
            for d in sorted(o["deps"]):
                if d < lo:
                    continue
                od = ops[d]
                if od["dma"]:
                    if waited_dma[e].get(od["slot"], 0) >= od["ordinal"]:
                        continue
                    waited_dma[e][od["slot"]] = od["ordinal"]
                    wl.append(("dma", od["slot"], od["ordinal"]))
                else:
                    f = od["eng"]
                    if f == e and (e == "pe" or not SAME_ENG_SYNC):
                        continue
                    if waited[e][f] >= d:
                        continue
                    waited[e][f] = d
                    needed[d] = True
                    wl.append(("eng", f, d))
            o["waits"] = wl
        last_of = {}
        for i in range(lo, hi):
            if not ops[i]["dma"]:
                last_of[ops[i]["eng"]] = i
        for e, i in last_of.items():
            needed[i] = True
        for i in range(lo, hi):
            o = ops[i]
            if o["dma"]:
                continue
            if needed.get(i):
                self.cnt[o["eng"]] += 1
                o["count"] = self.cnt[o["eng"]]
        per_eng = {e: [i for i in range(lo, hi) if ops[i]["eng"] == e] for e in ENGS}
        esem, dsem = self.esem, self.dsem
        first = self.nflush == 0
        self.nflush += 1
        final_waits = {e: [] for e in ENGS}
        cur_last = {}
        for i in range(lo, hi):
            if ops[i]["dma"]:
                cur_last[ops[i]["slot"]] = i
        for s, i in cur_last.items():
            final_waits[ops[i]["eng"]].append((s, ops[i]["ordinal"]))

        def replay(ename, eng):
            if not first:
                for f in ENGS:
                    if f != ename and base_cnt[f] > 0:
                        eng.wait_ge(esem[f], base_cnt[f])
            for i in per_eng[ename]:
                o = ops[i]
                for wt in o["waits"]:
                    if wt[0] == "dma":
                        eng.wait_ge(dsem[wt[1]], 16 * wt[2])
                    else:
                        eng.wait_ge(esem[wt[1]], ops[wt[2]]["count"])
                ins = o["fn"](eng)
                if o["dma"]:
                    ins.then_inc(dsem[o["slot"]], 16)
                elif needed.get(i):
                    ins.then_inc(esem[ename], 1)
                o["fn"] = None
            for s, ordn in final_waits[ename]:
                eng.wait_ge(dsem[s], 16 * ordn)

        with nc.Block() as block:
            def wrap(ename):
                def f(eng):
                    replay(ename, eng)
                    if final_waits[ename]:
                        self.cnt[ename] += 1
                        eng.nop().then_inc(esem[ename], 1)
                return f
            block.tensor(wrap("pe"))
            block.scalar(wrap("act"))
            block.vector(wrap("dve"))
            block.gpsimd(wrap("pool"))
            block.sync(wrap("sp"))
        self.flushed = hi
        self.slot_ord_flushed = list(self.slot_ord)


class B:
    def __init__(self, P):
        self.P = P

    def dma(self, out, in_, r, w, eng="sp"):
        return self.P.op(eng, lambda e: e.dma_start(out=out, in_=in_), r=r, w=w, dma=True)

    def mm(self, out, lhsT, rhs, start, stop, r, w, **kw):
        return self.P.op("pe", lambda e: e.matmul(out, lhsT=lhsT, rhs=rhs, start=start, stop=stop,
                                                  skip_group_check=True, **kw), r=r, w=w)

    def tr(self, out, in_, ident, r, w):
        return self.P.op("pe", lambda e: e.transpose(out=out, in_=in_, identity=ident), r=r, w=w)

    def act(self, out, in_, func, r, w, scale=1.0, bias=0.0):
        return self.P.op("act", lambda e: e.activation(out=out, in_=in_, func=func, scale=scale, bias=bias), r=r, w=w)

    def tt(self, out, in0, in1, op, r, w, eng="dve"):
        return self.P.op(eng, lambda e: e.tensor_tensor(out=out, in0=in0, in1=in1, op=op), r=r, w=w)

    def ts(self, out, in0, s1, s2, op0, op1, r, w, eng="dve"):
        return self.P.op(eng, lambda e: e.tensor_scalar(out=out, in0=in0, scalar1=s1, scalar2=s2, op0=op0, op1=op1),
                         r=r, w=w)

    def ts1(self, out, in0, s1, op0, r, w, eng="dve"):
        return self.P.op(eng, lambda e: e.tensor_single_scalar(out=out, in_=in0, scalar=s1, op=op0), r=r, w=w)

    def stt(self, out, in0, scalar, in1, op0, op1, r, w, eng="dve"):
        return self.P.op(eng, lambda e: e.scalar_tensor_tensor(out=out, in0=in0, scalar=scalar, in1=in1,
                                                               op0=op0, op1=op1), r=r, w=w)

    def copy(self, out, in_, r, w, eng="dve"):
        return self.P.op(eng, lambda e: e.tensor_copy(out=out, in_=in_), r=r, w=w)

    def recip(self, out, in_, r, w):
        return self.P.op("dve", lambda e: e.reciprocal(out=out, in_=in_), r=r, w=w)

    def memset(self, ap, val, w, eng="pool"):
        return self.P.op(eng, lambda e: e.memset(ap, val), r=(), w=w)

    def scan(self, out, d0, d1, initial, r, w):
        return self.P.op("dve", lambda e: e.tensor_tensor_scan(out=out, data0=d0, data1=d1, initial=initial,
                                                               op0=ALU.mult, op1=ALU.add), r=r, w=w)


def build(S, n_layers, stop_after=None):
    NB = S // 512
    NT = S // 128
    nc = bass.Bass("TRN2", target_bir_lowering=False)

    def din(name, shape, dt=F32):
        return nc.dram_tensor(name, list(shape), dt, kind="ExternalInput").ap()

    def dscr(name, shape, dt):
        return nc.dram_tensor(name, list(shape), dt, kind="Internal").ap()

    xT = din("xT", [D, S])
    cT = din("cT", [128, KC])
    gT = din("gT", [128, 4, KC])
    ada_w = din("ada_w", [4, D, 3 * D])
    ada_bT = din("ada_bT", [128, 4, 24])
    lbT = din("lbT", [128, 2, 8])
    hg_w_in = din("hg_w_in", [2, D, 4096])
    ogT = din("ogT", [128, 2, 8])
    hg_w_out = din("hg_w_out", [2, D, D])
    fx_w_in = din("fx_w_in", [2, D, 4112])
    bfT = din("bfT", [16, 2])
    qgT = din("qgT", [128, 2])
    kgT = din("kgT", [128, 2])
    fx_w_out = din("fx_w_out", [2, D, D])
    c_maskbias = din("c_maskbias", [128, 128])
    c_mask01 = din("c_mask01", [128, 128])
    c_mj = din("c_mj", [128, 4])
    c_blk64 = din("c_blk64", [128, 128])
    yT = nc.dram_tensor("yT", [D, S], F32, kind="ExternalOutput").ap()

    sA = dscr("sA", [8, 128, S], BF16)
    sK = dscr("sK", [8, 128, S], BF16)
    sG = dscr("sG", [8, 128, S], F32)
    sV = dscr("sV", [NT, 128, 1024], BF16)
    sZ = dscr("sZ", [8, 128, S], BF16)
    sC = dscr("sC", [16, 6, S], BF16)

    with contextlib.ExitStack() as es:
        P = Prog(nc, es)
        b = B(P)

        uid = [0]

        def sb(stack, name, shape, dt):
            uid[0] += 1
            return stack.enter_context(nc.sbuf_tensor("%s_u%d" % (name, uid[0]), list(shape), dt))

        banks = [es.enter_context(nc.psum_tensor("bank%d" % i, [128, 512], F32)) for i in range(7)]
        bankT = es.enter_context(nc.psum_tensor("bankT", [128, 1024], BF16))
        ident = sb(es, "ident", [128, 128], BF16)
        ones_bf = sb(es, "ones_bf", [128, 128], BF16)
        blk64 = sb(es, "blk64", [128, 128], BF16)
        maskbias = sb(es, "maskbias", [128, 128], BF16)
        mask01 = sb(es, "mask01", [128, 128], F32)
        mj = sb(es, "mj", [128, 4], F32)
        ctmp = sb(es, "ctmp", [128, 128], F32)
        cact = sb(es, "cact", [128, KC], F32)
        gsb = sb(es, "gsb", [128, 4, KC], F32)
        modsb = sb(es, "modsb", [128, 4, 24], F32)
        asb = sb(es, "asb", [128, 4, KC], F32)
        lbsb = sb(es, "lbsb", [128, 2, 8], F32)
        omlsb = sb(es, "omlsb", [128, 2, 8], F32)
        ogsb = sb(es, "ogsb", [128, 2, 8], F32)
        nbf = sb(es, "nbf", [16, 2], F32)
        qg = sb(es, "qg", [128, 2], F32)
        kg = sb(es, "kg", [128, 2], F32)
        wout = sb(es, "wout", [128, KC, D], BF16)
        onesf = sb(es, "onesf", [128, 512], F32)

        with contextlib.ExitStack() as ph:
            adast = [sb(ph, "adast%d" % i, [128, KC, 512], F32) for i in range(2)]
            b.memset(ident[:], 0.0, w=["ident"])
            P.op("pool", lambda e: e.affine_select(out=ident[:], in_=ident[:], pattern=[[-1, 128]],
                                                   compare_op=ALU.not_equal, fill=1.0, base=0,
                                                   channel_multiplier=1), r=["ident"], w=["ident"])
            b.memset(ones_bf[:], 1.0, w=["ones_bf"])
            b.memset(onesf[:], 1.0, w=["onesf"])
            for (src, dst, nm) in ((c_maskbias, maskbias, "maskbias"), (c_blk64, blk64, "blk64")):
                b.dma(ctmp[:], src, r=[], w=["ctmp"])
                b.copy(dst[:], ctmp[:], r=["ctmp"], w=[nm])
            b.dma(mask01[:], c_mask01, r=[], w=["mask01"])
            b.dma(mj[:], c_mj, r=[], w=["mj"])
            b.dma(cact[:], cT, r=[], w=["cact"])
            b.dma(gsb[:], gT, r=[], w=["gsb"])
            b.dma(modsb[:], ada_bT, r=[], w=["modsb"])
            b.dma(lbsb[:], lbT, r=[], w=["lbsb"])
            b.dma(ogsb[:], ogT, r=[], w=["ogsb"])
            b.dma(nbf[:], bfT, r=[], w=["nbf"])
            b.dma(qg[:], qgT, r=[], w=["qg"])
            b.dma(kg[:], kgT, r=[], w=["kg"])
            b.act(cact[:], cact[:], AF.Silu, r=["cact"], w=["cact"])
            b.tt(lbsb[:, 1, :], lbsb[:, 1, :], lbsb[:, 0, :], ALU.subtract, r=["lbsb"], w=["lbsb"])
            b.act(lbsb[:, 1, :], lbsb[:, 1, :], AF.Sigmoid, r=["lbsb"], w=["lbsb"])
            b.memset(lbsb[:, 0, :], 0.0, w=["lbsb"], eng="dve")
            b.ts(omlsb[:], lbsb[:], -1.0, 1.0, ALU.mult, ALU.add, r=["lbsb"], w=["omlsb"])
            b.ts1(nbf[:], nbf[:], -1.0, ALU.mult, r=["nbf"], w=["nbf"])
            b.ts1(qg[:], qg[:], 0.125, ALU.mult, r=["qg"], w=["qg"])
            modps = banks[0]
            k = 0
            for l in range(n_layers):
                for jb in range(6):
                    st = adast[k % 2]
                    key = "adast%d" % (k % 2)
                    k += 1
                    b.dma(st[:], ada_w[l].rearrange("(kc p) j -> p kc j", p=128)[:, :, jb * 512:(jb + 1) * 512],
                          r=[], w=[key])
                    for jj in range(4):
                        col = l * 24 + jb * 4 + jj
                        for kc in range(KC):
                            b.mm(modps[:, col:col + 1], st[:, kc, jj * 128:(jj + 1) * 128], cact[:, kc:kc + 1],
                                 kc == 0, kc == KC - 1, r=[key, "cact"], w=["bank0"])
            nl = n_layers
            b.tt(modsb[:, 0:nl, :], modsb[:, 0:nl, :], modps[:, 0:nl * 24].rearrange("p (l j) -> p l j", j=24),
                 ALU.add, r=["bank0", "modsb"], w=["modsb"])
            b.stt(asb[:, 0:nl, :], modsb[:, 0:nl, 8:16], 1.0, gsb[:, 0:nl, :], ALU.add, ALU.mult,
                  r=["modsb", "gsb"], w=["asb"])
            P.flush()
        if stop_after == "p0":
            return nc

        for l in range(n_layers):
            is_hg = (l % 2 == 0)
            j = l // 2
            xsrc = xT if l == 0 else yT
            w_in = hg_w_in[j] if is_hg else fx_w_in[j]
            w_out = hg_w_out[j] if is_hg else fx_w_out[j]
            NCOL = 4096 if is_hg else 4112
            xsrc_v = xsrc.rearrange("(kc p) t -> p kc t", p=128)
            y_v = yT.rearrange("(kc p) t -> p kc t", p=128)

            with contextlib.ExitStack() as ph:
                win = sb(ph, "win", [128, KC, NCOL], BF16)
                wst = [sb(ph, "wst%d" % i, [128, 2048], F32) for i in range(2)]
                xblk = [sb(ph, "xblk%d" % i, [128, KC, 512], F32) for i in range(1)]
                hT = [sb(ph, "hT%d" % i, [128, KC, 512], BF16) for i in range(2)]
                sq = [sb(ph, "sq%d" % i, [128, 512], BF16) for i in range(2)]
                rstd = sb(ph, "rstd", [128, 512], F32)
                tmpf = [sb(ph, "tmpf%d" % i, [128, 512], F32) for i in range(4)]
                stb = [sb(ph, "stb%d" % i, [128, 512], BF16) for i in range(6)]
                stf = [sb(ph, "stf%d" % i, [128, 512], F32) for i in range(3)]
                ncum = [sb(ph, "ncum%d" % i, [16, 512], F32) for i in range(2)]
                c6 = [sb(ph, "c6_%d" % i, [16, 6, 512], BF16) for i in range(2)]
                cnt = {"tmpf": 0, "stb": 0, "stf": 0, "bank": 0, "wst": 0}

                def rot(name, lst):
                    i = cnt[name] % len(lst)
                    cnt[name] += 1
                    return lst[i], "%s%d" % (name, i)

                def pbank():
                    i = 1 + cnt["bank"] % 4
                    cnt["bank"] += 1
                    return banks[i], "bank%d" % i

                w_in_v = w_in.rearrange("(kc p) f -> p kc f", p=128)
                for kc in range(KC):
                    for c0 in range(0, NCOL, 2048):
                        c1 = min(NCOL, c0 + 2048)
                        st, key = rot("wst", wst)
                        b.dma(st[:, 0:c1 - c0], w_in_v[:, kc, c0:c1], r=[], w=[key])
                        b.copy(win[:, kc, c0:c1], st[:, 0:c1 - c0], r=[key], w=["win"], eng="pool")
                w_out_v = w_out.rearrange("(kc p) f -> p kc f", p=128)
                for kc in range(0, KC, 2):
                    st, key = rot("wst", wst)
                    b.dma(st[:].rearrange("p (a f) -> p a f", a=2), w_out_v[:, kc:kc + 2, :], r=[], w=[key])
                    b.copy(wout[:, kc:kc + 2, :], st[:].rearrange("p (a f) -> p a f", a=2), r=[key], w=["wout"],
                           eng="pool")

                for tb in range(NB):
                    ts_ = slice(tb * 512, (tb + 1) * 512)
                    xb, xk = xblk[0], "xblk0"
                    hb, hk = hT[tb % 2], "hT%d" % (tb % 2)
                    if tb == 0:
                        b.dma(xb[:], xsrc_v[:, :, ts_], r=[("x", tb)], w=[xk])
                    ssp = banks[0]
                    for kc in range(KC):
                        b.act(sq[kc % 2][:], xb[:, kc, :], AF.Square, r=[xk], w=["sq%d" % (kc % 2)])
                        b.mm(ssp[:], ones_bf[:], sq[kc % 2][:], kc == 0, kc == KC - 1,
                             r=["sq%d" % (kc % 2), "ones_bf"], w=["bank0"])
                    b.act(rstd[:], ssp[:], AF.Sqrt, r=["bank0"], w=["rstd"], scale=1.0 / D, bias=EPS)
                    b.recip(rstd[:], rstd[:], r=["rstd"], w=["rstd"])
                    for kc in range(KC):
                        t, tk = rot("tmpf", tmpf)
                        b.stt(t[:], xb[:, kc, :], asb[:, l, kc:kc + 1], rstd[:], ALU.mult, ALU.mult,
                              r=[xk, "asb", "rstd"], w=[tk])
                        b.act(hb[:, kc, :], t[:], AF.Identity, r=[tk, "modsb"], w=[hk],
                              bias=modsb[:, l, kc:kc + 1])

                    if tb + 1 < NB:
                        b.dma(xb[:], xsrc_v[:, :, (tb + 1) * 512:(tb + 2) * 512], r=[("x", tb + 1)], w=[xk])

                    def proj_fm(c0, M=128):
                        pb, pk = pbank()
                        for kc in range(KC):
                            b.mm(pb[0:M, :], win[:, kc, c0:c0 + M], hb[:, kc, :], kc == 0, kc == KC - 1,
                                 r=["win", hk], w=[pk])
                        return pb, pk

                    if is_hg:
                        for h in range(8):
                            pb, pk = proj_fm(h * 128)
                            s, sk = rot("stb", stb)
                            b.act(s[:], pb[:], AF.Copy, r=[pk], w=[sk])
                            b.dma(sA[h][:, ts_], s[:], r=[sk], w=[("sA", h, tb)], eng="pool")
                            pb, pk = proj_fm(1024 + h * 128)
                            t, tk = rot("tmpf", tmpf)
                            b.act(t[:], pb[:], AF.Sigmoid, r=[pk], w=[tk])
                            b.ts(t[:], t[:], omlsb[:, j, h:h + 1], lbsb[:, j, h:h + 1], ALU.mult, ALU.add,
                                 r=[tk, "omlsb", "lbsb"], w=[tk])
                            g_, gk = rot("stf", stf)
                            b.act(g_[:], t[:], AF.Ln, r=[tk], w=[gk])
                            b.dma(sG[h][:, ts_], g_[:], r=[gk], w=[("sG", h, tb)], eng="pool")
                            s, sk = rot("stb", stb)
                            b.ts(s[:], t[:], -1.0, 1.0, ALU.mult, ALU.add, r=[tk], w=[sk])
                            b.dma(sK[h][:, ts_], s[:], r=[sk], w=[("sK", h, tb)], eng="pool")
                    else:
                        for hp in range(8):
                            for (c0, dst, gv, nm) in ((hp * 128, sA, qg, "sA"), (1024 + hp * 128, sK, kg, "sK")):
                                pb, pk = proj_fm(c0)
                                s2, s2k = rot("stb", stb)
                                b.act(s2[:], pb[:], AF.Square, r=[pk], w=[s2k])
                                t, tk = rot("tmpf", tmpf)
                                b.copy(t[:], pb[:], r=[pk], w=[tk])
                                b.mm(banks[5][:], blk64[:], s2[:], True, True, r=[s2k, "blk64"], w=["bank5"])
                                r2, r2k = rot("tmpf", tmpf)
                                b.act(r2[:], banks[5][:], AF.Sqrt, r=["bank5"], w=[r2k], scale=1.0 / 64, bias=EPS)
                                b.recip(r2[:], r2[:], r=[r2k], w=[r2k])
                                s, sk = rot("stb", stb)
                                b.stt(s[:], t[:], gv[:, j:j + 1], r2[:], ALU.mult, ALU.mult,
                                      r=[tk, r2k, "qg", "kg"], w=[sk])
                                b.dma(dst[hp][:, ts_], s[:], r=[sk], w=[(nm, hp, tb)], eng="pool")
                        pb, pk = proj_fm(4096, M=16)
                        t, tk = rot("tmpf", tmpf)
                        b.act(t[0:16, :], pb[0:16, :], AF.Exp, r=[pk, "nbf"], w=[tk], scale=-1.0, bias=nbf[:, j:j + 1])
                        b.act(t[0:16, :], t[0:16, :], AF.Ln, r=[tk], w=[tk], scale=1.0, bias=1.0)
                        nc_, nk = ncum[tb % 2], "ncum%d" % (tb % 2)
                        pc_, pck = ncum[(tb + 1) % 2], "ncum%d" % ((tb + 1) % 2)
                        init = 0.0 if tb == 0 else pc_[:, 511:512]
                        b.scan(nc_[:], onesf[0:16, :], t[0:16, :], init, r=[tk, pck, "onesf"], w=[nk])
                        cc, ck = c6[tb % 2], "c6_%d" % (tb % 2)
                        r1, r1k = rot("tmpf", tmpf)
                        b.copy(cc[:, 3, :], nc_[:], r=[nk], w=[ck])
                        b.tt(r1[0:16, :], nc_[:], cc[:, 3, :], ALU.subtract, r=[nk, ck], w=[r1k])
                        b.copy(cc[:, 4, :], r1[0:16, :], r=[r1k], w=[ck])
                        b.tt(r1[0:16, :], r1[0:16, :], cc[:, 4, :], ALU.subtract, r=[r1k, ck], w=[r1k])
                        b.copy(cc[:, 5, :], r1[0:16, :], r=[r1k], w=[ck])
                        b.ts1(cc[:, 0:3, :], cc[:, 3:6, :], -1.0, ALU.mult, r=[ck], w=[ck])
                        b.dma(sC[:, :, ts_], cc[:], r=[ck], w=[("sC", tb)], eng="pool")

                    for tt_ in range(4):
                        for half in range(2):
                            pb, pk = pbank()
                            for kc in range(KC):
                                b.mm(pb[:], hb[:, kc, tt_ * 128:(tt_ + 1) * 128],
                                     win[:, kc, 2048 + half * 512:2048 + (half + 1) * 512], kc == 0, kc == KC - 1,
                                     r=["win", hk], w=[pk])
                            s, sk = rot("stb", stb)
                            b.act(s[:], pb[:], AF.Copy, r=[pk], w=[sk])
                            b.dma(sV[tb * 4 + tt_][:, half * 512:(half + 1) * 512], s[:], r=[sk],
                                  w=[("sV", tb * 4 + tt_, half)], eng="pool")
                    for h in range(8):
                        pb, pk = proj_fm(3072 + h * 128)
                        s, sk = rot("stb", stb)
                        b.act(s[:], pb[:], AF.Silu, r=[pk], w=[sk])
                        b.dma(sZ[h][:, ts_], s[:], r=[sk], w=[("sZ", h, tb)], eng="pool")
                P.flush()
            if stop_after == "A%d" % l:
                return nc

            with contextlib.ExitStack() as phOT:
                OT = sb(phOT, "OT", [128, KC, S], BF16)
                if is_hg:
                    with contextlib.ExitStack() as ph:
                        SEG = 256
                        NSEG = S // SEG
                        Ql = [sb(ph, "Ql%d" % i, [128, 8, SEG], BF16) for i in range(2)]
                        Kl = [sb(ph, "Kl%d" % i, [128, 8, SEG], BF16) for i in range(2)]
                        Gl = [sb(ph, "Gl%d" % i, [128, 8, SEG], F32) for i in range(2)]
                        Vl = [sb(ph, "Vl%d" % i, [128, 2, 1024], BF16) for i in range(2)]
                        Zl = [sb(ph, "Zl%d" % i, [128, 8, SEG], BF16) for i in range(2)]
                        rmask = sb(ph, "rmask", [128, 4 * SEG], F32)
                        bcum = sb(ph, "bcum", [128, 4 * SEG], F32)
                        ebt = sb(ph, "ebt", [128, 4 * SEG], F32)
                        tmpe = sb(ph, "tmpe", [128, 4 * SEG], F32)
                        qd = sb(ph, "qd", [128, 4 * SEG], BF16)
                        kinv = sb(ph, "kinv", [128, 4 * SEG], BF16)
                        kdT = sb(ph, "kdT", [128, 4 * SEG], BF16)
                        ATs = [sb(ph, "ATs%d" % i, [128, 128], BF16) for i in range(4)]
                        kdk = [sb(ph, "kdk%d" % i, [128, 128], BF16) for i in range(4)]
                        Sf = sb(ph, "Sf", [128, 8, 128], F32)
                        Sb = sb(ph, "Sb", [128, 8, 128], BF16)
                        of = [sb(ph, "of%d" % i, [128, SEG], F32) for i in range(2)]
                        osq = [sb(ph, "osq%d" % i, [128, SEG], BF16) for i in range(2)]
                        orst = [sb(ph, "orst%d" % i, [128, SEG], F32) for i in range(2)]
                        b.memset(rmask[:], 1.0, w=["rmask"])
                        b.memset(rmask[:].rearrange("p (c t) -> p c t", t=32)[:, :, 0:1], 0.0, w=["rmask"])
                        b.memset(Sf[:], 0.0, w=["Sf"])
                        b.memset(Sb[:], 0.0, w=["Sb"])
                        rc = {"at": 0, "ats": 0, "kdk": 0, "u": 0, "tp": 0, "post": 0}
                        for sg in range(NSEG):
                            sl = slice(sg * SEG, (sg + 1) * SEG)
                            q_, k_, g_, v_, z_ = Ql[sg % 2], Kl[sg % 2], Gl[sg % 2], Vl[sg % 2], Zl[sg % 2]
                            lk = "L%d" % (sg % 2)
                            tbk = sg * SEG // 512
                            b.dma(q_[:], sA.rearrange("h p t -> p h t")[:, :, sl],
                                  r=[("sA", h, tbk) for h in range(8)], w=[lk + "q"])
                            b.dma(k_[:], sK.rearrange("h p t -> p h t")[:, :, sl],
                                  r=[("sK", h, tbk) for h in range(8)], w=[lk + "k"])
                            b.dma(g_[:], sG.rearrange("h p t -> p h t")[:, :, sl],
                                  r=[("sG", h, tbk) for h in range(8)], w=[lk + "g"])
                            b.dma(v_[:], sV.rearrange("n p f -> p n f")[:, sg * 2:sg * 2 + 2, :],
                                  r=[("sV", sg * 2 + a, hh) for a in range(2) for hh in range(2)], w=[lk + "v"])
                            b.dma(z_[:], sZ.rearrange("h p t -> p h t")[:, :, sl],
                                  r=[("sZ", h, tbk) for h in range(8)], w=[lk + "z"])
                            for hg in range(2):
                                hs = slice(hg * 4, hg * 4 + 4)
                                gk = "hg"
                                qv = q_[:, hs, :]
                                kv = k_[:, hs, :]
                                gv = g_[:, hs, :]
                                v3 = lambda t_: t_[:].rearrange("p (h t) -> p h t", h=4)
                                for hh in range(4):
                                    b.scan(bcum[:, hh * SEG:(hh + 1) * SEG], rmask[:, hh * SEG:(hh + 1) * SEG],
                                           g_[:, hg * 4 + hh, :], 0.0, r=[lk + "g", "rmask"], w=["bcum"])
                                b.act(ebt[:], bcum[:], AF.Exp, r=["bcum"], w=["ebt"])
                                b.tt(v3(qd), qv, v3(ebt), ALU.mult, r=[lk + "q", "ebt"], w=["qd"])
                                b.act(tmpe[:], bcum[:], AF.Exp, r=["bcum"], w=["tmpe"], scale=-1.0)
                                b.tt(v3(kinv), kv, v3(tmpe), ALU.mult, r=[lk + "k", "tmpe"], w=["kinv"])
                                bc3 = bcum[:].rearrange("p (c t) -> p c t", t=32)
                                lastb = bc3[:, :, 31:32].to_broadcast([128, 4 * SEG // 32, 32])
                                b.tt(tmpe[:].rearrange("p (c t) -> p c t", t=32), lastb, bc3, ALU.subtract,
                                     r=["bcum", "tmpe"], w=["tmpe"])
                                b.act(tmpe[:], tmpe[:], AF.Exp, r=["tmpe"], w=["tmpe"])
                                b.tt(v3(kdT), kv, v3(tmpe), ALU.mult, r=[lk + "k", "tmpe"], w=["kdT"])
                                eb3 = ebt[:].rearrange("p (c t) -> p c t", t=32)
                                for tl in range(SEG // 128):
                                    for hh in range(4):
                                        h = hg * 4 + hh
                                        cs = slice(hh * SEG + tl * 128, hh * SEG + (tl + 1) * 128)
                                        ai = rc["at"] % 2
                                        rc["at"] += 1
                                        atp = banks[4][:, ai * 128:(ai + 1) * 128]
                                        b.mm(atp, kinv[:, cs], qd[:, cs], True, True, r=["kinv", "qd"],
                                             w=["bank4"])
                                        asi = rc["ats"] % 4
                                        rc["ats"] += 1
                                        b.tt(ATs[asi][:], atp, mask01[:], ALU.mult, r=["bank4", "mask01"],
                                             w=[("ATs", asi)])
                                        ti = rc["tp"] % 8
                                        rc["tp"] += 1
                                        tpp = bankT[:, ti * 128:(ti + 1) * 128]
                                        b.tr(tpp, kdT[:, cs], ident[:], r=["kdT", "ident"], w=["bankT"])
                                        ki = rc["kdk"] % 4
                                        rc["kdk"] += 1
                                        b.act(kdk[ki][:], tpp, AF.Copy, r=["bankT"], w=[("kdk", ki)])
                                        ob = banks[hh]
                                        oc = slice(tl * 128, (tl + 1) * 128)
                                        b.mm(ob[:, oc], v_[:, tl, h * 128:(h + 1) * 128], ATs[asi][:], True, False,
                                             r=[lk + "v", ("ATs", asi)], w=["bank%d" % hh])
                                    for jj in range(4):
                                        for hh in range(4):
                                            h = hg * 4 + hh
                                            ob = banks[hh]
                                            cj = hh * SEG + tl * 128 + jj * 32
                                            ki = (rc["kdk"] - 4 + hh) % 4
                                            b.mm(ob[:, tl * 128 + jj * 32: tl * 128 + jj * 32 + 32],
                                                 Sb[:, h, :], qd[:, cj:cj + 32], False, jj == 3,
                                                 r=[("Sb", h), "qd"], w=["bank%d" % hh])
                                            ui = rc["u"] % 2
                                            rc["u"] += 1
                                            up = banks[5 + ui][:, 0:128]
                                            tkw = {"tile_position": (96, 0)} if jj == 3 else {}
                                            b.mm(up, kdk[ki][32 * jj:32 * jj + 32, :],
                                                 v_[32 * jj:32 * jj + 32, tl, h * 128:(h + 1) * 128], True, True,
                                                 r=[("kdk", ki), lk + "v"], w=["bank%d" % (5 + ui)], **tkw)
                                            chunk = (hh * SEG + tl * 128 + jj * 32) // 32
                                            b.stt(Sf[:, h, :], Sf[:, h, :], eb3[:, chunk, 31:32], up, ALU.mult, ALU.add,
                                                  r=[("Sf", h), "ebt", "bank%d" % (5 + ui)], w=[("Sf", h)])
                                            b.act(Sb[:, h, :], Sf[:, h, :], AF.Copy, r=[("Sf", h)], w=[("Sb", h)])
                                for hh in range(4):
                                    h = hg * 4 + hh
                                    pi = rc["post"] % 2
                                    rc["post"] += 1
                                    ob = banks[hh]
                                    b.act(osq[pi][:], ob[:, 0:SEG], AF.Square, r=["bank%d" % hh], w=[("osq", pi)])
                                    b.copy(of[pi][:], ob[:, 0:SEG], r=["bank%d" % hh], w=[("of", pi)])
                                    ssb = banks[4][:, 256:256 + SEG]
                                    b.mm(ssb, ones_bf[:], osq[pi][:], True, True, r=[("osq", pi), "ones_bf"],
                                         w=["bank4"])
                                    b.act(orst[pi][:], ssb, AF.Sqrt, r=["bank4"], w=[("orst", pi)],
                                          scale=1.0 / 128, bias=EPS)
                                    b.recip(orst[pi][:], orst[pi][:], r=[("orst", pi)], w=[("orst", pi)])
                                    b.stt(of[pi][:], of[pi][:], ogsb[:, j, h:h + 1], orst[pi][:], ALU.mult, ALU.mult,
                                          r=[("of", pi), ("orst", pi), "ogsb"], w=[("of", pi)])
                                    b.tt(OT[:, h, sl], of[pi][:], z_[:, h, :], ALU.mult, r=[("of", pi), lk + "z"],
                                         w=[("OT", sg * SEG // 512)], eng="pool")
                        P.flush()
                else:
                    with contextlib.ExitStack() as ph:
                        qa = [sb(ph, "qa%d" % i, [70, S], BF16) for i in range(2)]
                        ka = [sb(ph, "ka%d" % i, [70, S], BF16) for i in range(2)]
                        va = [sb(ph, "va%d" % i, [128, NT, 128], BF16) for i in range(2)]
                        zt = [sb(ph, "zt%d" % i, [64, S], BF16) for i in range(2)]
                        PT = [sb(ph, "PT%d" % i, [128, 512], BF16) for i in range(4)]
                        rl = [sb(ph, "rl%d" % i, [64, 512], F32) for i in range(2)]
                        t1 = [sb(ph, "t1%d" % i, [64, 512], F32) for i in range(2)]
                        for i in range(2):
                            b.memset(qa[i][64:70, :], 1.0, w=["qa%d" % i])
                            b.memset(ka[i][64:70, :], 1.0, w=["ka%d" % i])
                            b.memset(va[i][:, :, 64:128], 1.0, w=["va%d" % i])
                        rc = {"st": 0, "pt": 0, "oa": 0, "fin": 0}

                        def load_head(h):
                            hp, ho = h // 2, (h % 2) * 64
                            i = h % 2
                            allA = [("sA", hp, tb) for tb in range(NB)]
                            allK = [("sK", hp, tb) for tb in range(NB)]
                            allC = [("sC", tb) for tb in range(NB)]
                            b.dma(qa[i][0:64, :], sA[hp][ho:ho + 64, :], r=allA, w=["qa%d" % i])
                            b.dma(qa[i][64:67, :], sC[h, 0:3, :], r=allC, w=["qa%d" % i])
                            b.dma(ka[i][0:64, :], sK[hp][ho:ho + 64, :], r=allK, w=["ka%d" % i])
                            b.dma(ka[i][67:70, :], sC[h, 3:6, :], r=allC, w=["ka%d" % i])
                            for n0 in range(0, NT, 8):
                                n1 = min(NT, n0 + 8)
                                b.dma(va[i][:, n0:n1, 0:64],
                                      sV.rearrange("n p f -> p n f")[:, n0:n1, h * 64:(h + 1) * 64],
                                      r=[("sV", n, hh) for n in range(n0, n1) for hh in range(2)], w=["va%d" % i])
                            b.dma(zt[i][:, :], sZ[hp][ho:ho + 64, :], r=[("sZ", hp, tb) for tb in range(NB)],
                                  w=["zt%d" % i])

                        steps = [(h, qs, kb) for h in range(16) for qs in range(NB) for kb in range(4 * qs + 4)]
                        info = {}
                        span_oa = {}
                        LOOK = 2

                        def emit_qk(idx):
                            h, qs, kb = steps[idx]
                            i = h % 2
                            jd = kb - 4 * qs
                            c0 = 128 * jd if jd > 0 else 0
                            si = rc["st"] % 3
                            rc["st"] += 1
                            ST = banks[si]
                            b.mm(ST[:, c0:512], ka[i][0:70, kb * 128:(kb + 1) * 128],
                                 qa[i][0:70, qs * 512 + c0:(qs + 1) * 512], True, jd < 0,
                                 r=["ka%d" % i, "qa%d" % i], w=["bank%d" % si])
                            if jd >= 0:
                                b.mm(ST[:, c0:c0 + 128], ident[:], maskbias[:], False, True,
                                     r=["ident", "maskbias"], w=["bank%d" % si])
                            pi = rc["pt"] % 4
                            rc["pt"] += 1
                            b.act(PT[pi][:, c0:512], ST[:, c0:512], AF.Exp, r=["bank%d" % si], w=[("PT", pi)])
                            info[idx] = (c0, pi)

                        def emit_pv(idx):
                            h, qs, kb = steps[idx]
                            hp, ho = h // 2, (h % 2) * 64
                            i = h % 2
                            c0, pi = info.pop(idx)
                            nkb = 4 * qs + 4
                            if kb == 0:
                                span_oa[(h, qs)] = rc["oa"] % 2
                                rc["oa"] += 1
                            oi = span_oa[(h, qs)]
                            OA = banks[3 + oi]
                            b.mm(OA[:, c0:512], va[i][:, kb, :], PT[pi][:, c0:512], kb == 0, kb == nkb - 1,
                                 r=["va%d" % i, ("PT", pi)], w=["bank%d" % (3 + oi)])
                            if kb == nkb - 1:
                                fi = rc["fin"] % 2
                                rc["fin"] += 1
                                b.recip(rl[fi][:], OA[64:128, :], r=["bank%d" % (3 + oi)], w=[("rl", fi)])
                                b.tt(t1[fi][:], OA[0:64, :], rl[fi][:], ALU.mult, r=["bank%d" % (3 + oi), ("rl", fi)],
                                     w=[("t1", fi)])
                                b.tt(OT[ho:ho + 64, hp, qs * 512:(qs + 1) * 512], t1[fi][:],
                                     zt[i][:, qs * 512:(qs + 1) * 512], ALU.mult, r=[("t1", fi), "zt%d" % i],
                                     w=[("OT", qs)], eng="pool")
                                if qs == NB - 1 and h + 2 < 16:
                                    load_head(h + 2)

                        load_head(0)
                        load_head(1)
                        for idx in range(len(steps) + LOOK):
                            if idx < len(steps):
                                emit_qk(idx)
                            if idx - LOOK >= 0:
                                emit_pv(idx - LOOK)
                        P.flush()

                if stop_after == "B%d" % l:
                    return nc
                with contextlib.ExitStack() as ph:
                    xc = [sb(ph, "xc%d" % i, [128, KC, 512], F32) for i in range(2)]
                    nb_ = 0
                    for tb in range(NB):
                        ts_ = slice(tb * 512, (tb + 1) * 512)
                        xb, xk = xc[tb % 2], "xc%d" % (tb % 2)
                        if tb == 0:
                            b.dma(xb[:], xsrc_v[:, :, ts_], r=[("x", tb)], w=[xk])
                        if tb + 1 < NB:
                            b.dma(xc[(tb + 1) % 2][:], xsrc_v[:, :, (tb + 1) * 512:(tb + 2) * 512],
                                  r=[("x", tb + 1)], w=["xc%d" % ((tb + 1) % 2)])
                        for dc in range(KC):
                            bi = nb_ % 4
                            nb_ += 1
                            pb, pk = banks[bi], "bank%d" % bi
                            for fc in range(KC):
                                b.mm(pb[:], wout[:, fc, dc * 128:(dc + 1) * 128], OT[:, fc, ts_], fc == 0, fc == KC - 1,
                                     r=["wout", ("OT", tb)], w=[pk])
                            b.stt(xb[:, dc, :], pb[:], modsb[:, l, 16 + dc:17 + dc], xb[:, dc, :], ALU.mult, ALU.add,
                                  r=[pk, "modsb", xk], w=[xk])
                        b.dma(y_v[:, :, ts_], xb[:], r=[xk], w=[("x", tb)], eng="pool")
                    P.flush()
    return nc


def _consts():
    s = np.arange(128)[:, None]
    t = np.arange(128)[None, :]
    maskbias = np.where(s > t, NEG, 0.0).astype(np.float32)
    mask01 = ((s // 32 == t // 32) & (s <= t)).astype(np.float32)
    mj = (s // 32 == np.arange(4)[None, :]).astype(np.float32)
    blk64 = (s // 64 == t // 64).astype(np.float32)
    return dict(c_maskbias=maskbias, c_mask01=mask01, c_mj=mj, c_blk64=blk64)


def _pcol(v, n):
    return np.ascontiguousarray(np.asarray(v, np.float32).reshape(n, 128).T)


def make_in_maps(inputs, n_cores, S):
    f = lambda a: np.ascontiguousarray(np.asarray(a, dtype=np.float32))
    x = f(inputs["x"])
    c = f(inputs["c"])
    shared = dict(
        gT=np.ascontiguousarray(np.stack([_pcol(g, 8) for g in f(inputs["norm_g"])], 1)),
        ada_w=f(inputs["ada_w"]),
        ada_bT=np.ascontiguousarray(np.stack([_pcol(v, 24) for v in f(inputs["ada_b"])], 1)),
        lbT=np.ascontiguousarray(np.stack([_pcol(v, 8) for v in f(inputs["hg_lb_logits"])], 1)),
        hg_w_in=f(inputs["hg_w_in"]),
        ogT=np.ascontiguousarray(np.stack([_pcol(v, 8) for v in f(inputs["hg_o_g"])], 1)),
        hg_w_out=f(inputs["hg_w_out"]),
        fx_w_in=f(inputs["fx_w_in"]),
        bfT=np.ascontiguousarray(f(inputs["fx_b_f"]).T),
        qgT=np.ascontiguousarray(np.tile(f(inputs["fx_q_g"]).T, (2, 1))),
        kgT=np.ascontiguousarray(np.tile(f(inputs["fx_k_g"]).T, (2, 1))),
        fx_w_out=f(inputs["fx_w_out"]),
    )
    shared.update(_consts())
    maps = []
    for i in range(n_cores):
        m = dict(shared)
        m["xT"] = np.ascontiguousarray(x[i].T)
        m["cT"] = _pcol(c[i], 8)
        maps.append(m)
    return maps


_NC_CACHE = {}


def run(inputs, n_layers=4, stop_after=None):
    x = np.asarray(inputs["x"])
    Bn, S, _ = x.shape
    key = (S, n_layers, stop_after)
    if key not in _NC_CACHE:
        _NC_CACHE[key] = build(S, n_layers, stop_after)
    nc = _NC_CACHE[key]
    maps = make_in_maps(inputs, Bn, S)
    res = run_bass_kernel_spmd(nc, maps, core_ids=list(range(Bn)))
    out = np.stack([np.ascontiguousarray(res.results[i]["yT"].T) for i in range(Bn)], 0)
    return out.astype(np.float32)


def kernel(**inputs):
    return run(inputs, n_layers=4)
```

```python
import contextlib
import numpy as np
import concourse.bass as bass
import concourse.mybir as mybir
from concourse.bass_utils import run_bass_kernel_spmd

F32 = mybir.dt.float32
BF16 = mybir.dt.bfloat16
ALU = mybir.AluOpType
AF = mybir.ActivationFunctionType

ENGS = ("pe", "act", "dve", "pool", "sp")
D = 1024
KC = 8
EPS = 1e-6
SAME_ENG_SYNC = False
NEG = -30000.0


class Prog:
    def __init__(self, nc, es, n_dma_sems=40):
        self.nc = nc
        self.ops = []
        self.buf = {}
        self.n_dma_sems = n_dma_sems
        self.dma_rr = 0
        self.dma_rr_e = {}
        self.slot_ord = [0] * n_dma_sems
        self.slot_last = {}
        self.flushed = 0
        self.cnt = {e: 0 for e in ENGS}
        self.esem = {e: es.enter_context(nc.semaphore("s_" + e)) for e in ENGS}
        self.dsem = [es.enter_context(nc.semaphore("d_%d" % k)) for k in range(n_dma_sems)]
        self.nflush = 0

    def op(self, eng, fn, r=(), w=(), dma=False):
        pk_ = [k for k in r if isinstance(k, str) and k.startswith("bank")]
        if pk_:
            w = list(w) + [k for k in pk_ if k not in w]
            r = [k for k in r if k not in pk_]
        deps = set()
        for b in r:
            st = self.buf.get(b)
            if st is not None and st[0] is not None:
                deps.add(st[0])
        for b in w:
            st = self.buf.get(b)
            if st is not None:
                if st[0] is not None:
                    deps.add(st[0])
                deps.update(st[1])
        i = len(self.ops)
        o = dict(eng=eng, fn=fn, deps=deps, dma=dma, slot=None, ordinal=None, count=None)
        if dma:
            lo_, hi_ = (0, 28) if eng == "sp" else (28, self.n_dma_sems)
            k_ = self.dma_rr_e.get(eng, 0)
            self.dma_rr_e[eng] = k_ + 1
            s = lo_ + k_ % (hi_ - lo_)
            prev = self.slot_last.get(s)
            if prev is not None:
                deps.add(prev)
            self.slot_ord[s] += 1
            o["ordinal"] = self.slot_ord[s]
            o["slot"] = s
            self.slot_last[s] = i
        self.ops.append(o)
        for b in r:
            st = self.buf.setdefault(b, [None, []])
            st[1].append(i)
        for b in w:
            self.buf[b] = [i, []]
        return i

    def flush(self):
        nc = self.nc
        ops = self.ops
        lo, hi = self.flushed, len(ops)
        if lo == hi:
            return
        base_cnt = dict(self.cnt)
        base_ord = list(self.slot_ord_flushed) if hasattr(self, "slot_ord_flushed") else [0] * self.n_dma_sems
        waited = {e: {f: -1 for f in ENGS} for e in ENGS}
        waited_dma = {e: {} for e in ENGS}
        needed = {}
        for i in range(lo, hi):
            o = ops[i]
            e = o["eng"]
            wl = []
            for d in sorted(o["deps"]):
                if d < lo:
                    continue
                od = ops[d]
                if od["dma"]:
                    if waited_dma[e].get(od["slot"], 0) >= od["ordinal"]:
                        continue
                    waited_dma[e][od["slot"]] = od["ordinal"]
                    wl.append(("dma", od["slot"], od["ordinal"]))
                else:
                    f = od["eng"]
                    if f == e and (e == "pe" or not SAME_ENG_SYNC):
                        continue
                    if waited[e][f] >= d:
                        continue
                    waited[e][f] = d
                    needed[d] = True
                    wl.append(("eng", f, d))
            o["waits"] = wl
        last_of = {}
        for i in range(lo, hi):
            if not ops[i]["dma"]:
                last_of[ops[i]["eng"]] = i
        for e, i in last_of.items():
            needed[i] = True
        for i in range(lo, hi):
            o = ops[i]
            if o["dma"]:
                continue
            if needed.get(i):
                self.cnt[o["eng"]] += 1
                o["count"] = self.cnt[o["eng"]]
        per_eng = {e: [i for i in range(lo, hi) if ops[i]["eng"] == e] for e in ENGS}
        esem, dsem = self.esem, self.dsem
        first = self.nflush == 0
        self.nflush += 1
        final_waits = {e: [] for e in ENGS}
        cur_last = {}
        for i in range(lo, hi):
            if ops[i]["dma"]:
                cur_last[ops[i]["slot"]] = i
        for s, i in cur_last.items():
            final_waits[ops[i]["eng"]].append((s, ops[i]["ordinal"]))

        def replay(ename, eng):
            if not first:
                for f in ENGS:
                    if f != ename and base_cnt[f] > 0:
                        eng.wait_ge(esem[f], base_cnt[f])
            for i in per_eng[ename]:
                o = ops[i]
                for wt in o["waits"]:
                    if wt[0] == "dma":
                        eng.wait_ge(dsem[wt[1]], 16 * wt[2])
                    else:
                        eng.wait_ge(esem[wt[1]], ops[wt[2]]["count"])
                ins = o["fn"](eng)
                if o["dma"]:
                    ins.then_inc(dsem[o["slot"]], 16)
                elif needed.get(i):
                    ins.then_inc(esem[ename], 1)
                o["fn"] = None
            for s, ordn in final_waits[ename]:
                eng.wait_ge(dsem[s], 16 * ordn)

        with nc.Block() as block:
            def wrap(ename):
                def f(eng):
                    replay(ename, eng)
                    if final_waits[ename]:
                        self.cnt[ename] += 1
                        eng.nop().then_inc(esem[ename], 1)
                return f
            block.tensor(wrap("pe"))
            block.scalar(wrap("act"))
            block.vector(wrap("dve"))
            block.gpsimd(wrap("pool"))
            block.sync(wrap("sp"))
        self.flushed = hi
        self.slot_ord_flushed = list(self.slot_ord)


class B:
    def __init__(self, P):
        self.P = P

    def dma(self, out, in_, r, w, eng="sp"):
        return self.P.op(eng, lambda e: e.dma_start(out=out, in_=in_), r=r, w=w, dma=True)

    def mm(self, out, lhsT, rhs, start, stop, r, w, **kw):
        return self.P.op("pe", lambda e: e.matmul(out, lhsT=lhsT, rhs=rhs, start=start, stop=stop,
                                                  skip_group_check=True, **kw), r=r, w=w)

    def tr(self, out, in_, ident, r, w):
        return self.P.op("pe", lambda e: e.transpose(out=out, in_=in_, identity=ident), r=r, w=w)

    def act(self, out, in_, func, r, w, scale=1.0, bias=0.0):
        return self.P.op("act", lambda e: e.activation(out=out, in_=in_, func=func, scale=scale, bias=bias), r=r, w=w)

    def tt(self, out, in0, in1, op, r, w, eng="dve"):
        return self.P.op(eng, lambda e: e.tensor_tensor(out=out, in0=in0, in1=in1, op=op), r=r, w=w)

    def ts(self, out, in0, s1, s2, op0, op1, r, w, eng="dve"):
        return self.P.op(eng, lambda e: e.tensor_scalar(out=out, in0=in0, scalar1=s1, scalar2=s2, op0=op0, op1=op1),
                         r=r, w=w)

    def ts1(self, out, in0, s1, op0, r, w, eng="dve"):
        return self.P.op(eng, lambda e: e.tensor_single_scalar(out=out, in_=in0, scalar=s1, op=op0), r=r, w=w)

    def stt(self, out, in0, scalar, in1, op0, op1, r, w, eng="dve"):
        return self.P.op(eng, lambda e: e.scalar_tensor_tensor(out=out, in0=in0, scalar=scalar, in1=in1,
                                                               op0=op0, op1=op1), r=r, w=w)

    def copy(self, out, in_, r, w, eng="dve"):
        return self.P.op(eng, lambda e: e.tensor_copy(out=out, in_=in_), r=r, w=w)

    def recip(self, out, in_, r, w):
        return self.P.op("dve", lambda e: e.reciprocal(out=out, in_=in_), r=r, w=w)

    def memset(self, ap, val, w, eng="pool"):
        return self.P.op(eng, lambda e: e.memset(ap, val), r=(), w=w)

    def scan(self, out, d0, d1, initial, r, w):
        return self.P.op("dve", lambda e: e.tensor_tensor_scan(out=out, data0=d0, data1=d1, initial=initial,
                                                               op0=ALU.mult, op1=ALU.add), r=r, w=w)


def build(S, n_layers, stop_after=None):
    NB = S // 512
    NT = S // 128
    nc = bass.Bass("TRN2", target_bir_lowering=False)

    def din(name, shape, dt=F32):
        return nc.dram_tensor(name, list(shape), dt, kind="ExternalInput").ap()

    def dscr(name, shape, dt):
        return nc.dram_tensor(name, list(shape), dt, kind="Internal").ap()

    xT = din("xT", [D, S])
    cT = din("cT", [128, KC])
    gT = din("gT", [128, 4, KC])
    ada_w = din("ada_w", [4, D, 3 * D])
    ada_bT = din("ada_bT", [128, 4, 24])
    lbT = din("lbT", [128, 2, 8])
    hg_w_in = din("hg_w_in", [2, D, 4096])
    ogT = din("ogT", [128, 2, 8])
    hg_w_out = din("hg_w_out", [2, D, D])
    fx_w_in = din("fx_w_in", [2, D, 4112])
    bfT = din("bfT", [16, 2])
    qgT = din("qgT", [128, 2])
    kgT = din("kgT", [128, 2])
    fx_w_out = din("fx_w_out", [2, D, D])
    c_maskbias = din("c_maskbias", [128, 128])
    c_mask01 = din("c_mask01", [128, 128])
    c_mj = din("c_mj", [128, 4])
    c_blk64 = din("c_blk64", [128, 128])
    yT = nc.dram_tensor("yT", [D, S], F32, kind="ExternalOutput").ap()

    sA = dscr("sA", [8, 128, S], BF16)
    sK = dscr("sK", [8, 128, S], BF16)
    sG = dscr("sG", [8, 128, S], F32)
    sV = dscr("sV", [NT, 128, 1024], BF16)
    sZ = dscr("sZ", [8, 128, S], BF16)
    sC = dscr("sC", [16, 6, S], BF16)

    with contextlib.ExitStack() as es:
        P = Prog(nc, es)
        b = B(P)

        uid = [0]

        def sb(stack, name, shape, dt):
            uid[0] += 1
            return stack.enter_context(nc.sbuf_tensor("%s_u%d" % (name, uid[0]), list(shape), dt))

        banks = [es.enter_context(nc.psum_tensor("bank%d" % i, [128, 512], F32)) for i in range(7)]
        bankT = es.enter_context(nc.psum_tensor("bankT", [128, 1024], BF16))
        ident = sb(es, "ident", [128, 128], BF16)
        ones_bf = sb(es, "ones_bf", [128, 128], BF16)
        blk64 = sb(es, "blk64", [128, 128], BF16)
        maskbias = sb(es, "maskbias", [128, 128], BF16)
        mask01 = sb(es, "mask01", [128, 128], F32)
        mj = sb(es, "mj", [128, 4], F32)
        ctmp = sb(es, "ctmp", [128, 128], F32)
        cact = sb(es, "cact", [128, KC], F32)
        gsb = sb(es, "gsb", [128, 4, KC], F32)
        modsb = sb(es, "modsb", [128, 4, 24], F32)
        asb = sb(es, "asb", [128, 4, KC], F32)
        lbsb = sb(es, "lbsb", [128, 2, 8], F32)
        omlsb = sb(es, "omlsb", [128, 2, 8], F32)
        ogsb = sb(es, "ogsb", [128, 2, 8], F32)
        nbf = sb(es, "nbf", [16, 2], F32)
        qg = sb(es, "qg", [128, 2], F32)
        kg = sb(es, "kg", [128, 2], F32)
        wout = sb(es, "wout", [128, KC, D], BF16)
        onesf = sb(es, "onesf", [128, 512], F32)
        win = sb(es, "win", [128, KC, 4112], BF16)
        wst = [sb(es, "wst%d" % i, [128, 2048], F32) for i in range(2)]
        sO = dscr("sO", [8, 128, S], BF16)
        wq = {"steps": [], "k": 0}

        def w_make(l_):
            is_hg_ = (l_ % 2 == 0)
            w_ = (hg_w_in[l_ // 2] if is_hg_ else fx_w_in[l_ // 2]).rearrange("(kc p) f -> p kc f", p=128)
            ncol = 4096 if is_hg_ else 4112
            for kc in range(KC):
                for c0 in range(0, ncol, 2048):
                    wq["steps"].append((w_, kc, c0, min(ncol, c0 + 2048)))

        def w_tick(n=1):
            for _ in range(n):
                pend = wq.get("pend")
                if pend is not None:
                    kc, c0, c1, st, key = pend
                    b.copy(win[:, kc, c0:c1], st[:, 0:c1 - c0], r=[key], w=["win"], eng="pool")
                    wq["pend"] = None
                if not wq["steps"]:
                    continue
                w_, kc, c0, c1 = wq["steps"].pop(0)
                k_ = wq["k"]
                wq["k"] += 1
                st, key = wst[k_ % 2], "wst%d" % (k_ % 2)
                b.dma(st[:, 0:c1 - c0], w_[:, kc, c0:c1], r=[], w=[key])
                wq["pend"] = (kc, c0, c1, st, key)

        def w_drain():
            while wq["steps"] or wq.get("pend") is not None:
                w_tick()

        with contextlib.ExitStack() as ph:
            adast = [sb(ph, "adast%d" % i, [128, KC, 512], F32) for i in range(2)]
            b.memset(ident[:], 0.0, w=["ident"])
            P.op("pool", lambda e: e.affine_select(out=ident[:], in_=ident[:], pattern=[[-1, 128]],
                                                   compare_op=ALU.not_equal, fill=1.0, base=0,
                                                   channel_multiplier=1), r=["ident"], w=["ident"])
            b.memset(ones_bf[:], 1.0, w=["ones_bf"])
            b.memset(onesf[:], 1.0, w=["onesf"])
            for (src, dst, nm) in ((c_maskbias, maskbias, "maskbias"), (c_blk64, blk64, "blk64")):
                b.dma(ctmp[:], src, r=[], w=["ctmp"])
                b.copy(dst[:], ctmp[:], r=["ctmp"], w=[nm])
            b.dma(mask01[:], c_mask01, r=[], w=["mask01"])
            b.dma(mj[:], c_mj, r=[], w=["mj"])
            b.dma(cact[:], cT, r=[], w=["cact"])
            b.dma(gsb[:], gT, r=[], w=["gsb"])
            b.dma(modsb[:], ada_bT, r=[], w=["modsb"])
            b.dma(lbsb[:], lbT, r=[], w=["lbsb"])
            b.dma(ogsb[:], ogT, r=[], w=["ogsb"])
            b.dma(nbf[:], bfT, r=[], w=["nbf"])
            b.dma(qg[:], qgT, r=[], w=["qg"])
            b.dma(kg[:], kgT, r=[], w=["kg"])
            b.act(cact[:], cact[:], AF.Silu, r=["cact"], w=["cact"])
            b.tt(lbsb[:, 1, :], lbsb[:, 1, :], lbsb[:, 0, :], ALU.subtract, r=["lbsb"], w=["lbsb"])
            b.act(lbsb[:, 1, :], lbsb[:, 1, :], AF.Sigmoid, r=["lbsb"], w=["lbsb"])
            b.memset(lbsb[:, 0, :], 0.0, w=["lbsb"], eng="dve")
            b.ts(omlsb[:], lbsb[:], -1.0, 1.0, ALU.mult, ALU.add, r=["lbsb"], w=["omlsb"])
            b.ts1(nbf[:], nbf[:], -1.0, ALU.mult, r=["nbf"], w=["nbf"])
            b.ts1(qg[:], qg[:], 0.125, ALU.mult, r=["qg"], w=["qg"])
            modps = banks[0]
            k = 0
            for l in range(n_layers):
                for jb in range(6):
                    st = adast[k % 2]
                    key = "adast%d" % (k % 2)
                    k += 1
                    b.dma(st[:], ada_w[l].rearrange("(kc p) j -> p kc j", p=128)[:, :, jb * 512:(jb + 1) * 512],
                          r=[], w=[key])
                    for jj in range(4):
                        col = l * 24 + jb * 4 + jj
                        for kc in range(KC):
                            b.mm(modps[:, col:col + 1], st[:, kc, jj * 128:(jj + 1) * 128], cact[:, kc:kc + 1],
                                 kc == 0, kc == KC - 1, r=[key, "cact"], w=["bank0"])
            nl = n_layers
            b.tt(modsb[:, 0:nl, :], modsb[:, 0:nl, :], modps[:, 0:nl * 24].rearrange("p (l j) -> p l j", j=24),
                 ALU.add, r=["bank0", "modsb"], w=["modsb"])
            b.stt(asb[:, 0:nl, :], modsb[:, 0:nl, 8:16], 1.0, gsb[:, 0:nl, :], ALU.add, ALU.mult,
                  r=["modsb", "gsb"], w=["asb"])
            P.flush()
        if stop_after == "p0":
            return nc

        for l in range(n_layers):
            is_hg = (l % 2 == 0)
            j = l // 2
            xsrc = xT if l == 0 else yT
            w_in = hg_w_in[j] if is_hg else fx_w_in[j]
            w_out = hg_w_out[j] if is_hg else fx_w_out[j]
            NCOL = 4096 if is_hg else 4112
            xsrc_v = xsrc.rearrange("(kc p) t -> p kc t", p=128)
            y_v = yT.rearrange("(kc p) t -> p kc t", p=128)

            with contextlib.ExitStack() as ph:
                xblk = [sb(ph, "xblk%d" % i, [128, KC, 512], F32) for i in range(1)]
                hT = [sb(ph, "hT%d" % i, [128, KC, 512], BF16) for i in range(2)]
                sq = [sb(ph, "sq%d" % i, [128, 512], BF16) for i in range(2)]
                rstd = sb(ph, "rstd", [128, 512], F32)
                tmpf = [sb(ph, "tmpf%d" % i, [128, 512], F32) for i in range(4)]
                stb = [sb(ph, "stb%d" % i, [128, 512], BF16) for i in range(6)]
                stf = [sb(ph, "stf%d" % i, [128, 512], F32) for i in range(3)]
                ncum = [sb(ph, "ncum%d" % i, [16, 512], F32) for i in range(2)]
                c6 = [sb(ph, "c6_%d" % i, [16, 6, 512], BF16) for i in range(2)]
                cnt = {"tmpf": 0, "stb": 0, "stf": 0, "bank": 0, "wst": 0}

                def rot(name, lst):
                    i = cnt[name] % len(lst)
                    cnt[name] += 1
                    return lst[i], "%s%d" % (name, i)

                def pbank():
                    i = 1 + cnt["bank"] % 4
                    cnt["bank"] += 1
                    return banks[i], "bank%d" % i

                if l == 0:
                    w_make(0)
                w_drain()
                w_out_v = w_out.rearrange("(kc p) f -> p kc f", p=128)
                for kc in range(0, KC, 2):
                    k_ = wq["k"]
                    wq["k"] += 1
                    st, key = wst[k_ % 2], "wst%d" % (k_ % 2)
                    b.dma(st[:].rearrange("p (a f) -> p a f", a=2), w_out_v[:, kc:kc + 2, :], r=[], w=[key])
                    b.copy(wout[:, kc:kc + 2, :], st[:].rearrange("p (a f) -> p a f", a=2), r=[key], w=["wout"],
                           eng="pool")

                for tb in range(NB):
                    ts_ = slice(tb * 512, (tb + 1) * 512)
                    xb, xk = xblk[0], "xblk0"
                    hb, hk = hT[tb % 2], "hT%d" % (tb % 2)
                    if tb == 0:
                        b.dma(xb[:], xsrc_v[:, :, ts_], r=[("x", tb)], w=[xk])
                    ssp = banks[0]
                    for kc in range(KC):
                        b.act(sq[kc % 2][:], xb[:, kc, :], AF.Square, r=[xk], w=["sq%d" % (kc % 2)])
                        b.mm(ssp[:], ones_bf[:], sq[kc % 2][:], kc == 0, kc == KC - 1,
                             r=["sq%d" % (kc % 2), "ones_bf"], w=["bank0"])
                    b.act(rstd[:], ssp[:], AF.Sqrt, r=["bank0"], w=["rstd"], scale=1.0 / D, bias=EPS)
                    b.recip(rstd[:], rstd[:], r=["rstd"], w=["rstd"])
                    for kc in range(KC):
                        t, tk = rot("tmpf", tmpf)
                        b.stt(t[:], xb[:, kc, :], asb[:, l, kc:kc + 1], rstd[:], ALU.mult, ALU.mult,
                              r=[xk, "asb", "rstd"], w=[tk])
                        b.act(hb[:, kc, :], t[:], AF.Identity, r=[tk, "modsb"], w=[hk],
                              bias=modsb[:, l, kc:kc + 1])

                    if tb + 1 < NB:
                        b.dma(xb[:], xsrc_v[:, :, (tb + 1) * 512:(tb + 2) * 512], r=[("x", tb + 1)], w=[xk])

                    def proj_fm(c0, M=128):
                        pb, pk = pbank()
                        for kc in range(KC):
                            b.mm(pb[0:M, :], win[:, kc, c0:c0 + M], hb[:, kc, :], kc == 0, kc == KC - 1,
                                 r=["win", hk], w=[pk])
                        return pb, pk

                    if is_hg:
                        for h in range(8):
                            pb, pk = proj_fm(h * 128)
                            s, sk = rot("stb", stb)
                            b.act(s[:], pb[:], AF.Copy, r=[pk], w=[sk])
                            b.dma(sA[h][:, ts_], s[:], r=[sk], w=[("sA", h, tb)], eng="pool")
                            pb, pk = proj_fm(1024 + h * 128)
                            t, tk = rot("tmpf", tmpf)
                            b.act(t[:], pb[:], AF.Sigmoid, r=[pk], w=[tk])
                            b.ts(t[:], t[:], omlsb[:, j, h:h + 1], lbsb[:, j, h:h + 1], ALU.mult, ALU.add,
                                 r=[tk, "omlsb", "lbsb"], w=[tk])
                            g_, gk = rot("stf", stf)
                            b.act(g_[:], t[:], AF.Ln, r=[tk], w=[gk])
                            b.dma(sG[h][:, ts_], g_[:], r=[gk], w=[("sG", h, tb)], eng="pool")
                            s, sk = rot("stb", stb)
                            b.ts(s[:], t[:], -1.0, 1.0, ALU.mult, ALU.add, r=[tk], w=[sk])
                            b.dma(sK[h][:, ts_], s[:], r=[sk], w=[("sK", h, tb)], eng="pool")
                    else:
                        for hp in range(8):
                            for (c0, dst, gv, nm) in ((hp * 128, sA, qg, "sA"), (1024 + hp * 128, sK, kg, "sK")):
                                pb, pk = proj_fm(c0)
                                s2, s2k = rot("stb", stb)
                                b.act(s2[:], pb[:], AF.Square, r=[pk], w=[s2k])
                                b.mm(banks[5][:], blk64[:], s2[:], True, True, r=[s2k, "blk64"], w=["bank5"])
                                r2, r2k = rot("tmpf", tmpf)
                                b.act(r2[:], banks[5][:], AF.Sqrt, r=["bank5"], w=[r2k], scale=1.0 / 64, bias=EPS)
                                b.recip(r2[:], r2[:], r=[r2k], w=[r2k])
                                s, sk = rot("stb", stb)
                                b.stt(s[:], pb[:], gv[:, j:j + 1], r2[:], ALU.mult, ALU.mult,
                                      r=[pk, r2k, "qg", "kg"], w=[sk])
                                b.dma(dst[hp][:, ts_], s[:], r=[sk], w=[(nm, hp, tb)], eng="pool")
                        pb, pk = proj_fm(4096, M=16)
                        t, tk = rot("tmpf", tmpf)
                        b.act(t[0:16, :], pb[0:16, :], AF.Exp, r=[pk, "nbf"], w=[tk], scale=-1.0, bias=nbf[:, j:j + 1])
                        b.act(t[0:16, :], t[0:16, :], AF.Ln, r=[tk], w=[tk], scale=1.0, bias=1.0)
                        nc_, nk = ncum[tb % 2], "ncum%d" % (tb % 2)
                        pc_, pck = ncum[(tb + 1) % 2], "ncum%d" % ((tb + 1) % 2)
                        init = 0.0 if tb == 0 else pc_[:, 511:512]
                        b.scan(nc_[:], onesf[0:16, :], t[0:16, :], init, r=[tk, pck, "onesf"], w=[nk])
                        cc, ck = c6[tb % 2], "c6_%d" % (tb % 2)
                        r1, r1k = rot("tmpf", tmpf)
                        b.copy(cc[:, 3, :], nc_[:], r=[nk], w=[ck])
                        b.tt(r1[0:16, :], nc_[:], cc[:, 3, :], ALU.subtract, r=[nk, ck], w=[r1k])
                        b.copy(cc[:, 4, :], r1[0:16, :], r=[r1k], w=[ck])
                        b.tt(r1[0:16, :], r1[0:16, :], cc[:, 4, :], ALU.subtract, r=[r1k, ck], w=[r1k])
                        b.copy(cc[:, 5, :], r1[0:16, :], r=[r1k], w=[ck])
                        b.ts1(cc[:, 0:3, :], cc[:, 3:6, :], -1.0, ALU.mult, r=[ck], w=[ck])
                        b.dma(sC[:, :, ts_], cc[:], r=[ck], w=[("sC", tb)], eng="pool")

                    for tt_ in range(4):
                        for half in range(2):
                            pb, pk = pbank()
                            for kc in range(KC):
                                b.mm(pb[:], hb[:, kc, tt_ * 128:(tt_ + 1) * 128],
                                     win[:, kc, 2048 + half * 512:2048 + (half + 1) * 512], kc == 0, kc == KC - 1,
                                     r=["win", hk], w=[pk])
                            s, sk = rot("stb", stb)
                            b.act(s[:], pb[:], AF.Copy, r=[pk], w=[sk])
                            b.dma(sV[tb * 4 + tt_][:, half * 512:(half + 1) * 512], s[:], r=[sk],
                                  w=[("sV", tb * 4 + tt_, half)], eng="pool")
                    for h in range(8):
                        pb, pk = proj_fm(3072 + h * 128)
                        s, sk = rot("stb", stb)
                        b.act(s[:], pb[:], AF.Silu, r=[pk], w=[sk])
                        b.dma(sZ[h][:, ts_], s[:], r=[sk], w=[("sZ", h, tb)], eng="pool")
                P.flush()
            if stop_after == "A%d" % l:
                return nc

            with contextlib.ExitStack() as phOT:
                if l + 1 < n_layers:
                    w_make(l + 1)
                if is_hg:
                    with contextlib.ExitStack() as ph:
                        SEG = 256
                        NSEG = S // SEG
                        Ql = [sb(ph, "Ql%d" % i, [128, 8, SEG], BF16) for i in range(2)]
                        Kl = [sb(ph, "Kl%d" % i, [128, 8, SEG], BF16) for i in range(2)]
                        Gl = [sb(ph, "Gl%d" % i, [128, 8, SEG], F32) for i in range(2)]
                        Vl = [sb(ph, "Vl%d" % i, [128, 2, 1024], BF16) for i in range(2)]
                        Zl = [sb(ph, "Zl%d" % i, [128, 8, SEG], BF16) for i in range(2)]
                        rmask = sb(ph, "rmask", [128, 4 * SEG], F32)
                        bcum = sb(ph, "bcum", [128, 4 * SEG], F32)
                        ebt = sb(ph, "ebt", [128, 4 * SEG], F32)
                        tmpe = sb(ph, "tmpe", [128, 4 * SEG], F32)
                        qd = sb(ph, "qd", [128, 4 * SEG], BF16)
                        kinv = sb(ph, "kinv", [128, 4 * SEG], BF16)
                        kdT = sb(ph, "kdT", [128, 4 * SEG], BF16)
                        ATs = [sb(ph, "ATs%d" % i, [128, 128], BF16) for i in range(4)]
                        kdk = [sb(ph, "kdk%d" % i, [128, 128], BF16) for i in range(4)]
                        Sf = sb(ph, "Sf", [128, 8, 128], F32)
                        Sb = sb(ph, "Sb", [128, 8, 128], BF16)
                        of = [sb(ph, "of%d" % i, [128, SEG], F32) for i in range(2)]
                        osq = [sb(ph, "osq%d" % i, [128, SEG], BF16) for i in range(2)]
                        orst = [sb(ph, "orst%d" % i, [128, SEG], F32) for i in range(2)]
                        ost = [sb(ph, "ost%d" % i, [128, SEG], BF16) for i in range(4)]
                        b.memset(rmask[:], 1.0, w=["rmask"])
                        b.memset(rmask[:].rearrange("p (c t) -> p c t", t=32)[:, :, 0:1], 0.0, w=["rmask"])
                        b.memset(Sf[:], 0.0, w=["Sf"])
                        b.memset(Sb[:], 0.0, w=["Sb"])
                        rc = {"at": 0, "ats": 0, "kdk": 0, "u": 0, "tp": 0, "post": 0, "ost": 0}
                        for sg in range(NSEG):
                            sl = slice(sg * SEG, (sg + 1) * SEG)
                            q_, k_, g_, v_, z_ = Ql[sg % 2], Kl[sg % 2], Gl[sg % 2], Vl[sg % 2], Zl[sg % 2]
                            lk = "L%d" % (sg % 2)
                            tbk = sg * SEG // 512
                            b.dma(q_[:], sA.rearrange("h p t -> p h t")[:, :, sl],
                                  r=[("sA", h, tbk) for h in range(8)], w=[lk + "q"])
                            b.dma(k_[:], sK.rearrange("h p t -> p h t")[:, :, sl],
                                  r=[("sK", h, tbk) for h in range(8)], w=[lk + "k"])
                            b.dma(g_[:], sG.rearrange("h p t -> p h t")[:, :, sl],
                                  r=[("sG", h, tbk) for h in range(8)], w=[lk + "g"])
                            b.dma(v_[:], sV.rearrange("n p f -> p n f")[:, sg * 2:sg * 2 + 2, :],
                                  r=[("sV", sg * 2 + a, hh) for a in range(2) for hh in range(2)], w=[lk + "v"])
                            b.dma(z_[:], sZ.rearrange("h p t -> p h t")[:, :, sl],
                                  r=[("sZ", h, tbk) for h in range(8)], w=[lk + "z"])
                            for hg in range(2):
                                hs = slice(hg * 4, hg * 4 + 4)
                                gk = "hg"
                                qv = q_[:, hs, :]
                                kv = k_[:, hs, :]
                                gv = g_[:, hs, :]
                                v3 = lambda t_: t_[:].rearrange("p (h t) -> p h t", h=4)
                                for hh in range(4):
                                    b.scan(bcum[:, hh * SEG:(hh + 1) * SEG], rmask[:, hh * SEG:(hh + 1) * SEG],
                                           g_[:, hg * 4 + hh, :], 0.0, r=[lk + "g", "rmask"], w=["bcum"])
                                b.act(ebt[:], bcum[:], AF.Exp, r=["bcum"], w=["ebt"])
                                b.tt(v3(qd), qv, v3(ebt), ALU.mult, r=[lk + "q", "ebt"], w=["qd"], eng="pool")
                                b.act(tmpe[:], bcum[:], AF.Exp, r=["bcum"], w=["tmpe"], scale=-1.0)
                                b.tt(v3(kinv), kv, v3(tmpe), ALU.mult, r=[lk + "k", "tmpe"], w=["kinv"], eng="pool")
                                eb3 = ebt[:].rearrange("p (c t) -> p c t", t=32)
                                b.tt(kdT[:].rearrange("p (c t) -> p c t", t=32), kinv[:].rearrange("p (c t) -> p c t", t=32),
                                     eb3[:, :, 31:32].to_broadcast([128, 4 * SEG // 32, 32]), ALU.mult,
                                     r=["kinv", "ebt"], w=["kdT"], eng="pool")
                                for tl in range(SEG // 128):
                                    for hh in range(4):
                                        h = hg * 4 + hh
                                        cs = slice(hh * SEG + tl * 128, hh * SEG + (tl + 1) * 128)
                                        ai = rc["at"] % 2
                                        rc["at"] += 1
                                        atp = banks[4][:, ai * 128:(ai + 1) * 128]
                                        b.mm(atp, kinv[:, cs], qd[:, cs], True, True, r=["kinv", "qd"],
                                             w=["bank4"])
                                        asi = rc["ats"] % 4
                                        rc["ats"] += 1
                                        b.tt(ATs[asi][:], atp, mask01[:], ALU.mult, r=["bank4", "mask01"],
                                             w=[("ATs", asi)])
                                        ti = rc["tp"] % 8
                                        rc["tp"] += 1
                                        tpp = bankT[:, ti * 128:(ti + 1) * 128]
                                        b.tr(tpp, kdT[:, cs], ident[:], r=["kdT", "ident"], w=["bankT"])
                                        ki = rc["kdk"] % 4
                                        rc["kdk"] += 1
                                        b.act(kdk[ki][:], tpp, AF.Copy, r=["bankT"], w=[("kdk", ki)])
                                        ob = banks[hh]
                                        oc = slice(tl * 128, (tl + 1) * 128)
                                        b.mm(ob[:, oc], v_[:, tl, h * 128:(h + 1) * 128], ATs[asi][:], True, False,
                                             r=[lk + "v", ("ATs", asi)], w=["bank%d" % hh])
                                    for jj in range(4):
                                        for hh in range(4):
                                            h = hg * 4 + hh
                                            ob = banks[hh]
                                            cj = hh * SEG + tl * 128 + jj * 32
                                            ki = (rc["kdk"] - 4 + hh) % 4
                                            b.mm(ob[:, tl * 128 + jj * 32: tl * 128 + jj * 32 + 32],
                                                 Sb[:, h, :], qd[:, cj:cj + 32], False, jj == 3,
                                                 r=[("Sb", h), "qd"], w=["bank%d" % hh])
                                            ui = rc["u"] % 2
                                            rc["u"] += 1
                                            up = banks[5 + ui][:, 0:128]
                                            tkw = {"tile_position": (96, 0)} if jj == 3 else {}
                                            b.mm(up, kdk[ki][32 * jj:32 * jj + 32, :],
                                                 v_[32 * jj:32 * jj + 32, tl, h * 128:(h + 1) * 128], True, True,
                                                 r=[("kdk", ki), lk + "v"], w=["bank%d" % (5 + ui)], **tkw)
                                            chunk = (hh * SEG + tl * 128 + jj * 32) // 32
                                            b.stt(Sf[:, h, :], Sf[:, h, :], eb3[:, chunk, 31:32], up, ALU.mult, ALU.add,
                                                  r=[("Sf", h), "ebt", "bank%d" % (5 + ui)], w=[("Sf", h)])
                                            b.act(Sb[:, h, :], Sf[:, h, :], AF.Copy, r=[("Sf", h)], w=[("Sb", h)])
                                for hh in range(4):
                                    h = hg * 4 + hh
                                    pi = rc["post"] % 2
                                    rc["post"] += 1
                                    ob = banks[hh]
                                    b.act(osq[pi][:], ob[:, 0:SEG], AF.Square, r=["bank%d" % hh], w=[("osq", pi)])
                                    b.copy(of[pi][:], ob[:, 0:SEG], r=["bank%d" % hh], w=[("of", pi)])
                                    ssb = banks[4][:, 256:256 + SEG]
                                    b.mm(ssb, ones_bf[:], osq[pi][:], True, True, r=[("osq", pi), "ones_bf"],
                                         w=["bank4"])
                                    b.act(orst[pi][:], ssb, AF.Sqrt, r=["bank4"], w=[("orst", pi)],
                                          scale=1.0 / 128, bias=EPS)
                                    b.recip(orst[pi][:], orst[pi][:], r=[("orst", pi)], w=[("orst", pi)])
                                    b.stt(of[pi][:], of[pi][:], ogsb[:, j, h:h + 1], orst[pi][:], ALU.mult, ALU.mult,
                                          r=[("of", pi), ("orst", pi), "ogsb"], w=[("of", pi)])
                                    oi_ = rc["ost"] % 4
                                    rc["ost"] += 1
                                    b.tt(ost[oi_][:], of[pi][:], z_[:, h, :], ALU.mult, r=[("of", pi), lk + "z"],
                                         w=[("ost", oi_)], eng="pool")
                                    b.dma(sO[h][:, sl], ost[oi_][:], r=[("ost", oi_)], w=[("sO", h, sg)], eng="pool")
                                w_tick()
                        w_drain()
                        P.flush()
                else:
                    with contextlib.ExitStack() as ph:
                        qa = [sb(ph, "qa%d" % i, [70, S], BF16) for i in range(2)]
                        ka = [sb(ph, "ka%d" % i, [70, S], BF16) for i in range(2)]
                        va = [sb(ph, "va%d" % i, [128, NT, 128], BF16) for i in range(2)]
                        zt = [sb(ph, "zt%d" % i, [64, S], BF16) for i in range(2)]
                        PT = [sb(ph, "PT%d" % i, [128, 512], BF16) for i in range(4)]
                        rl = [sb(ph, "rl%d" % i, [64, 512], F32) for i in range(2)]
                        t1 = [sb(ph, "t1%d" % i, [64, 512], F32) for i in range(2)]
                        ost = [sb(ph, "ost%d" % i, [64, 512], BF16) for i in range(2)]
                        for i in range(2):
                            b.memset(qa[i][64:70, :], 1.0, w=[("qa1", i), ("qac", i)])
                            b.memset(ka[i][64:70, :], 1.0, w=[("ka1", i), ("kac", i)])
                            b.memset(va[i][:, :, 64:128], 1.0, w=[("va1", i)])
                        rc = {"st": 0, "pt": 0, "oa": 0, "fin": 0}

                        def load_head(h):
                            hp, ho = h // 2, (h % 2) * 64
                            i = h % 2
                            allA = [("sA", hp, tb) for tb in range(NB)]
                            allK = [("sK", hp, tb) for tb in range(NB)]
                            allC = [("sC", tb) for tb in range(NB)]
                            b.dma(qa[i][0:64, :], sA[hp][ho:ho + 64, :], r=allA, w=[("qaq", i)])
                            b.dma(qa[i][64:67, :], sC[h, 0:3, :], r=allC, w=[("qac", i)])
                            b.dma(ka[i][0:64, :], sK[hp][ho:ho + 64, :], r=allK, w=[("kaq", i)])
                            b.dma(ka[i][67:70, :], sC[h, 3:6, :], r=allC, w=[("kac", i)])
                            for n0 in range(0, NT, 8):
                                n1 = min(NT, n0 + 8)
                                b.dma(va[i][:, n0:n1, 0:64],
                                      sV.rearrange("n p f -> p n f")[:, n0:n1, h * 64:(h + 1) * 64],
                                      r=[("sV", n, hh) for n in range(n0, n1) for hh in range(2)], w=[("vav", i, n0)])
                            b.dma(zt[i][:, :], sZ[hp][ho:ho + 64, :], r=[("sZ", hp, tb) for tb in range(NB)],
                                  w=["zt%d" % i])

                        steps = [(h, qs, kb) for h in range(16) for qs in range(NB) for kb in range(4 * qs + 4)]
                        info = {}
                        span_oa = {}
                        LOOK = 2

                        def emit_qk(idx):
                            h, qs, kb = steps[idx]
                            i = h % 2
                            jd = kb - 4 * qs
                            c0 = 128 * jd if jd > 0 else 0
                            si = rc["st"] % 3
                            rc["st"] += 1
                            ST = banks[si]
                            b.mm(ST[:, c0:512], ka[i][0:70, kb * 128:(kb + 1) * 128],
                                 qa[i][0:70, qs * 512 + c0:(qs + 1) * 512], True, jd < 0,
                                 r=[("kaq", i), ("kac", i), ("ka1", i), ("qaq", i), ("qac", i), ("qa1", i)],
                                 w=["bank%d" % si])
                            if jd >= 0:
                                b.mm(ST[:, c0:c0 + 128], ident[:], maskbias[:], False, True,
                                     r=["ident", "maskbias"], w=["bank%d" % si])
                            pi = rc["pt"] % 4
                            rc["pt"] += 1
                            b.act(PT[pi][:, c0:512], ST[:, c0:512], AF.Exp, r=["bank%d" % si], w=[("PT", pi)])
                            info[idx] = (c0, pi)

                        def emit_pv(idx):
                            h, qs, kb = steps[idx]
                            hp, ho = h // 2, (h % 2) * 64
                            i = h % 2
                            c0, pi = info.pop(idx)
                            nkb = 4 * qs + 4
                            if kb == 0:
                                span_oa[(h, qs)] = rc["oa"] % 2
                                rc["oa"] += 1
                            oi = span_oa[(h, qs)]
                            OA = banks[3 + oi]
                            b.mm(OA[:, c0:512], va[i][:, kb, :], PT[pi][:, c0:512], kb == 0, kb == nkb - 1,
                                 r=[("vav", i, (kb // 8) * 8), ("va1", i), ("PT", pi)], w=["bank%d" % (3 + oi)])
                            if kb == nkb - 1:
                                fi = rc["fin"] % 2
                                rc["fin"] += 1
                                b.recip(rl[fi][:], OA[64:128, :], r=["bank%d" % (3 + oi)], w=[("rl", fi)])
                                b.tt(t1[fi][:], OA[0:64, :], rl[fi][:], ALU.mult, r=["bank%d" % (3 + oi), ("rl", fi)],
                                     w=[("t1", fi)])
                                b.tt(ost[fi][:], t1[fi][:],
                                     zt[i][:, qs * 512:(qs + 1) * 512], ALU.mult, r=[("t1", fi), "zt%d" % i],
                                     w=[("ost", fi)], eng="pool")
                                b.dma(sO[hp][ho:ho + 64, qs * 512:(qs + 1) * 512], ost[fi][:], r=[("ost", fi)],
                                      w=[("sO", h, qs)], eng="pool")
                                if qs == NB - 1:
                                    w_tick()
                                    if h + 2 < 16:
                                        load_head(h + 2)

                        load_head(0)
                        load_head(1)
                        for idx in range(len(steps) + LOOK):
                            if idx < len(steps):
                                emit_qk(idx)
                            if idx - LOOK >= 0:
                                emit_pv(idx - LOOK)
                        w_drain()
                        P.flush()

                if stop_after == "B%d" % l:
                    return nc
                with contextlib.ExitStack() as ph:
                    xc = [sb(ph, "xc%d" % i, [128, KC, 512], F32) for i in range(2)]
                    otb = [sb(ph, "otb%d" % i, [128, KC, 512], BF16) for i in range(2)]
                    sO_v = sO.rearrange("h p t -> p h t")
                    nb_ = 0
                    for tb in range(NB):
                        ts_ = slice(tb * 512, (tb + 1) * 512)
                        xb, xk = xc[tb % 2], "xc%d" % (tb % 2)
                        ob_, ok_ = otb[tb % 2], "otb%d" % (tb % 2)
                        if tb == 0:
                            b.dma(ob_[:], sO_v[:, :, ts_], r=[], w=[ok_])
                            b.dma(xb[:], xsrc_v[:, :, ts_], r=[("x", tb)], w=[xk])
                        if tb + 1 < NB:
                            b.dma(otb[(tb + 1) % 2][:], sO_v[:, :, (tb + 1) * 512:(tb + 2) * 512], r=[],
                                  w=["otb%d" % ((tb + 1) % 2)])
                            b.dma(xc[(tb + 1) % 2][:], xsrc_v[:, :, (tb + 1) * 512:(tb + 2) * 512],
                                  r=[("x", tb + 1)], w=["xc%d" % ((tb + 1) % 2)])
                        for dc in range(KC):
                            bi = nb_ % 4
                            nb_ += 1
                            pb, pk = banks[bi], "bank%d" % bi
                            for fc in range(KC):
                                b.mm(pb[:], wout[:, fc, dc * 128:(dc + 1) * 128], ob_[:, fc, :], fc == 0, fc == KC - 1,
                                     r=["wout", ok_], w=[pk])
                            b.stt(xb[:, dc, :], pb[:], modsb[:, l, 16 + dc:17 + dc], xb[:, dc, :], ALU.mult, ALU.add,
                                  r=[pk, "modsb", xk], w=[xk])
                        b.dma(y_v[:, :, ts_], xb[:], r=[xk], w=[("x", tb)], eng="pool")
                    P.flush()
    return nc


def _consts():
    s = np.arange(128)[:, None]
    t = np.arange(128)[None, :]
    maskbias = np.where(s > t, NEG, 0.0).astype(np.float32)
    mask01 = ((s // 32 == t // 32) & (s <= t)).astype(np.float32)
    mj = (s // 32 == np.arange(4)[None, :]).astype(np.float32)
    blk64 = (s // 64 == t // 64).astype(np.float32)
    return dict(c_maskbias=maskbias, c_mask01=mask01, c_mj=mj, c_blk64=blk64)


def _pcol(v, n):
    return np.ascontiguousarray(np.asarray(v, np.float32).reshape(n, 128).T)


def make_in_maps(inputs, n_cores, S):
    f = lambda a: np.ascontiguousarray(np.asarray(a, dtype=np.float32))
    x = f(inputs["x"])
    c = f(inputs["c"])
    shared = dict(
        gT=np.ascontiguousarray(np.stack([_pcol(g, 8) for g in f(inputs["norm_g"])], 1)),
        ada_w=f(inputs["ada_w"]),
        ada_bT=np.ascontiguousarray(np.stack([_pcol(v, 24) for v in f(inputs["ada_b"])], 1)),
        lbT=np.ascontiguousarray(np.stack([_pcol(v, 8) for v in f(inputs["hg_lb_logits"])], 1)),
        hg_w_in=f(inputs["hg_w_in"]),
        ogT=np.ascontiguousarray(np.stack([_pcol(v, 8) for v in f(inputs["hg_o_g"])], 1)),
        hg_w_out=f(inputs["hg_w_out"]),
        fx_w_in=f(inputs["fx_w_in"]),
        bfT=np.ascontiguousarray(f(inputs["fx_b_f"]).T),
        qgT=np.ascontiguousarray(np.tile(f(inputs["fx_q_g"]).T, (2, 1))),
        kgT=np.ascontiguousarray(np.tile(f(inputs["fx_k_g"]).T, (2, 1))),
        fx_w_out=f(inputs["fx_w_out"]),
    )
    shared.update(_consts())
    maps = []
    for i in range(n_cores):
        m = dict(shared)
        m["xT"] = np.ascontiguousarray(x[i].T)
        m["cT"] = _pcol(c[i], 8)
        maps.append(m)
    return maps


_NC_CACHE = {}


def run(inputs, n_layers=4, stop_after=None):
    x = np.asarray(inputs["x"])
    Bn, S, _ = x.shape
    key = (S, n_layers, stop_after)
    if key not in _NC_CACHE:
        _NC_CACHE[key] = build(S, n_layers, stop_after)
    nc = _NC_CACHE[key]
    maps = make_in_maps(inputs, Bn, S)
    res = run_bass_kernel_spmd(nc, maps, core_ids=list(range(Bn)))
    out = np.stack([np.ascontiguousarray(res.results[i]["yT"].T) for i in range(Bn)], 0)
    return out.astype(np.float32)


def kernel(**inputs):
    return run(inputs, n_layers=4)
```

```python
import contextlib
import numpy as np
import concourse.bass as bass
import concourse.mybir as mybir
from concourse.bass_utils import run_bass_kernel_spmd

F32 = mybir.dt.float32
BF16 = mybir.dt.bfloat16
ALU = mybir.AluOpType
AF = mybir.ActivationFunctionType

ENGS = ("pe", "act", "dve", "pool", "sp")
D = 1024
KC = 8
EPS = 1e-6
SAME_ENG_SYNC = False
NEG = -30000.0


class Prog:
    def __init__(self, nc, es, n_dma_sems=40):
        self.nc = nc
        self.ops = []
        self.buf = {}
        self.n_dma_sems = n_dma_sems
        self.dma_rr = 0
        self.dma_rr_e = {}
        self.slot_ord = [0] * n_dma_sems
        self.slot_last = {}
        self.flushed = 0
        self.cnt = {e: 0 for e in ENGS}
        self.esem = {e: es.enter_context(nc.semaphore("s_" + e)) for e in ENGS}
        self.dsem = [es.enter_context(nc.semaphore("d_%d" % k)) for k in range(n_dma_sems)]
        self.nflush = 0

    def op(self, eng, fn, r=(), w=(), dma=False):
        pk_ = [k for k in r if isinstance(k, str) and k.startswith("bank")]
        if pk_:
            w = list(w) + [k for k in pk_ if k not in w]
            r = [k for k in r if k not in pk_]
        deps = set()
        for b in r:
            st = self.buf.get(b)
            if st is not None and st[0] is not None:
                deps.add(st[0])
        for b in w:
            st = self.buf.get(b)
            if st is not None:
                if st[0] is not None:
                    deps.add(st[0])
                deps.update(st[1])
        i = len(self.ops)
        o = dict(eng=eng, fn=fn, deps=deps, dma=dma, slot=None, ordinal=None, count=None)
        if dma:
            lo_, hi_ = (0, 28) if eng == "sp" else (28, self.n_dma_sems)
            k_ = self.dma_rr_e.get(eng, 0)
            self.dma_rr_e[eng] = k_ + 1
            s = lo_ + k_ % (hi_ - lo_)
            prev = self.slot_last.get(s)
            if prev is not None:
                deps.add(prev)
            self.slot_ord[s] += 1
            o["ordinal"] = self.slot_ord[s]
            o["slot"] = s
            self.slot_last[s] = i
        self.ops.append(o)
        for b in r:
            st = self.buf.setdefault(b, [None, []])
            st[1].append(i)
        for b in w:
            self.buf[b] = [i, []]
        return i

    def flush(self):
        nc = self.nc
        ops = self.ops
        lo, hi = self.flushed, len(ops)
        if lo == hi:
            return
        base_cnt = dict(self.cnt)
        base_ord = list(self.slot_ord_flushed) if hasattr(self, "slot_ord_flushed") else [0] * self.n_dma_sems
        waited = {e: {f: -1 for f in ENGS} for e in ENGS}
        waited_dma = {e: {} for e in ENGS}
        needed = {}
        for i in range(lo, hi):
            o = ops[i]
            e = o["eng"]
            wl = []
            for d in sorted(o["deps"]):
                if d < lo:
                    continue
                od = ops[d]
                if od["dma"]:
                    if waited_dma[e].get(od["slot"], 0) >= od["ordinal"]:
                        continue
                    waited_dma[e][od["slot"]] = od["ordinal"]
                    wl.append(("dma", od["slot"], od["ordinal"]))
                else:
                    f = od["eng"]
                    if f == e and (e == "pe" or (not SAME_ENG_SYNC and e != "pool")):
                        continue
                    if waited[e][f] >= d:
                        continue
                    waited[e][f] = d
                    needed[d] = True
                    wl.append(("eng", f, d))
            o["waits"] = wl
        last_of = {}
        for i in range(lo, hi):
            if not ops[i]["dma"]:
                last_of[ops[i]["eng"]] = i
        for e, i in last_of.items():
            needed[i] = True
        for i in range(lo, hi):
            o = ops[i]
            if o["dma"]:
                continue
            if needed.get(i):
                self.cnt[o["eng"]] += 1
                o["count"] = self.cnt[o["eng"]]
        per_eng = {e: [i for i in range(lo, hi) if ops[i]["eng"] == e] for e in ENGS}
        esem, dsem = self.esem, self.dsem
        first = self.nflush == 0
        self.nflush += 1
        final_waits = {e: [] for e in ENGS}
        cur_last = {}
        for i in range(lo, hi):
            if ops[i]["dma"]:
                cur_last[ops[i]["slot"]] = i
        for s, i in cur_last.items():
            final_waits[ops[i]["eng"]].append((s, ops[i]["ordinal"]))

        def replay(ename, eng):
            if not first:
                for f in ENGS:
                    if f != ename and base_cnt[f] > 0:
                        eng.wait_ge(esem[f], base_cnt[f])
            for i in per_eng[ename]:
                o = ops[i]
                for wt in o["waits"]:
                    if wt[0] == "dma":
                        eng.wait_ge(dsem[wt[1]], 16 * wt[2])
                    else:
                        eng.wait_ge(esem[wt[1]], ops[wt[2]]["count"])
                ins = o["fn"](eng)
                if o["dma"]:
                    ins.then_inc(dsem[o["slot"]], 16)
                elif needed.get(i):
                    ins.then_inc(esem[ename], 1)
                o["fn"] = None
            for s, ordn in final_waits[ename]:
                eng.wait_ge(dsem[s], 16 * ordn)

        with nc.Block() as block:
            def wrap(ename):
                def f(eng):
                    replay(ename, eng)
                    if final_waits[ename]:
                        self.cnt[ename] += 1
                        eng.nop().then_inc(esem[ename], 1)
                return f
            block.tensor(wrap("pe"))
            block.scalar(wrap("act"))
            block.vector(wrap("dve"))
            block.gpsimd(wrap("pool"))
            block.sync(wrap("sp"))
        self.flushed = hi
        self.slot_ord_flushed = list(self.slot_ord)


class B:
    def __init__(self, P):
        self.P = P

    def dma(self, out, in_, r, w, eng="sp"):
        return self.P.op(eng, lambda e: e.dma_start(out=out, in_=in_), r=r, w=w, dma=True)

    def mm(self, out, lhsT, rhs, start, stop, r, w, **kw):
        return self.P.op("pe", lambda e: e.matmul(out, lhsT=lhsT, rhs=rhs, start=start, stop=stop,
                                                  skip_group_check=True, **kw), r=r, w=w)

    def tr(self, out, in_, ident, r, w):
        return self.P.op("pe", lambda e: e.transpose(out=out, in_=in_, identity=ident), r=r, w=w)

    def act(self, out, in_, func, r, w, scale=1.0, bias=0.0):
        return self.P.op("act", lambda e: e.activation(out=out, in_=in_, func=func, scale=scale, bias=bias), r=r, w=w)

    def tt(self, out, in0, in1, op, r, w, eng="dve"):
        return self.P.op(eng, lambda e: e.tensor_tensor(out=out, in0=in0, in1=in1, op=op), r=r, w=w)

    def ts(self, out, in0, s1, s2, op0, op1, r, w, eng="dve"):
        return self.P.op(eng, lambda e: e.tensor_scalar(out=out, in0=in0, scalar1=s1, scalar2=s2, op0=op0, op1=op1),
                         r=r, w=w)

    def ts1(self, out, in0, s1, op0, r, w, eng="dve"):
        return self.P.op(eng, lambda e: e.tensor_single_scalar(out=out, in_=in0, scalar=s1, op=op0), r=r, w=w)

    def stt(self, out, in0, scalar, in1, op0, op1, r, w, eng="dve"):
        return self.P.op(eng, lambda e: e.scalar_tensor_tensor(out=out, in0=in0, scalar=scalar, in1=in1,
                                                               op0=op0, op1=op1), r=r, w=w)

    def copy(self, out, in_, r, w, eng="dve"):
        return self.P.op(eng, lambda e: e.tensor_copy(out=out, in_=in_), r=r, w=w)

    def recip(self, out, in_, r, w):
        return self.P.op("dve", lambda e: e.reciprocal(out=out, in_=in_), r=r, w=w)

    def memset(self, ap, val, w, eng="pool"):
        return self.P.op(eng, lambda e: e.memset(ap, val), r=(), w=w)

    def scan(self, out, d0, d1, initial, r, w):
        return self.P.op("dve", lambda e: e.tensor_tensor_scan(out=out, data0=d0, data1=d1, initial=initial,
                                                               op0=ALU.mult, op1=ALU.add), r=r, w=w)


def build(S, n_layers, stop_after=None):
    NB = S // 512
    NT = S // 128
    nc = bass.Bass("TRN2", target_bir_lowering=False)

    def din(name, shape, dt=F32):
        return nc.dram_tensor(name, list(shape), dt, kind="ExternalInput").ap()

    def dscr(name, shape, dt):
        return nc.dram_tensor(name, list(shape), dt, kind="Internal").ap()

    xT = din("xT", [D, S])
    cT = din("cT", [128, KC])
    gT = din("gT", [128, 4, KC])
    ada_w = din("ada_w", [4, D, 3 * D])
    ada_bT = din("ada_bT", [128, 4, 24])
    lbT = din("lbT", [128, 2, 8])
    hg_w_in = din("hg_w_in", [2, D, 4096])
    ogT = din("ogT", [128, 2, 8])
    hg_w_out = din("hg_w_out", [2, D, D])
    fx_w_in = din("fx_w_in", [2, D, 4112])
    bfT = din("bfT", [16, 2])
    qgT = din("qgT", [128, 2])
    kgT = din("kgT", [128, 2])
    fx_w_out = din("fx_w_out", [2, D, D])
    c_maskbias = din("c_maskbias", [128, 128])
    c_mask01 = din("c_mask01", [128, 128])
    c_mj = din("c_mj", [128, 4])
    c_blk64 = din("c_blk64", [128, 128])
    yT = nc.dram_tensor("yT", [D, S], F32, kind="ExternalOutput").ap()

    sA = dscr("sA", [8, 128, S], BF16)
    sK = dscr("sK", [8, 128, S], BF16)
    sG = dscr("sG", [8, 128, S], F32)
    sV = dscr("sV", [NT, 128, 1024], BF16)
    sZ = dscr("sZ", [8, 128, S], BF16)
    sC = dscr("sC", [16, 6, S], BF16)

    with contextlib.ExitStack() as es:
        P = Prog(nc, es)
        b = B(P)

        uid = [0]

        def sb(stack, name, shape, dt):
            uid[0] += 1
            return stack.enter_context(nc.sbuf_tensor("%s_u%d" % (name, uid[0]), list(shape), dt))

        banks = [es.enter_context(nc.psum_tensor("bank%d" % i, [128, 512], F32)) for i in range(7)]
        bankT = es.enter_context(nc.psum_tensor("bankT", [128, 1024], BF16))
        ident = sb(es, "ident", [128, 128], BF16)
        ones_bf = sb(es, "ones_bf", [128, 128], BF16)
        blk64 = sb(es, "blk64", [128, 128], BF16)
        maskbias = sb(es, "maskbias", [128, 128], BF16)
        mask01 = sb(es, "mask01", [128, 128], F32)
        mj = sb(es, "mj", [128, 4], F32)
        ctmp = sb(es, "ctmp", [128, 128], F32)
        cact = sb(es, "cact", [128, KC], F32)
        gsb = sb(es, "gsb", [128, 4, KC], F32)
        modsb = sb(es, "modsb", [128, 4, 24], F32)
        asb = sb(es, "asb", [128, 4, KC], F32)
        lbsb = sb(es, "lbsb", [128, 2, 8], F32)
        omlsb = sb(es, "omlsb", [128, 2, 8], F32)
        ogsb = sb(es, "ogsb", [128, 2, 8], F32)
        nbf = sb(es, "nbf", [16, 2], F32)
        qg = sb(es, "qg", [128, 2], F32)
        kg = sb(es, "kg", [128, 2], F32)
        wout = sb(es, "wout", [128, KC, D], BF16)
        onesf = sb(es, "onesf", [128, 512], F32)
        win = sb(es, "win", [128, KC, 4112], BF16)
        wst = [sb(es, "wst%d" % i, [128, 2048], F32) for i in range(2)]
        sO = dscr("sO", [8, 128, S], BF16)
        wq = {"steps": [], "k": 0}

        def w_make(l_):
            is_hg_ = (l_ % 2 == 0)
            w_ = (hg_w_in[l_ // 2] if is_hg_ else fx_w_in[l_ // 2]).rearrange("(kc p) f -> p kc f", p=128)
            ncol = 4096 if is_hg_ else 4112
            for kc in range(KC):
                for c0 in range(0, ncol, 2048):
                    wq["steps"].append((w_, kc, c0, min(ncol, c0 + 2048)))

        def w_tick(n=1):
            for _ in range(n):
                pend = wq.get("pend")
                if pend is not None:
                    kc, c0, c1, st, key = pend
                    b.copy(win[:, kc, c0:c1], st[:, 0:c1 - c0], r=[key], w=["win"], eng="pool")
                    wq["pend"] = None
                if not wq["steps"]:
                    continue
                w_, kc, c0, c1 = wq["steps"].pop(0)
                k_ = wq["k"]
                wq["k"] += 1
                st, key = wst[k_ % 2], "wst%d" % (k_ % 2)
                b.dma(st[:, 0:c1 - c0], w_[:, kc, c0:c1], r=[], w=[key])
                wq["pend"] = (kc, c0, c1, st, key)

        def w_drain():
            while wq["steps"] or wq.get("pend") is not None:
                w_tick()

        with contextlib.ExitStack() as ph:
            adast = [sb(ph, "adast%d" % i, [128, KC, 512], F32) for i in range(2)]
            b.memset(ident[:], 0.0, w=["ident"])
            P.op("pool", lambda e: e.affine_select(out=ident[:], in_=ident[:], pattern=[[-1, 128]],
                                                   compare_op=ALU.not_equal, fill=1.0, base=0,
                                                   channel_multiplier=1), r=["ident"], w=["ident"])
            b.memset(ones_bf[:], 1.0, w=["ones_bf"])
            b.memset(onesf[:], 1.0, w=["onesf"])
            for (src, dst, nm) in ((c_maskbias, maskbias, "maskbias"), (c_blk64, blk64, "blk64")):
                b.dma(ctmp[:], src, r=[], w=["ctmp"])
                b.copy(dst[:], ctmp[:], r=["ctmp"], w=[nm])
            b.dma(mask01[:], c_mask01, r=[], w=["mask01"])
            b.dma(mj[:], c_mj, r=[], w=["mj"])
            b.dma(cact[:], cT, r=[], w=["cact"])
            b.dma(gsb[:], gT, r=[], w=["gsb"])
            b.dma(modsb[:], ada_bT, r=[], w=["modsb"])
            b.dma(lbsb[:], lbT, r=[], w=["lbsb"])
            b.dma(ogsb[:], ogT, r=[], w=["ogsb"])
            b.dma(nbf[:], bfT, r=[], w=["nbf"])
            b.dma(qg[:], qgT, r=[], w=["qg"])
            b.dma(kg[:], kgT, r=[], w=["kg"])
            b.act(cact[:], cact[:], AF.Silu, r=["cact"], w=["cact"])
            b.tt(lbsb[:, 1, :], lbsb[:, 1, :], lbsb[:, 0, :], ALU.subtract, r=["lbsb"], w=["lbsb"])
            b.act(lbsb[:, 1, :], lbsb[:, 1, :], AF.Sigmoid, r=["lbsb"], w=["lbsb"])
            b.memset(lbsb[:, 0, :], 0.0, w=["lbsb"], eng="dve")
            b.ts(omlsb[:], lbsb[:], -1.0, 1.0, ALU.mult, ALU.add, r=["lbsb"], w=["omlsb"])
            b.ts1(nbf[:], nbf[:], -1.0, ALU.mult, r=["nbf"], w=["nbf"])
            b.ts1(qg[:], qg[:], 0.125, ALU.mult, r=["qg"], w=["qg"])
            modps = banks[0]
            k = 0
            for l in range(n_layers):
                for jb in range(6):
                    st = adast[k % 2]
                    key = "adast%d" % (k % 2)
                    k += 1
                    b.dma(st[:], ada_w[l].rearrange("(kc p) j -> p kc j", p=128)[:, :, jb * 512:(jb + 1) * 512],
                          r=[], w=[key])
                    for jj in range(4):
                        col = l * 24 + jb * 4 + jj
                        for kc in range(KC):
                            b.mm(modps[:, col:col + 1], st[:, kc, jj * 128:(jj + 1) * 128], cact[:, kc:kc + 1],
                                 kc == 0, kc == KC - 1, r=[key, "cact"], w=["bank0"])
            nl = n_layers
            b.tt(modsb[:, 0:nl, :], modsb[:, 0:nl, :], modps[:, 0:nl * 24].rearrange("p (l j) -> p l j", j=24),
                 ALU.add, r=["bank0", "modsb"], w=["modsb"])
            b.stt(asb[:, 0:nl, :], modsb[:, 0:nl, 8:16], 1.0, gsb[:, 0:nl, :], ALU.add, ALU.mult,
                  r=["modsb", "gsb"], w=["asb"])
            P.flush()
        if stop_after == "p0":
            return nc

        for l in range(n_layers):
            is_hg = (l % 2 == 0)
            j = l // 2
            xsrc = xT if l == 0 else yT
            w_in = hg_w_in[j] if is_hg else fx_w_in[j]
            w_out = hg_w_out[j] if is_hg else fx_w_out[j]
            NCOL = 4096 if is_hg else 4112
            xsrc_v = xsrc.rearrange("(kc p) t -> p kc t", p=128)
            y_v = yT.rearrange("(kc p) t -> p kc t", p=128)

            with contextlib.ExitStack() as ph:
                xblk = [sb(ph, "xblk%d" % i, [128, KC, 512], F32) for i in range(1)]
                hT = [sb(ph, "hT%d" % i, [128, KC, 512], BF16) for i in range(2)]
                sq = [sb(ph, "sq%d" % i, [128, 512], BF16) for i in range(2)]
                rstd = sb(ph, "rstd", [128, 512], F32)
                tmpf = [sb(ph, "tmpf%d" % i, [128, 512], F32) for i in range(4)]
                stb = [sb(ph, "stb%d" % i, [128, 512], BF16) for i in range(6)]
                stf = [sb(ph, "stf%d" % i, [128, 512], F32) for i in range(3)]
                ncum = [sb(ph, "ncum%d" % i, [16, 512], F32) for i in range(2)]
                c6 = [sb(ph, "c6_%d" % i, [16, 6, 512], BF16) for i in range(2)]
                cnt = {"tmpf": 0, "stb": 0, "stf": 0, "bank": 0, "wst": 0}

                def rot(name, lst):
                    i = cnt[name] % len(lst)
                    cnt[name] += 1
                    return lst[i], "%s%d" % (name, i)

                def pbank():
                    i = 1 + cnt["bank"] % 4
                    cnt["bank"] += 1
                    return banks[i], "bank%d" % i

                if l == 0:
                    w_make(0)
                w_drain()
                w_out_v = w_out.rearrange("(kc p) f -> p kc f", p=128)
                for kc in range(0, KC, 2):
                    k_ = wq["k"]
                    wq["k"] += 1
                    st, key = wst[k_ % 2], "wst%d" % (k_ % 2)
                    b.dma(st[:].rearrange("p (a f) -> p a f", a=2), w_out_v[:, kc:kc + 2, :], r=[], w=[key])
                    b.copy(wout[:, kc:kc + 2, :], st[:].rearrange("p (a f) -> p a f", a=2), r=[key], w=["wout"],
                           eng="pool")

                for tb in range(NB):
                    ts_ = slice(tb * 512, (tb + 1) * 512)
                    xb, xk = xblk[0], "xblk0"
                    hb, hk = hT[tb % 2], "hT%d" % (tb % 2)
                    if tb == 0:
                        b.dma(xb[:], xsrc_v[:, :, ts_], r=[("x", tb)], w=[xk])
                    ssp = banks[0]
                    for kc in range(KC):
                        b.act(sq[kc % 2][:], xb[:, kc, :], AF.Square, r=[xk], w=["sq%d" % (kc % 2)])
                        b.mm(ssp[:], ones_bf[:], sq[kc % 2][:], kc == 0, kc == KC - 1,
                             r=["sq%d" % (kc % 2), "ones_bf"], w=["bank0"])
                    b.act(rstd[:], ssp[:], AF.Sqrt, r=["bank0"], w=["rstd"], scale=1.0 / D, bias=EPS)
                    b.recip(rstd[:], rstd[:], r=["rstd"], w=["rstd"])
                    for kc in range(KC):
                        t, tk = rot("tmpf", tmpf)
                        b.stt(t[:], xb[:, kc, :], asb[:, l, kc:kc + 1], rstd[:], ALU.mult, ALU.mult,
                              r=[xk, "asb", "rstd"], w=[tk])
                        b.act(hb[:, kc, :], t[:], AF.Identity, r=[tk, "modsb"], w=[hk],
                              bias=modsb[:, l, kc:kc + 1])

                    if tb + 1 < NB:
                        b.dma(xb[:], xsrc_v[:, :, (tb + 1) * 512:(tb + 2) * 512], r=[("x", tb + 1)], w=[xk])

                    def proj_fm(c0, M=128):
                        pb, pk = pbank()
                        for kc in range(KC):
                            b.mm(pb[0:M, :], win[:, kc, c0:c0 + M], hb[:, kc, :], kc == 0, kc == KC - 1,
                                 r=["win", hk], w=[pk])
                        return pb, pk

                    if is_hg:
                        for h in range(8):
                            pb, pk = proj_fm(h * 128)
                            s, sk = rot("stb", stb)
                            b.act(s[:], pb[:], AF.Copy, r=[pk], w=[sk])
                            b.dma(sA[h][:, ts_], s[:], r=[sk], w=[("sA", h, tb)], eng="pool")
                            pb, pk = proj_fm(1024 + h * 128)
                            t, tk = rot("tmpf", tmpf)
                            b.act(t[:], pb[:], AF.Sigmoid, r=[pk], w=[tk])
                            b.ts(t[:], t[:], omlsb[:, j, h:h + 1], lbsb[:, j, h:h + 1], ALU.mult, ALU.add,
                                 r=[tk, "omlsb", "lbsb"], w=[tk])
                            g_, gk = rot("stf", stf)
                            b.act(g_[:], t[:], AF.Ln, r=[tk], w=[gk])
                            b.dma(sG[h][:, ts_], g_[:], r=[gk], w=[("sG", h, tb)], eng="pool")
                            s, sk = rot("stb", stb)
                            b.ts(s[:], t[:], -1.0, 1.0, ALU.mult, ALU.add, r=[tk], w=[sk])
                            b.dma(sK[h][:, ts_], s[:], r=[sk], w=[("sK", h, tb)], eng="pool")
                    else:
                        for hp in range(8):
                            for (c0, dst, gv, nm) in ((hp * 128, sA, qg, "sA"), (1024 + hp * 128, sK, kg, "sK")):
                                pb, pk = proj_fm(c0)
                                s2, s2k = rot("stb", stb)
                                b.act(s2[:], pb[:], AF.Square, r=[pk], w=[s2k])
                                b.mm(banks[5][:], blk64[:], s2[:], True, True, r=[s2k, "blk64"], w=["bank5"])
                                r2, r2k = rot("tmpf", tmpf)
                                b.act(r2[:], banks[5][:], AF.Sqrt, r=["bank5"], w=[r2k], scale=1.0 / 64, bias=EPS)
                                b.recip(r2[:], r2[:], r=[r2k], w=[r2k])
                                s, sk = rot("stb", stb)
                                b.stt(s[:], pb[:], gv[:, j:j + 1], r2[:], ALU.mult, ALU.mult,
                                      r=[pk, r2k, "qg", "kg"], w=[sk])
                                b.dma(dst[hp][:, ts_], s[:], r=[sk], w=[(nm, hp, tb)], eng="pool")
                        pb, pk = proj_fm(4096, M=16)
                        t, tk = rot("tmpf", tmpf)
                        b.act(t[0:16, :], pb[0:16, :], AF.Exp, r=[pk, "nbf"], w=[tk], scale=-1.0, bias=nbf[:, j:j + 1])
                        b.act(t[0:16, :], t[0:16, :], AF.Ln, r=[tk], w=[tk], scale=1.0, bias=1.0)
                        nc_, nk = ncum[tb % 2], "ncum%d" % (tb % 2)
                        pc_, pck = ncum[(tb + 1) % 2], "ncum%d" % ((tb + 1) % 2)
                        init = 0.0 if tb == 0 else pc_[:, 511:512]
                        b.scan(nc_[:], onesf[0:16, :], t[0:16, :], init, r=[tk, pck, "onesf"], w=[nk])
                        cc, ck = c6[tb % 2], "c6_%d" % (tb % 2)
                        r1, r1k = rot("tmpf", tmpf)
                        b.copy(cc[:, 3, :], nc_[:], r=[nk], w=[ck])
                        b.tt(r1[0:16, :], nc_[:], cc[:, 3, :], ALU.subtract, r=[nk, ck], w=[r1k])
                        b.copy(cc[:, 4, :], r1[0:16, :], r=[r1k], w=[ck])
                        b.tt(r1[0:16, :], r1[0:16, :], cc[:, 4, :], ALU.subtract, r=[r1k, ck], w=[r1k])
                        b.copy(cc[:, 5, :], r1[0:16, :], r=[r1k], w=[ck])
                        b.ts1(cc[:, 0:3, :], cc[:, 3:6, :], -1.0, ALU.mult, r=[ck], w=[ck])
                        b.dma(sC[:, :, ts_], cc[:], r=[ck], w=[("sC", tb)], eng="pool")

                    for tt_ in range(4):
                        for half in range(2):
                            pb, pk = pbank()
                            for kc in range(KC):
                                b.mm(pb[:], hb[:, kc, tt_ * 128:(tt_ + 1) * 128],
                                     win[:, kc, 2048 + half * 512:2048 + (half + 1) * 512], kc == 0, kc == KC - 1,
                                     r=["win", hk], w=[pk])
                            s, sk = rot("stb", stb)
                            b.act(s[:], pb[:], AF.Copy, r=[pk], w=[sk])
                            b.dma(sV[tb * 4 + tt_][:, half * 512:(half + 1) * 512], s[:], r=[sk],
                                  w=[("sV", tb * 4 + tt_, half)], eng="pool")
                    for h in range(8):
                        pb, pk = proj_fm(3072 + h * 128)
                        s, sk = rot("stb", stb)
                        b.act(s[:], pb[:], AF.Silu, r=[pk], w=[sk])
                        b.dma(sZ[h][:, ts_], s[:], r=[sk], w=[("sZ", h, tb)], eng="pool")
                P.flush()
            if stop_after == "A%d" % l:
                return nc

            with contextlib.ExitStack() as phOT:
                if l + 1 < n_layers:
                    w_make(l + 1)
                if is_hg:
                    with contextlib.ExitStack() as ph:
                        SEG = 256
                        NSEG = S // SEG
                        Ql = [sb(ph, "Ql%d" % i, [128, 8, SEG], BF16) for i in range(2)]
                        Kl = [sb(ph, "Kl%d" % i, [128, 8, SEG], BF16) for i in range(2)]
                        Gl = [sb(ph, "Gl%d" % i, [128, 8, SEG], F32) for i in range(2)]
                        Vl = [sb(ph, "Vl%d" % i, [128, 2, 1024], BF16) for i in range(2)]
                        Zl = [sb(ph, "Zl%d" % i, [128, 8, SEG], BF16) for i in range(2)]
                        rmask = sb(ph, "rmask", [128, SEG], F32)
                        bcum = sb(ph, "bcum", [128, 4 * SEG], F32)
                        tmpe = sb(ph, "tmpe", [128, 4 * SEG], F32)
                        ebtL = [sb(ph, "ebt%d" % i, [128, 4 * SEG], F32) for i in range(2)]
                        qdL = [sb(ph, "qd%d" % i, [128, 4 * SEG], BF16) for i in range(2)]
                        kinvL = [sb(ph, "kinv%d" % i, [128, 4 * SEG], BF16) for i in range(2)]
                        kdTL = [sb(ph, "kdT%d" % i, [128, 4 * SEG], BF16) for i in range(2)]
                        ATs = [sb(ph, "ATs%d" % i, [128, 128], BF16) for i in range(4)]
                        kdk = [sb(ph, "kdk%d" % i, [128, 128], BF16) for i in range(4)]
                        Sf = sb(ph, "Sf", [128, 8, 128], F32)
                        Sb = sb(ph, "Sb", [128, 8, 128], BF16)
                        of = [sb(ph, "of%d" % i, [128, SEG], F32) for i in range(2)]
                        osq = [sb(ph, "osq%d" % i, [128, SEG], BF16) for i in range(2)]
                        orst = [sb(ph, "orst%d" % i, [128, SEG], F32) for i in range(2)]
                        ost = [sb(ph, "ost%d" % i, [128, SEG], BF16) for i in range(4)]
                        b.memset(rmask[:], 1.0, w=["rmask"])
                        b.memset(rmask[:].rearrange("p (c t) -> p c t", t=32)[:, :, 0:1], 0.0, w=["rmask"])
                        b.memset(Sf[:], 0.0, w=["Sf"])
                        b.memset(Sb[:], 0.0, w=["Sb"])
                        rc = {"at": 0, "ats": 0, "kdk": 0, "u": 0, "tp": 0, "post": 0, "ost": 0}
                        v3 = lambda t_: t_[:].rearrange("p (h t) -> p h t", h=4)
                        c3 = lambda t_: t_[:].rearrange("p (c t) -> p c t", t=32)

                        def hg_loads(sg):
                            sl = slice(sg * SEG, (sg + 1) * SEG)
                            lk = "L%d" % (sg % 2)
                            tbk = sg * SEG // 512
                            i2 = sg % 2
                            b.dma(Gl[i2][:], sG.rearrange("h p t -> p h t")[:, :, sl],
                                  r=[("sG", h, tbk) for h in range(8)], w=[lk + "g"])
                            b.dma(Ql[i2][:], sA.rearrange("h p t -> p h t")[:, :, sl],
                                  r=[("sA", h, tbk) for h in range(8)], w=[lk + "q"])
                            b.dma(Kl[i2][:], sK.rearrange("h p t -> p h t")[:, :, sl],
                                  r=[("sK", h, tbk) for h in range(8)], w=[lk + "k"])
                            b.dma(Vl[i2][:], sV.rearrange("n p f -> p n f")[:, sg * 2:sg * 2 + 2, :],
                                  r=[("sV", sg * 2 + a, hh) for a in range(2) for hh in range(2)], w=[lk + "v"])
                            b.dma(Zl[i2][:], sZ.rearrange("h p t -> p h t")[:, :, sl],
                                  r=[("sZ", h, tbk) for h in range(8)], w=[lk + "z"])

                        def hg_prep(u):
                            sg, hg = u // 2, u % 2
                            lk = "L%d" % (sg % 2)
                            p2 = u % 2
                            hs = slice(hg * 4, hg * 4 + 4)
                            ebt, qd, kinv, kdT = ebtL[p2], qdL[p2], kinvL[p2], kdTL[p2]
                            ek, qk_, kk_, dk_ = "ebt%d" % p2, "qd%d" % p2, "kinv%d" % p2, "kdT%d" % p2
                            for hh in range(4):
                                b.scan(bcum[:, hh * SEG:(hh + 1) * SEG], rmask[:], Gl[sg % 2][:, hg * 4 + hh, :], 0.0,
                                       r=[lk + "g", "rmask"], w=["bcum"])
                            b.act(ebt[:], bcum[:], AF.Exp, r=["bcum"], w=[ek])
                            b.act(tmpe[:], bcum[:], AF.Exp, r=["bcum"], w=["tmpe"], scale=-1.0)
                            b.tt(v3(qd), Ql[sg % 2][:, hs, :], v3(ebt), ALU.mult, r=[lk + "q", ek], w=[qk_], eng="pool")
                            b.tt(v3(kinv), Kl[sg % 2][:, hs, :], v3(tmpe), ALU.mult, r=[lk + "k", "tmpe"], w=[kk_],
                                 eng="pool")
                            b.tt(c3(kdT), c3(kinv), c3(ebt)[:, :, 31:32].to_broadcast([128, 4 * SEG // 32, 32]), ALU.mult,
                                 r=[kk_, ek], w=[dk_], eng="pool")

                        def hg_steps(u):
                            sg, hg = u // 2, u % 2
                            sl = slice(sg * SEG, (sg + 1) * SEG)
                            lk = "L%d" % (sg % 2)
                            p2 = u % 2
                            v_, z_ = Vl[sg % 2], Zl[sg % 2]
                            ebt, qd, kinv, kdT = ebtL[p2], qdL[p2], kinvL[p2], kdTL[p2]
                            ek, qk_, kk_, dk_ = "ebt%d" % p2, "qd%d" % p2, "kinv%d" % p2, "kdT%d" % p2
                            eb3 = c3(ebt)
                            for tl in range(SEG // 128):
                                asis = []
                                for hh in range(4):
                                    h = hg * 4 + hh
                                    cs = slice(hh * SEG + tl * 128, hh * SEG + (tl + 1) * 128)
                                    ai = rc["at"] % 2
                                    rc["at"] += 1
                                    atp = banks[4][:, ai * 128:(ai + 1) * 128]
                                    b.mm(atp, kinv[:, cs], qd[:, cs], True, True, r=[kk_, qk_], w=["bank4"])
                                    asi = rc["ats"] % 4
                                    rc["ats"] += 1
                                    b.tt(ATs[asi][:], atp, mask01[:], ALU.mult, r=["bank4", "mask01"], w=[("ATs", asi)])
                                    ti = rc["tp"] % 8
                                    rc["tp"] += 1
                                    tpp = bankT[:, ti * 128:(ti + 1) * 128]
                                    b.tr(tpp, kdT[:, cs], ident[:], r=[dk_, "ident"], w=["bankT"])
                                    ki = rc["kdk"] % 4
                                    rc["kdk"] += 1
                                    b.act(kdk[ki][:], tpp, AF.Copy, r=["bankT"], w=[("kdk", ki)])
                                    ob = banks[hh]
                                    oc = slice(tl * 128, (tl + 1) * 128)
                                    b.mm(ob[:, oc], v_[:, tl, h * 128:(h + 1) * 128], ATs[asi][:], True, False,
                                         r=[lk + "v", ("ATs", asi)], w=["bank%d" % hh])
                                for jj in range(4):
                                    for hh in range(4):
                                        h = hg * 4 + hh
                                        ob = banks[hh]
                                        cj = hh * SEG + tl * 128 + jj * 32
                                        ki = (rc["kdk"] - 4 + hh) % 4
                                        b.mm(ob[:, tl * 128 + jj * 32: tl * 128 + jj * 32 + 32],
                                             Sb[:, h, :], qd[:, cj:cj + 32], False, jj == 3,
                                             r=[("Sb", h), qk_], w=["bank%d" % hh])
                                        ui = rc["u"] % 2
                                        rc["u"] += 1
                                        up = banks[5 + ui][:, 0:128]
                                        tkw = {"tile_position": (96, 0)} if jj == 3 else {}
                                        b.mm(up, kdk[ki][32 * jj:32 * jj + 32, :],
                                             v_[32 * jj:32 * jj + 32, tl, h * 128:(h + 1) * 128], True, True,
                                             r=[("kdk", ki), lk + "v"], w=["bank%d" % (5 + ui)], **tkw)
                                        chunk = (hh * SEG + tl * 128 + jj * 32) // 32
                                        b.stt(Sf[:, h, :], Sf[:, h, :], eb3[:, chunk, 31:32], up, ALU.mult, ALU.add,
                                              r=[("Sf", h), ek, "bank%d" % (5 + ui)], w=[("Sf", h)])
                                        b.act(Sb[:, h, :], Sf[:, h, :], AF.Copy, r=[("Sf", h)], w=[("Sb", h)])
                            for hh in range(4):
                                h = hg * 4 + hh
                                pi = rc["post"] % 2
                                rc["post"] += 1
                                ob = banks[hh]
                                b.act(osq[pi][:], ob[:, 0:SEG], AF.Square, r=["bank%d" % hh], w=[("osq", pi)])
                                b.copy(of[pi][:], ob[:, 0:SEG], r=["bank%d" % hh], w=[("of", pi)])
                                ssb = banks[4][:, 256:256 + SEG]
                                b.mm(ssb, ones_bf[:], osq[pi][:], True, True, r=[("osq", pi), "ones_bf"], w=["bank4"])
                                b.act(orst[pi][:], ssb, AF.Sqrt, r=["bank4"], w=[("orst", pi)], scale=1.0 / 128, bias=EPS)
                                b.recip(orst[pi][:], orst[pi][:], r=[("orst", pi)], w=[("orst", pi)])
                                b.stt(of[pi][:], of[pi][:], ogsb[:, j, h:h + 1], orst[pi][:], ALU.mult, ALU.mult,
                                      r=[("of", pi), ("orst", pi), "ogsb"], w=[("of", pi)])
                                oi_ = rc["ost"] % 4
                                rc["ost"] += 1
                                b.tt(ost[oi_][:], of[pi][:], z_[:, h, :], ALU.mult, r=[("of", pi), lk + "z"],
                                     w=[("ost", oi_)], eng="pool")
                                b.dma(sO[h][:, sl], ost[oi_][:], r=[("ost", oi_)], w=[("sO", h, sg)], eng="pool")

                        NU = NSEG * 2
                        hg_loads(0)
                        hg_prep(0)
                        for u in range(NU):
                            if u % 2 == 0 and u // 2 + 1 < NSEG:
                                hg_loads(u // 2 + 1)
                            if u + 1 < NU:
                                hg_prep(u + 1)
                            hg_steps(u)
                            w_tick()
                        w_drain()
                        P.flush()
                else:
                    with contextlib.ExitStack() as ph:
                        qa = [sb(ph, "qa%d" % i, [70, S], BF16) for i in range(2)]
                        ka = [sb(ph, "ka%d" % i, [70, S], BF16) for i in range(2)]
                        va = [sb(ph, "va%d" % i, [128, NT, 128], BF16) for i in range(2)]
                        zt = [sb(ph, "zt%d" % i, [64, S], BF16) for i in range(2)]
                        PT = [sb(ph, "PT%d" % i, [128, 512], BF16) for i in range(4)]
                        rl = [sb(ph, "rl%d" % i, [64, 512], F32) for i in range(2)]
                        t1 = [sb(ph, "t1%d" % i, [64, 512], F32) for i in range(2)]
                        ost = [sb(ph, "ost%d" % i, [64, 512], BF16) for i in range(2)]
                        for i in range(2):
                            b.memset(qa[i][64:70, :], 1.0, w=[("qa1", i), ("qac", i)])
                            b.memset(ka[i][64:70, :], 1.0, w=[("ka1", i), ("kac", i)])
                            b.memset(va[i][:, :, 64:128], 1.0, w=[("va1", i)])
                        rc = {"st": 0, "pt": 0, "oa": 0, "fin": 0}

                        def load_head(h):
                            hp, ho = h // 2, (h % 2) * 64
                            i = h % 2
                            allA = [("sA", hp, tb) for tb in range(NB)]
                            allK = [("sK", hp, tb) for tb in range(NB)]
                            allC = [("sC", tb) for tb in range(NB)]
                            b.dma(qa[i][0:64, :], sA[hp][ho:ho + 64, :], r=allA, w=[("qaq", i)])
                            b.dma(qa[i][64:67, :], sC[h, 0:3, :], r=allC, w=[("qac", i)])
                            b.dma(ka[i][0:64, :], sK[hp][ho:ho + 64, :], r=allK, w=[("kaq", i)])
                            b.dma(ka[i][67:70, :], sC[h, 3:6, :], r=allC, w=[("kac", i)])
                            for n0 in range(0, NT, 8):
                                n1 = min(NT, n0 + 8)
                                b.dma(va[i][:, n0:n1, 0:64],
                                      sV.rearrange("n p f -> p n f")[:, n0:n1, h * 64:(h + 1) * 64],
                                      r=[("sV", n, hh) for n in range(n0, n1) for hh in range(2)], w=[("vav", i, n0)])
                            b.dma(zt[i][:, :], sZ[hp][ho:ho + 64, :], r=[("sZ", hp, tb) for tb in range(NB)],
                                  w=["zt%d" % i])

                        steps = [(h, qs, kb) for h in range(16) for qs in range(NB) for kb in range(4 * qs + 4)]
                        info = {}
                        span_oa = {}
                        LOOK = 2

                        def emit_qk(idx):
                            h, qs, kb = steps[idx]
                            i = h % 2
                            jd = kb - 4 * qs
                            c0 = 128 * jd if jd > 0 else 0
                            si = rc["st"] % 3
                            rc["st"] += 1
                            ST = banks[si]
                            b.mm(ST[:, c0:512], ka[i][0:70, kb * 128:(kb + 1) * 128],
                                 qa[i][0:70, qs * 512 + c0:(qs + 1) * 512], True, jd < 0,
                                 r=[("kaq", i), ("kac", i), ("ka1", i), ("qaq", i), ("qac", i), ("qa1", i)],
                                 w=["bank%d" % si])
                            if jd >= 0:
                                b.mm(ST[:, c0:c0 + 128], ident[:], maskbias[:], False, True,
                                     r=["ident", "maskbias"], w=["bank%d" % si])
                            pi = rc["pt"] % 4
                            rc["pt"] += 1
                            b.act(PT[pi][:, c0:512], ST[:, c0:512], AF.Exp, r=["bank%d" % si], w=[("PT", pi)])
                            info[idx] = (c0, pi)

                        def emit_pv(idx):
                            h, qs, kb = steps[idx]
                            hp, ho = h // 2, (h % 2) * 64
                            i = h % 2
                            c0, pi = info.pop(idx)
                            nkb = 4 * qs + 4
                            if kb == 0:
                                span_oa[(h, qs)] = rc["oa"] % 2
                                rc["oa"] += 1
                            oi = span_oa[(h, qs)]
                            OA = banks[3 + oi]
                            b.mm(OA[:, c0:512], va[i][:, kb, :], PT[pi][:, c0:512], kb == 0, kb == nkb - 1,
                                 r=[("vav", i, (kb // 8) * 8), ("va1", i), ("PT", pi)], w=["bank%d" % (3 + oi)])
                            if kb == nkb - 1:
                                fi = rc["fin"] % 2
                                rc["fin"] += 1
                                b.recip(rl[fi][:], OA[64:128, :], r=["bank%d" % (3 + oi)], w=[("rl", fi)])
                                b.tt(t1[fi][:], OA[0:64, :], rl[fi][:], ALU.mult, r=["bank%d" % (3 + oi), ("rl", fi)],
                                     w=[("t1", fi)])
                                b.tt(ost[fi][:], t1[fi][:],
                                     zt[i][:, qs * 512:(qs + 1) * 512], ALU.mult, r=[("t1", fi), "zt%d" % i],
                                     w=[("ost", fi)], eng="pool")
                                b.dma(sO[hp][ho:ho + 64, qs * 512:(qs + 1) * 512], ost[fi][:], r=[("ost", fi)],
                                      w=[("sO", h, qs)], eng="pool")
                                if qs == NB - 1:
                                    w_tick()
                                    if h + 2 < 16:
                                        load_head(h + 2)

                        load_head(0)
                        load_head(1)
                        for idx in range(len(steps) + LOOK):
                            if idx < len(steps):
                                emit_qk(idx)
                            if idx - LOOK >= 0:
                                emit_pv(idx - LOOK)
                        w_drain()
                        P.flush()

                if stop_after == "B%d" % l:
                    return nc
                with contextlib.ExitStack() as ph:
                    xc = [sb(ph, "xc%d" % i, [128, KC, 512], F32) for i in range(2)]
                    otb = [sb(ph, "otb%d" % i, [128, KC, 512], BF16) for i in range(2)]
                    sO_v = sO.rearrange("h p t -> p h t")
                    nb_ = 0
                    for tb in range(NB):
                        ts_ = slice(tb * 512, (tb + 1) * 512)
                        xb, xk = xc[tb % 2], "xc%d" % (tb % 2)
                        ob_, ok_ = otb[tb % 2], "otb%d" % (tb % 2)
                        if tb == 0:
                            b.dma(ob_[:], sO_v[:, :, ts_], r=[], w=[ok_])
                            b.dma(xb[:], xsrc_v[:, :, ts_], r=[("x", tb)], w=[xk])
                        if tb + 1 < NB:
                            b.dma(otb[(tb + 1) % 2][:], sO_v[:, :, (tb + 1) * 512:(tb + 2) * 512], r=[],
                                  w=["otb%d" % ((tb + 1) % 2)])
                            b.dma(xc[(tb + 1) % 2][:], xsrc_v[:, :, (tb + 1) * 512:(tb + 2) * 512],
                                  r=[("x", tb + 1)], w=["xc%d" % ((tb + 1) % 2)])
                        for dc in range(KC):
                            bi = nb_ % 4
                            nb_ += 1
                            pb, pk = banks[bi], "bank%d" % bi
                            for fc in range(KC):
                                b.mm(pb[:], wout[:, fc, dc * 128:(dc + 1) * 128], ob_[:, fc, :], fc == 0, fc == KC - 1,
                                     r=["wout", ok_], w=[pk])
                            b.stt(xb[:, dc, :], pb[:], modsb[:, l, 16 + dc:17 + dc], xb[:, dc, :], ALU.mult, ALU.add,
                                  r=[pk, "modsb", xk], w=[xk])
                        b.dma(y_v[:, :, ts_], xb[:], r=[xk], w=[("x", tb)], eng="pool")
                    P.flush()
    return nc


def _consts():
    s = np.arange(128)[:, None]
    t = np.arange(128)[None, :]
    maskbias = np.where(s > t, NEG, 0.0).astype(np.float32)
    mask01 = ((s // 32 == t // 32) & (s <= t)).astype(np.float32)
    mj = (s // 32 == np.arange(4)[None, :]).astype(np.float32)
    blk64 = (s // 64 == t // 64).astype(np.float32)
    return dict(c_maskbias=maskbias, c_mask01=mask01, c_mj=mj, c_blk64=blk64)


def _pcol(v, n):
    return np.ascontiguousarray(np.asarray(v, np.float32).reshape(n, 128).T)


def make_in_maps(inputs, n_cores, S):
    f = lambda a: np.ascontiguousarray(np.asarray(a, dtype=np.float32))
    x = f(inputs["x"])
    c = f(inputs["c"])
    shared = dict(
        gT=np.ascontiguousarray(np.stack([_pcol(g, 8) for g in f(inputs["norm_g"])], 1)),
        ada_w=f(inputs["ada_w"]),
        ada_bT=np.ascontiguousarray(np.stack([_pcol(v, 24) for v in f(inputs["ada_b"])], 1)),
        lbT=np.ascontiguousarray(np.stack([_pcol(v, 8) for v in f(inputs["hg_lb_logits"])], 1)),
        hg_w_in=f(inputs["hg_w_in"]),
        ogT=np.ascontiguousarray(np.stack([_pcol(v, 8) for v in f(inputs["hg_o_g"])], 1)),
        hg_w_out=f(inputs["hg_w_out"]),
        fx_w_in=f(inputs["fx_w_in"]),
        bfT=np.ascontiguousarray(f(inputs["fx_b_f"]).T),
        qgT=np.ascontiguousarray(np.tile(f(inputs["fx_q_g"]).T, (2, 1))),
        kgT=np.ascontiguousarray(np.tile(f(inputs["fx_k_g"]).T, (2, 1))),
        fx_w_out=f(inputs["fx_w_out"]),
    )
    shared.update(_consts())
    maps = []
    for i in range(n_cores):
        m = dict(shared)
        m["xT"] = np.ascontiguousarray(x[i].T)
        m["cT"] = _pcol(c[i], 8)
        maps.append(m)
    return maps


_NC_CACHE = {}


def run(inputs, n_layers=4, stop_after=None):
    x = np.asarray(inputs["x"])
    Bn, S, _ = x.shape
    key = (S, n_layers, stop_after)
    if key not in _NC_CACHE:
        _NC_CACHE[key] = build(S, n_layers, stop_after)
    nc = _NC_CACHE[key]
    maps = make_in_maps(inputs, Bn, S)
    res = run_bass_kernel_spmd(nc, maps, core_ids=list(range(Bn)))
    out = np.stack([np.ascontiguousarray(res.results[i]["yT"].T) for i in range(Bn)], 0)
    return out.astype(np.float32)


def kernel(**inputs):
    return run(inputs, n_layers=4)
```

```python
import contextlib
import numpy as np
import concourse.bass as bass
import concourse.mybir as mybir
from concourse.bass_utils import run_bass_kernel_spmd

F32 = mybir.dt.float32
BF16 = mybir.dt.bfloat16
ALU = mybir.AluOpType
AF = mybir.ActivationFunctionType

ENGS = ("pe", "act", "dve", "pool", "sp")
D = 1024
KC = 8
EPS = 1e-6
SAME_ENG_SYNC = False
NEG = -30000.0


class Prog:
    def __init__(self, nc, es, n_dma_sems=40):
        self.nc = nc
        self.ops = []
        self.buf = {}
        self.n_dma_sems = n_dma_sems
        self.dma_rr = 0
        self.dma_rr_e = {}
        self.slot_ord = [0] * n_dma_sems
        self.slot_last = {}
        self.flushed = 0
        self.cnt = {e: 0 for e in ENGS}
        self.esem = {e: es.enter_context(nc.semaphore("s_" + e)) for e in ENGS}
        self.dsem = [es.enter_context(nc.semaphore("d_%d" % k)) for k in range(n_dma_sems)]
        self.nflush = 0

    def op(self, eng, fn, r=(), w=(), dma=False):
        pk_ = [k for k in r if isinstance(k, str) and k.startswith("bank")]
        if pk_:
            w = list(w) + [k for k in pk_ if k not in w]
            r = [k for k in r if k not in pk_]
        deps = set()
        for b in r:
            st = self.buf.get(b)
            if st is not None and st[0] is not None:
                deps.add(st[0])
        for b in w:
            st = self.buf.get(b)
            if st is not None:
                if st[0] is not None:
                    deps.add(st[0])
                deps.update(st[1])
        i = len(self.ops)
        o = dict(eng=eng, fn=fn, deps=deps, dma=dma, slot=None, ordinal=None, count=None)
        if dma:
            lo_, hi_ = (0, 28) if eng == "sp" else (28, self.n_dma_sems)
            k_ = self.dma_rr_e.get(eng, 0)
            self.dma_rr_e[eng] = k_ + 1
            s = lo_ + k_ % (hi_ - lo_)
            prev = self.slot_last.get(s)
            if prev is not None:
                deps.add(prev)
            self.slot_ord[s] += 1
            o["ordinal"] = self.slot_ord[s]
            o["slot"] = s
            self.slot_last[s] = i
        self.ops.append(o)
        for b in r:
            st = self.buf.setdefault(b, [None, []])
            st[1].append(i)
        for b in w:
            self.buf[b] = [i, []]
        return i

    def flush(self):
        nc = self.nc
        ops = self.ops
        lo, hi = self.flushed, len(ops)
        if lo == hi:
            return
        base_cnt = dict(self.cnt)
        base_ord = list(self.slot_ord_flushed) if hasattr(self, "slot_ord_flushed") else [0] * self.n_dma_sems
        waited = {e: {f: -1 for f in ENGS} for e in ENGS}
        waited_dma = {e: {} for e in ENGS}
        needed = {}
        for i in range(lo, hi):
            o = ops[i]
            e = o["eng"]
            wl = []
            for d in sorted(o["deps"]):
                if d < lo:
                    continue
                od = ops[d]
                if od["dma"]:
                    if waited_dma[e].get(od["slot"], 0) >= od["ordinal"]:
                        continue
                    waited_dma[e][od["slot"]] = od["ordinal"]
                    wl.append(("dma", od["slot"], od["ordinal"]))
                else:
                    f = od["eng"]
                    if f == e and (e == "pe" or (not SAME_ENG_SYNC and e != "pool")):
                        continue
                    if waited[e][f] >= d:
                        continue
                    waited[e][f] = d
                    needed[d] = True
                    wl.append(("eng", f, d))
            o["waits"] = wl
        last_of = {}
        for i in range(lo, hi):
            if not ops[i]["dma"]:
                last_of[ops[i]["eng"]] = i
        for e, i in last_of.items():
            needed[i] = True
        for i in range(lo, hi):
            o = ops[i]
            if o["dma"]:
                continue
            if needed.get(i):
                self.cnt[o["eng"]] += 1
                o["count"] = self.cnt[o["eng"]]
        per_eng = {e: [i for i in range(lo, hi) if ops[i]["eng"] == e] for e in ENGS}
        esem, dsem = self.esem, self.dsem
        first = self.nflush == 0
        self.nflush += 1
        final_waits = {e: [] for e in ENGS}
        cur_last = {}
        for i in range(lo, hi):
            if ops[i]["dma"]:
                cur_last[ops[i]["slot"]] = i
        for s, i in cur_last.items():
            final_waits[ops[i]["eng"]].append((s, ops[i]["ordinal"]))

        def replay(ename, eng):
            if not first:
                for f in ENGS:
                    if f != ename and base_cnt[f] > 0:
                        eng.wait_ge(esem[f], base_cnt[f])
            for i in per_eng[ename]:
                o = ops[i]
                for wt in o["waits"]:
                    if wt[0] == "dma":
                        eng.wait_ge(dsem[wt[1]], 16 * wt[2])
                    else:
                        eng.wait_ge(esem[wt[1]], ops[wt[2]]["count"])
                ins = o["fn"](eng)
                if o["dma"]:
                    ins.then_inc(dsem[o["slot"]], 16)
                elif needed.get(i):
                    ins.then_inc(esem[ename], 1)
                o["fn"] = None
            for s, ordn in final_waits[ename]:
                eng.wait_ge(dsem[s], 16 * ordn)

        with nc.Block() as block:
            def wrap(ename):
                def f(eng):
                    replay(ename, eng)
                    if final_waits[ename]:
                        self.cnt[ename] += 1
                        eng.nop().then_inc(esem[ename], 1)
                return f
            block.tensor(wrap("pe"))
            block.scalar(wrap("act"))
            block.vector(wrap("dve"))
            block.gpsimd(wrap("pool"))
            block.sync(wrap("sp"))
        self.flushed = hi
        self.slot_ord_flushed = list(self.slot_ord)


class B:
    def __init__(self, P):
        self.P = P

    def dma(self, out, in_, r, w, eng="sp"):
        return self.P.op(eng, lambda e: e.dma_start(out=out, in_=in_), r=r, w=w, dma=True)

    def mm(self, out, lhsT, rhs, start, stop, r, w, **kw):
        return self.P.op("pe", lambda e: e.matmul(out, lhsT=lhsT, rhs=rhs, start=start, stop=stop,
                                                  skip_group_check=True, **kw), r=r, w=w)

    def tr(self, out, in_, ident, r, w):
        return self.P.op("pe", lambda e: e.transpose(out=out, in_=in_, identity=ident), r=r, w=w)

    def act(self, out, in_, func, r, w, scale=1.0, bias=0.0):
        return self.P.op("act", lambda e: e.activation(out=out, in_=in_, func=func, scale=scale, bias=bias), r=r, w=w)

    def tt(self, out, in0, in1, op, r, w, eng="dve"):
        return self.P.op(eng, lambda e: e.tensor_tensor(out=out, in0=in0, in1=in1, op=op), r=r, w=w)

    def ts(self, out, in0, s1, s2, op0, op1, r, w, eng="dve"):
        return self.P.op(eng, lambda e: e.tensor_scalar(out=out, in0=in0, scalar1=s1, scalar2=s2, op0=op0, op1=op1),
                         r=r, w=w)

    def ts1(self, out, in0, s1, op0, r, w, eng="dve"):
        return self.P.op(eng, lambda e: e.tensor_single_scalar(out=out, in_=in0, scalar=s1, op=op0), r=r, w=w)

    def stt(self, out, in0, scalar, in1, op0, op1, r, w, eng="dve"):
        return self.P.op(eng, lambda e: e.scalar_tensor_tensor(out=out, in0=in0, scalar=scalar, in1=in1,
                                                               op0=op0, op1=op1), r=r, w=w)

    def copy(self, out, in_, r, w, eng="dve"):
        return self.P.op(eng, lambda e: e.tensor_copy(out=out, in_=in_), r=r, w=w)

    def recip(self, out, in_, r, w):
        return self.P.op("dve", lambda e: e.reciprocal(out=out, in_=in_), r=r, w=w)

    def memset(self, ap, val, w, eng="pool"):
        return self.P.op(eng, lambda e: e.memset(ap, val), r=(), w=w)

    def scan(self, out, d0, d1, initial, r, w):
        return self.P.op("dve", lambda e: e.tensor_tensor_scan(out=out, data0=d0, data1=d1, initial=initial,
                                                               op0=ALU.mult, op1=ALU.add), r=r, w=w)


def build(S, n_layers, stop_after=None):
    NB = S // 512
    NT = S // 128
    nc = bass.Bass("TRN2", target_bir_lowering=False)

    def din(name, shape, dt=F32):
        return nc.dram_tensor(name, list(shape), dt, kind="ExternalInput").ap()

    def dscr(name, shape, dt):
        return nc.dram_tensor(name, list(shape), dt, kind="Internal").ap()

    xT = din("xT", [D, S])
    cT = din("cT", [128, KC])
    gT = din("gT", [128, 4, KC])
    ada_w = din("ada_w", [4, D, 3 * D])
    ada_bT = din("ada_bT", [128, 4, 24])
    lbT = din("lbT", [128, 2, 8])
    hg_w_in = din("hg_w_in", [2, D, 4096])
    ogT = din("ogT", [128, 2, 8])
    hg_w_out = din("hg_w_out", [2, D, D])
    fx_w_in = din("fx_w_in", [2, D, 4112])
    bfT = din("bfT", [16, 2])
    qgT = din("qgT", [128, 2])
    kgT = din("kgT", [128, 2])
    fx_w_out = din("fx_w_out", [2, D, D])
    c_maskbias = din("c_maskbias", [128, 128])
    c_mask01 = din("c_mask01", [128, 128])
    c_mj = din("c_mj", [128, 4])
    c_blk64 = din("c_blk64", [128, 128])
    yT = nc.dram_tensor("yT", [D, S], F32, kind="ExternalOutput").ap()

    sA = dscr("sA", [8, 128, S], BF16)
    sK = dscr("sK", [8, 128, S], BF16)
    sG = dscr("sG", [8, 128, S], F32)
    sV = dscr("sV", [NT, 128, 1024], BF16)
    sZ = dscr("sZ", [8, 128, S], BF16)
    sC = dscr("sC", [16, 6, S], BF16)

    with contextlib.ExitStack() as es:
        P = Prog(nc, es)
        b = B(P)

        uid = [0]

        def sb(stack, name, shape, dt):
            uid[0] += 1
            return stack.enter_context(nc.sbuf_tensor("%s_u%d" % (name, uid[0]), list(shape), dt))

        banks = [es.enter_context(nc.psum_tensor("bank%d" % i, [128, 512], F32)) for i in range(7)]
        bankT = es.enter_context(nc.psum_tensor("bankT", [128, 1024], BF16))
        ident = sb(es, "ident", [128, 128], BF16)
        ones_bf = sb(es, "ones_bf", [128, 128], BF16)
        blk64 = sb(es, "blk64", [128, 128], BF16)
        maskbias = sb(es, "maskbias", [128, 128], BF16)
        mask01 = sb(es, "mask01", [128, 128], F32)
        mj = sb(es, "mj", [128, 4], F32)
        ctmp = sb(es, "ctmp", [128, 128], F32)
        cact = sb(es, "cact", [128, KC], F32)
        gsb = sb(es, "gsb", [128, 4, KC], F32)
        modsb = sb(es, "modsb", [128, 4, 24], F32)
        asb = sb(es, "asb", [128, 4, KC], F32)
        lbsb = sb(es, "lbsb", [128, 2, 8], F32)
        omlsb = sb(es, "omlsb", [128, 2, 8], F32)
        ogsb = sb(es, "ogsb", [128, 2, 8], F32)
        nbf = sb(es, "nbf", [16, 2], F32)
        qg = sb(es, "qg", [128, 2], F32)
        kg = sb(es, "kg", [128, 2], F32)
        wout = sb(es, "wout", [128, KC, D], BF16)
        onesf = sb(es, "onesf", [128, 512], F32)
        win = sb(es, "win", [128, KC, 4112], BF16)
        wst = [sb(es, "wst%d" % i, [128, 2048], F32) for i in range(2)]
        sO = dscr("sO", [8, 128, S], BF16)
        wq = {"steps": [], "k": 0}

        def w_make(l_):
            is_hg_ = (l_ % 2 == 0)
            w_ = (hg_w_in[l_ // 2] if is_hg_ else fx_w_in[l_ // 2]).rearrange("(kc p) f -> p kc f", p=128)
            ncol = 4096 if is_hg_ else 4112
            for kc in range(KC):
                for c0 in range(0, ncol, 2048):
                    wq["steps"].append((w_, kc, c0, min(ncol, c0 + 2048)))

        def w_tick(n=1):
            for _ in range(n):
                pend = wq.get("pend")
                if pend is not None:
                    kc, c0, c1, st, key = pend
                    b.copy(win[:, kc, c0:c1], st[:, 0:c1 - c0], r=[key], w=["win"], eng="pool")
                    wq["pend"] = None
                if not wq["steps"]:
                    continue
                w_, kc, c0, c1 = wq["steps"].pop(0)
                k_ = wq["k"]
                wq["k"] += 1
                st, key = wst[k_ % 2], "wst%d" % (k_ % 2)
                b.dma(st[:, 0:c1 - c0], w_[:, kc, c0:c1], r=[], w=[key])
                wq["pend"] = (kc, c0, c1, st, key)

        def w_drain():
            while wq["steps"] or wq.get("pend") is not None:
                w_tick()

        with contextlib.ExitStack() as ph:
            adast = [sb(ph, "adast%d" % i, [128, KC, 512], F32) for i in range(2)]
            b.memset(ident[:], 0.0, w=["ident"])
            P.op("pool", lambda e: e.affine_select(out=ident[:], in_=ident[:], pattern=[[-1, 128]],
                                                   compare_op=ALU.not_equal, fill=1.0, base=0,
                                                   channel_multiplier=1), r=["ident"], w=["ident"])
            b.memset(ones_bf[:], 1.0, w=["ones_bf"])
            b.memset(onesf[:], 1.0, w=["onesf"])
            for (src, dst, nm) in ((c_maskbias, maskbias, "maskbias"), (c_blk64, blk64, "blk64")):
                b.dma(ctmp[:], src, r=[], w=["ctmp"])
                b.copy(dst[:], ctmp[:], r=["ctmp"], w=[nm])
            b.dma(mask01[:], c_mask01, r=[], w=["mask01"])
            b.dma(mj[:], c_mj, r=[], w=["mj"])
            b.dma(cact[:], cT, r=[], w=["cact"])
            b.dma(gsb[:], gT, r=[], w=["gsb"])
            b.dma(modsb[:], ada_bT, r=[], w=["modsb"])
            b.dma(lbsb[:], lbT, r=[], w=["lbsb"])
            b.dma(ogsb[:], ogT, r=[], w=["ogsb"])
            b.dma(nbf[:], bfT, r=[], w=["nbf"])
            b.dma(qg[:], qgT, r=[], w=["qg"])
            b.dma(kg[:], kgT, r=[], w=["kg"])
            b.act(cact[:], cact[:], AF.Silu, r=["cact"], w=["cact"])
            b.tt(lbsb[:, 1, :], lbsb[:, 1, :], lbsb[:, 0, :], ALU.subtract, r=["lbsb"], w=["lbsb"])
            b.act(lbsb[:, 1, :], lbsb[:, 1, :], AF.Sigmoid, r=["lbsb"], w=["lbsb"])
            b.memset(lbsb[:, 0, :], 0.0, w=["lbsb"], eng="dve")
            b.ts(omlsb[:], lbsb[:], -1.0, 1.0, ALU.mult, ALU.add, r=["lbsb"], w=["omlsb"])
            b.ts1(nbf[:], nbf[:], -1.0, ALU.mult, r=["nbf"], w=["nbf"])
            b.ts1(qg[:], qg[:], 0.125, ALU.mult, r=["qg"], w=["qg"])
            modps = banks[0]
            k = 0
            for l in range(n_layers):
                for jb in range(6):
                    st = adast[k % 2]
                    key = "adast%d" % (k % 2)
                    k += 1
                    b.dma(st[:], ada_w[l].rearrange("(kc p) j -> p kc j", p=128)[:, :, jb * 512:(jb + 1) * 512],
                          r=[], w=[key])
                    for jj in range(4):
                        col = l * 24 + jb * 4 + jj
                        for kc in range(KC):
                            b.mm(modps[:, col:col + 1], st[:, kc, jj * 128:(jj + 1) * 128], cact[:, kc:kc + 1],
                                 kc == 0, kc == KC - 1, r=[key, "cact"], w=["bank0"])
            nl = n_layers
            b.tt(modsb[:, 0:nl, :], modsb[:, 0:nl, :], modps[:, 0:nl * 24].rearrange("p (l j) -> p l j", j=24),
                 ALU.add, r=["bank0", "modsb"], w=["modsb"])
            b.stt(asb[:, 0:nl, :], modsb[:, 0:nl, 8:16], 1.0, gsb[:, 0:nl, :], ALU.add, ALU.mult,
                  r=["modsb", "gsb"], w=["asb"])
            P.flush()
        if stop_after == "p0":
            return nc

        for l in range(n_layers):
            is_hg = (l % 2 == 0)
            j = l // 2
            xsrc = xT if l == 0 else yT
            w_in = hg_w_in[j] if is_hg else fx_w_in[j]
            w_out = hg_w_out[j] if is_hg else fx_w_out[j]
            NCOL = 4096 if is_hg else 4112
            xsrc_v = xsrc.rearrange("(kc p) t -> p kc t", p=128)
            y_v = yT.rearrange("(kc p) t -> p kc t", p=128)

            with contextlib.ExitStack() as ph:
                xblk = [sb(ph, "xblk%d" % i, [128, KC, 512], F32) for i in range(1)]
                hT = [sb(ph, "hT%d" % i, [128, KC, 512], BF16) for i in range(2)]
                sq = [sb(ph, "sq%d" % i, [128, 512], BF16) for i in range(2)]
                rstd = sb(ph, "rstd", [128, 512], F32)
                tmpf = [sb(ph, "tmpf%d" % i, [128, 512], F32) for i in range(4)]
                stb = [sb(ph, "stb%d" % i, [128, 512], BF16) for i in range(6)]
                stf = [sb(ph, "stf%d" % i, [128, 512], F32) for i in range(3)]
                ncum = [sb(ph, "ncum%d" % i, [16, 512], F32) for i in range(2)]
                c6 = [sb(ph, "c6_%d" % i, [16, 6, 512], BF16) for i in range(2)]
                cnt = {"tmpf": 0, "stb": 0, "stf": 0, "bank": 0, "wst": 0, "ss2": 0}

                def rot(name, lst):
                    i = cnt[name] % len(lst)
                    cnt[name] += 1
                    return lst[i], "%s%d" % (name, i)

                pb_list = [1, 2, 3, 4, 5, 6] if is_hg else [1, 2, 3, 4]

                def pbank():
                    i = pb_list[cnt["bank"] % len(pb_list)]
                    cnt["bank"] += 1
                    return banks[i], "bank%d" % i

                if l == 0:
                    w_make(0)
                w_drain()
                w_out_v = w_out.rearrange("(kc p) f -> p kc f", p=128)
                for kc in range(0, KC, 2):
                    k_ = wq["k"]
                    wq["k"] += 1
                    st, key = wst[k_ % 2], "wst%d" % (k_ % 2)
                    b.dma(st[:].rearrange("p (a f) -> p a f", a=2), w_out_v[:, kc:kc + 2, :], r=[], w=[key])
                    b.copy(wout[:, kc:kc + 2, :], st[:].rearrange("p (a f) -> p a f", a=2), r=[key], w=["wout"],
                           eng="pool")

                for tb in range(NB):
                    ts_ = slice(tb * 512, (tb + 1) * 512)
                    xb, xk = xblk[0], "xblk0"
                    hb, hk = hT[tb % 2], "hT%d" % (tb % 2)
                    if tb == 0:
                        b.dma(xb[:], xsrc_v[:, :, ts_], r=[("x", tb)], w=[xk])
                    ssp = banks[0]
                    for kc in range(KC):
                        b.act(sq[kc % 2][:], xb[:, kc, :], AF.Square, r=[xk], w=["sq%d" % (kc % 2)])
                        b.mm(ssp[:], ones_bf[:], sq[kc % 2][:], kc == 0, kc == KC - 1,
                             r=["sq%d" % (kc % 2), "ones_bf"], w=["bank0"])
                    b.act(rstd[:], ssp[:], AF.Sqrt, r=["bank0"], w=["rstd"], scale=1.0 / D, bias=EPS)
                    b.recip(rstd[:], rstd[:], r=["rstd"], w=["rstd"])
                    for kc in range(KC):
                        t, tk = rot("tmpf", tmpf)
                        b.stt(t[:], xb[:, kc, :], asb[:, l, kc:kc + 1], rstd[:], ALU.mult, ALU.mult,
                              r=[xk, "asb", "rstd"], w=[tk])
                        b.act(hb[:, kc, :], t[:], AF.Identity, r=[tk, "modsb"], w=[hk],
                              bias=modsb[:, l, kc:kc + 1])

                    if tb + 1 < NB:
                        b.dma(xb[:], xsrc_v[:, :, (tb + 1) * 512:(tb + 2) * 512], r=[("x", tb + 1)], w=[xk])

                    def proj_fm(c0, M=128):
                        pb, pk = pbank()
                        for kc in range(KC):
                            b.mm(pb[0:M, :], win[:, kc, c0:c0 + M], hb[:, kc, :], kc == 0, kc == KC - 1,
                                 r=["win", hk], w=[pk])
                        return pb, pk

                    if is_hg:
                        for h in range(8):
                            pb, pk = proj_fm(h * 128)
                            s, sk = rot("stb", stb)
                            b.act(s[:], pb[:], AF.Copy, r=[pk], w=[sk])
                            b.dma(sA[h][:, ts_], s[:], r=[sk], w=[("sA", h, tb)], eng="pool")
                            pb, pk = proj_fm(1024 + h * 128)
                            t, tk = rot("tmpf", tmpf)
                            b.act(t[:], pb[:], AF.Sigmoid, r=[pk], w=[tk])
                            b.ts(t[:], t[:], omlsb[:, j, h:h + 1], lbsb[:, j, h:h + 1], ALU.mult, ALU.add,
                                 r=[tk, "omlsb", "lbsb"], w=[tk])
                            g_, gk = rot("stf", stf)
                            b.act(g_[:], t[:], AF.Ln, r=[tk], w=[gk])
                            b.dma(sG[h][:, ts_], g_[:], r=[gk], w=[("sG", h, tb)], eng="pool")
                            s, sk = rot("stb", stb)
                            b.ts(s[:], t[:], -1.0, 1.0, ALU.mult, ALU.add, r=[tk], w=[sk])
                            b.dma(sK[h][:, ts_], s[:], r=[sk], w=[("sK", h, tb)], eng="pool")
                    else:
                        for hp in range(8):
                            for (c0, dst, gv, nm) in ((hp * 128, sA, qg, "sA"), (1024 + hp * 128, sK, kg, "sK")):
                                pb, pk = proj_fm(c0)
                                s2, s2k = rot("stb", stb)
                                b.act(s2[:], pb[:], AF.Square, r=[pk], w=[s2k])
                                sbi = 5 + cnt["ss2"] % 2
                                cnt["ss2"] += 1
                                b.mm(banks[sbi][:], blk64[:], s2[:], True, True, r=[s2k, "blk64"], w=["bank%d" % sbi])
                                r2, r2k = rot("tmpf", tmpf)
                                b.act(r2[:], banks[sbi][:], AF.Sqrt, r=["bank%d" % sbi], w=[r2k], scale=1.0 / 64, bias=EPS)
                                b.recip(r2[:], r2[:], r=[r2k], w=[r2k])
                                s, sk = rot("stb", stb)
                                b.stt(s[:], pb[:], gv[:, j:j + 1], r2[:], ALU.mult, ALU.mult,
                                      r=[pk, r2k, "qg", "kg"], w=[sk])
                                b.dma(dst[hp][:, ts_], s[:], r=[sk], w=[(nm, hp, tb)], eng="pool")
                        pb, pk = proj_fm(4096, M=16)
                        t, tk = rot("tmpf", tmpf)
                        b.act(t[0:16, :], pb[0:16, :], AF.Exp, r=[pk, "nbf"], w=[tk], scale=-1.0, bias=nbf[:, j:j + 1])
                        b.act(t[0:16, :], t[0:16, :], AF.Ln, r=[tk], w=[tk], scale=1.0, bias=1.0)
                        nc_, nk = ncum[tb % 2], "ncum%d" % (tb % 2)
                        pc_, pck = ncum[(tb + 1) % 2], "ncum%d" % ((tb + 1) % 2)
                        init = 0.0 if tb == 0 else pc_[:, 511:512]
                        b.scan(nc_[:], onesf[0:16, :], t[0:16, :], init, r=[tk, pck, "onesf"], w=[nk])
                        cc, ck = c6[tb % 2], "c6_%d" % (tb % 2)
                        r1, r1k = rot("tmpf", tmpf)
                        b.copy(cc[:, 3, :], nc_[:], r=[nk], w=[ck])
                        b.tt(r1[0:16, :], nc_[:], cc[:, 3, :], ALU.subtract, r=[nk, ck], w=[r1k])
                        b.copy(cc[:, 4, :], r1[0:16, :], r=[r1k], w=[ck])
                        b.tt(r1[0:16, :], r1[0:16, :], cc[:, 4, :], ALU.subtract, r=[r1k, ck], w=[r1k])
                        b.copy(cc[:, 5, :], r1[0:16, :], r=[r1k], w=[ck])
                        b.ts1(cc[:, 0:3, :], cc[:, 3:6, :], -1.0, ALU.mult, r=[ck], w=[ck])
                        b.dma(sC[:, :, ts_], cc[:], r=[ck], w=[("sC", tb)], eng="pool")

                    for tt_ in range(4):
                        for half in range(2):
                            pb, pk = pbank()
                            for kc in range(KC):
                                b.mm(pb[:], hb[:, kc, tt_ * 128:(tt_ + 1) * 128],
                                     win[:, kc, 2048 + half * 512:2048 + (half + 1) * 512], kc == 0, kc == KC - 1,
                                     r=["win", hk], w=[pk])
                            s, sk = rot("stb", stb)
                            b.act(s[:], pb[:], AF.Copy, r=[pk], w=[sk])
                            b.dma(sV[tb * 4 + tt_][:, half * 512:(half + 1) * 512], s[:], r=[sk],
                                  w=[("sV", tb * 4 + tt_, half)], eng="pool")
                    for h in range(8):
                        pb, pk = proj_fm(3072 + h * 128)
                        s, sk = rot("stb", stb)
                        b.act(s[:], pb[:], AF.Silu, r=[pk], w=[sk])
                        b.dma(sZ[h][:, ts_], s[:], r=[sk], w=[("sZ", h, tb)], eng="pool")
                P.flush()
            if stop_after == "A%d" % l:
                return nc

            with contextlib.ExitStack() as phOT:
                if l + 1 < n_layers:
                    w_make(l + 1)
                if is_hg:
                    with contextlib.ExitStack() as ph:
                        SEG = 256
                        NSEG = S // SEG
                        Ql = [sb(ph, "Ql%d" % i, [128, 8, SEG], BF16) for i in range(2)]
                        Kl = [sb(ph, "Kl%d" % i, [128, 8, SEG], BF16) for i in range(2)]
                        Gl = [sb(ph, "Gl%d" % i, [128, 8, SEG], F32) for i in range(2)]
                        Vl = [sb(ph, "Vl%d" % i, [128, 2, 1024], BF16) for i in range(2)]
                        Zl = [sb(ph, "Zl%d" % i, [128, 8, SEG], BF16) for i in range(2)]
                        rmask = sb(ph, "rmask", [128, SEG], F32)
                        bcum = sb(ph, "bcum", [128, 4 * SEG], F32)
                        tmpe = sb(ph, "tmpe", [128, 4 * SEG], F32)
                        ebtL = [sb(ph, "ebt%d" % i, [128, 4 * SEG], F32) for i in range(2)]
                        qdL = [sb(ph, "qd%d" % i, [128, 4 * SEG], BF16) for i in range(2)]
                        kinvL = [sb(ph, "kinv%d" % i, [128, 4 * SEG], BF16) for i in range(2)]
                        kdTL = [sb(ph, "kdT%d" % i, [128, 4 * SEG], BF16) for i in range(2)]
                        ATs = [sb(ph, "ATs%d" % i, [128, 128], BF16) for i in range(4)]
                        kdk = [sb(ph, "kdk%d" % i, [128, 128], BF16) for i in range(4)]
                        Sf = sb(ph, "Sf", [128, 8, 128], F32)
                        Sb = sb(ph, "Sb", [128, 8, 128], BF16)
                        of = [sb(ph, "of%d" % i, [128, SEG], F32) for i in range(2)]
                        osq = [sb(ph, "osq%d" % i, [128, SEG], BF16) for i in range(2)]
                        orst = [sb(ph, "orst%d" % i, [128, SEG], F32) for i in range(2)]
                        ost = [sb(ph, "ost%d" % i, [128, SEG], BF16) for i in range(4)]
                        b.memset(rmask[:], 1.0, w=["rmask"])
                        b.memset(rmask[:].rearrange("p (c t) -> p c t", t=32)[:, :, 0:1], 0.0, w=["rmask"])
                        b.memset(Sf[:], 0.0, w=["Sf"])
                        b.memset(Sb[:], 0.0, w=["Sb"])
                        rc = {"at": 0, "ats": 0, "kdk": 0, "u": 0, "tp": 0, "post": 0, "ost": 0}
                        v3 = lambda t_: t_[:].rearrange("p (h t) -> p h t", h=4)
                        c3 = lambda t_: t_[:].rearrange("p (c t) -> p c t", t=32)

                        def hg_loads(sg):
                            sl = slice(sg * SEG, (sg + 1) * SEG)
                            lk = "L%d" % (sg % 2)
                            tbk = sg * SEG // 512
                            i2 = sg % 2
                            b.dma(Gl[i2][:], sG.rearrange("h p t -> p h t")[:, :, sl],
                                  r=[("sG", h, tbk) for h in range(8)], w=[lk + "g"])
                            b.dma(Ql[i2][:], sA.rearrange("h p t -> p h t")[:, :, sl],
                                  r=[("sA", h, tbk) for h in range(8)], w=[lk + "q"])
                            b.dma(Kl[i2][:], sK.rearrange("h p t -> p h t")[:, :, sl],
                                  r=[("sK", h, tbk) for h in range(8)], w=[lk + "k"])
                            b.dma(Vl[i2][:], sV.rearrange("n p f -> p n f")[:, sg * 2:sg * 2 + 2, :],
                                  r=[("sV", sg * 2 + a, hh) for a in range(2) for hh in range(2)], w=[lk + "v"])
                            b.dma(Zl[i2][:], sZ.rearrange("h p t -> p h t")[:, :, sl],
                                  r=[("sZ", h, tbk) for h in range(8)], w=[lk + "z"])

                        def hg_prep(u):
                            sg, hg = u // 2, u % 2
                            lk = "L%d" % (sg % 2)
                            p2 = u % 2
                            hs = slice(hg * 4, hg * 4 + 4)
                            ebt, qd, kinv, kdT = ebtL[p2], qdL[p2], kinvL[p2], kdTL[p2]
                            ek, qk_, kk_, dk_ = "ebt%d" % p2, "qd%d" % p2, "kinv%d" % p2, "kdT%d" % p2
                            for hh in range(4):
                                b.scan(bcum[:, hh * SEG:(hh + 1) * SEG], rmask[:], Gl[sg % 2][:, hg * 4 + hh, :], 0.0,
                                       r=[lk + "g", "rmask"], w=["bcum"])
                            b.act(ebt[:], bcum[:], AF.Exp, r=["bcum"], w=[ek])
                            b.act(tmpe[:], bcum[:], AF.Exp, r=["bcum"], w=["tmpe"], scale=-1.0)
                            b.tt(v3(qd), Ql[sg % 2][:, hs, :], v3(ebt), ALU.mult, r=[lk + "q", ek], w=[qk_], eng="pool")
                            b.tt(v3(kinv), Kl[sg % 2][:, hs, :], v3(tmpe), ALU.mult, r=[lk + "k", "tmpe"], w=[kk_],
                                 eng="pool")
                            b.tt(c3(kdT), c3(kinv), c3(ebt)[:, :, 31:32].to_broadcast([128, 4 * SEG // 32, 32]), ALU.mult,
                                 r=[kk_, ek], w=[dk_], eng="pool")

                        def hg_steps(u):
                            sg, hg = u // 2, u % 2
                            sl = slice(sg * SEG, (sg + 1) * SEG)
                            lk = "L%d" % (sg % 2)
                            p2 = u % 2
                            v_, z_ = Vl[sg % 2], Zl[sg % 2]
                            ebt, qd, kinv, kdT = ebtL[p2], qdL[p2], kinvL[p2], kdTL[p2]
                            ek, qk_, kk_, dk_ = "ebt%d" % p2, "qd%d" % p2, "kinv%d" % p2, "kdT%d" % p2
                            eb3 = c3(ebt)
                            for tl in range(SEG // 128):
                                asis = []
                                for hh in range(4):
                                    h = hg * 4 + hh
                                    cs = slice(hh * SEG + tl * 128, hh * SEG + (tl + 1) * 128)
                                    ai = rc["at"] % 2
                                    rc["at"] += 1
                                    atp = banks[4][:, ai * 128:(ai + 1) * 128]
                                    b.mm(atp, kinv[:, cs], qd[:, cs], True, True, r=[kk_, qk_], w=["bank4"])
                                    asi = rc["ats"] % 4
                                    rc["ats"] += 1
                                    b.tt(ATs[asi][:], atp, mask01[:], ALU.mult, r=["bank4", "mask01"], w=[("ATs", asi)])
                                    ti = rc["tp"] % 8
                                    rc["tp"] += 1
                                    tpp = bankT[:, ti * 128:(ti + 1) * 128]
                                    b.tr(tpp, kdT[:, cs], ident[:], r=[dk_, "ident"], w=["bankT"])
                                    ki = rc["kdk"] % 4
                                    rc["kdk"] += 1
                                    b.act(kdk[ki][:], tpp, AF.Copy, r=["bankT"], w=[("kdk", ki)])
                                    ob = banks[hh]
                                    oc = slice(tl * 128, (tl + 1) * 128)
                                    b.mm(ob[:, oc], v_[:, tl, h * 128:(h + 1) * 128], ATs[asi][:], True, False,
                                         r=[lk + "v", ("ATs", asi)], w=["bank%d" % hh])
                                for jj in range(4):
                                    for hh in range(4):
                                        h = hg * 4 + hh
                                        ob = banks[hh]
                                        cj = hh * SEG + tl * 128 + jj * 32
                                        ki = (rc["kdk"] - 4 + hh) % 4
                                        b.mm(ob[:, tl * 128 + jj * 32: tl * 128 + jj * 32 + 32],
                                             Sb[:, h, :], qd[:, cj:cj + 32], False, jj == 3,
                                             r=[("Sb", h), qk_], w=["bank%d" % hh])
                                        ui = rc["u"] % 2
                                        rc["u"] += 1
                                        up = banks[5 + ui][:, 0:128]
                                        tkw = {"tile_position": (96, 0)} if jj == 3 else {}
                                        b.mm(up, kdk[ki][32 * jj:32 * jj + 32, :],
                                             v_[32 * jj:32 * jj + 32, tl, h * 128:(h + 1) * 128], True, True,
                                             r=[("kdk", ki), lk + "v"], w=["bank%d" % (5 + ui)], **tkw)
                                        chunk = (hh * SEG + tl * 128 + jj * 32) // 32
                                        b.stt(Sf[:, h, :], Sf[:, h, :], eb3[:, chunk, 31:32], up, ALU.mult, ALU.add,
                                              r=[("Sf", h), ek, "bank%d" % (5 + ui)], w=[("Sf", h)])
                                        b.act(Sb[:, h, :], Sf[:, h, :], AF.Copy, r=[("Sf", h)], w=[("Sb", h)])
                            for hh in range(4):
                                h = hg * 4 + hh
                                pi = rc["post"] % 2
                                rc["post"] += 1
                                ob = banks[hh]
                                b.act(osq[pi][:], ob[:, 0:SEG], AF.Square, r=["bank%d" % hh], w=[("osq", pi)])
                                b.copy(of[pi][:], ob[:, 0:SEG], r=["bank%d" % hh], w=[("of", pi)])
                                ssb = banks[4][:, 256:256 + SEG]
                                b.mm(ssb, ones_bf[:], osq[pi][:], True, True, r=[("osq", pi), "ones_bf"], w=["bank4"])
                                b.act(orst[pi][:], ssb, AF.Sqrt, r=["bank4"], w=[("orst", pi)], scale=1.0 / 128, bias=EPS)
                                b.recip(orst[pi][:], orst[pi][:], r=[("orst", pi)], w=[("orst", pi)])
                                b.stt(of[pi][:], of[pi][:], ogsb[:, j, h:h + 1], orst[pi][:], ALU.mult, ALU.mult,
                                      r=[("of", pi), ("orst", pi), "ogsb"], w=[("of", pi)])
                                oi_ = rc["ost"] % 4
                                rc["ost"] += 1
                                b.tt(ost[oi_][:], of[pi][:], z_[:, h, :], ALU.mult, r=[("of", pi), lk + "z"],
                                     w=[("ost", oi_)], eng="pool")
                                b.dma(sO[h][:, sl], ost[oi_][:], r=[("ost", oi_)], w=[("sO", h, sg)], eng="pool")

                        NU = NSEG * 2
                        hg_loads(0)
                        hg_prep(0)
                        for u in range(NU):
                            if u % 2 == 0 and u // 2 + 1 < NSEG:
                                hg_loads(u // 2 + 1)
                            if u + 1 < NU:
                                hg_prep(u + 1)
                            hg_steps(u)
                            w_tick()
                        w_drain()
                        P.flush()
                else:
                    with contextlib.ExitStack() as ph:
                        qa = [sb(ph, "qa%d" % i, [70, S], BF16) for i in range(2)]
                        ka = [sb(ph, "ka%d" % i, [70, S], BF16) for i in range(2)]
                        va = [sb(ph, "va%d" % i, [128, NT, 128], BF16) for i in range(2)]
                        zt = [sb(ph, "zt%d" % i, [64, S], BF16) for i in range(2)]
                        PT = [sb(ph, "PT%d" % i, [128, 512], BF16) for i in range(4)]
                        rl = [sb(ph, "rl%d" % i, [64, 512], F32) for i in range(2)]
                        t1 = [sb(ph, "t1%d" % i, [64, 512], F32) for i in range(2)]
                        ost = [sb(ph, "ost%d" % i, [64, 512], BF16) for i in range(2)]
                        for i in range(2):
                            b.memset(qa[i][64:70, :], 1.0, w=[("qa1", i), ("qac", i)])
                            b.memset(ka[i][64:70, :], 1.0, w=[("ka1", i), ("kac", i)])
                            b.memset(va[i][:, :, 64:128], 1.0, w=[("va1", i)])
                        rc = {"st": 0, "pt": 0, "oa": 0, "fin": 0}

                        def load_head(h):
                            hp, ho = h // 2, (h % 2) * 64
                            i = h % 2
                            allA = [("sA", hp, tb) for tb in range(NB)]
                            allK = [("sK", hp, tb) for tb in range(NB)]
                            allC = [("sC", tb) for tb in range(NB)]
                            b.dma(qa[i][0:64, :], sA[hp][ho:ho + 64, :], r=allA, w=[("qaq", i)])
                            b.dma(qa[i][64:67, :], sC[h, 0:3, :], r=allC, w=[("qac", i)])
                            b.dma(ka[i][0:64, :], sK[hp][ho:ho + 64, :], r=allK, w=[("kaq", i)])
                            b.dma(ka[i][67:70, :], sC[h, 3:6, :], r=allC, w=[("kac", i)])
                            for n0 in range(0, NT, 8):
                                n1 = min(NT, n0 + 8)
                                b.dma(va[i][:, n0:n1, 0:64],
                                      sV.rearrange("n p f -> p n f")[:, n0:n1, h * 64:(h + 1) * 64],
                                      r=[("sV", n, hh) for n in range(n0, n1) for hh in range(2)], w=[("vav", i, n0)])
                            b.dma(zt[i][:, :], sZ[hp][ho:ho + 64, :], r=[("sZ", hp, tb) for tb in range(NB)],
                                  w=["zt%d" % i])

                        steps = [(h, qs, kb) for h in range(16) for qs in range(NB) for kb in range(4 * qs + 4)]
                        info = {}
                        span_oa = {}
                        LOOK = 2

                        def emit_qk(idx):
                            h, qs, kb = steps[idx]
                            i = h % 2
                            jd = kb - 4 * qs
                            c0 = 128 * jd if jd > 0 else 0
                            si = rc["st"] % 3
                            rc["st"] += 1
                            ST = banks[si]
                            b.mm(ST[:, c0:512], ka[i][0:70, kb * 128:(kb + 1) * 128],
                                 qa[i][0:70, qs * 512 + c0:(qs + 1) * 512], True, jd < 0,
                                 r=[("kaq", i), ("kac", i), ("ka1", i), ("qaq", i), ("qac", i), ("qa1", i)],
                                 w=["bank%d" % si])
                            if jd >= 0:
                                b.mm(ST[:, c0:c0 + 128], ident[:], maskbias[:], False, True,
                                     r=["ident", "maskbias"], w=["bank%d" % si])
                            pi = rc["pt"] % 4
                            rc["pt"] += 1
                            b.act(PT[pi][:, c0:512], ST[:, c0:512], AF.Exp, r=["bank%d" % si], w=[("PT", pi)])
                            info[idx] = (c0, pi)

                        def emit_pv(idx):
                            h, qs, kb = steps[idx]
                            hp, ho = h // 2, (h % 2) * 64
                            i = h % 2
                            c0, pi = info.pop(idx)
                            nkb = 4 * qs + 4
                            if kb == 0:
                                span_oa[(h, qs)] = rc["oa"] % 2
                                rc["oa"] += 1
                            oi = span_oa[(h, qs)]
                            OA = banks[3 + oi]
                            b.mm(OA[:, c0:512], va[i][:, kb, :], PT[pi][:, c0:512], kb == 0, kb == nkb - 1,
                                 r=[("vav", i, (kb // 8) * 8), ("va1", i), ("PT", pi)], w=["bank%d" % (3 + oi)])
                            if kb == nkb - 1:
                                fi = rc["fin"] % 2
                                rc["fin"] += 1
                                b.recip(rl[fi][:], OA[64:128, :], r=["bank%d" % (3 + oi)], w=[("rl", fi)])
                                b.tt(t1[fi][:], OA[0:64, :], rl[fi][:], ALU.mult, r=["bank%d" % (3 + oi), ("rl", fi)],
                                     w=[("t1", fi)])
                                b.tt(ost[fi][:], t1[fi][:],
                                     zt[i][:, qs * 512:(qs + 1) * 512], ALU.mult, r=[("t1", fi), "zt%d" % i],
                                     w=[("ost", fi)], eng="pool")
                                b.dma(sO[hp][ho:ho + 64, qs * 512:(qs + 1) * 512], ost[fi][:], r=[("ost", fi)],
                                      w=[("sO", h, qs)], eng="pool")
                                if qs == NB - 1:
                                    w_tick()
                                    if h + 2 < 16:
                                        load_head(h + 2)

                        load_head(0)
                        load_head(1)
                        for idx in range(len(steps) + LOOK):
                            if idx < len(steps):
                                emit_qk(idx)
                            if idx - LOOK >= 0:
                                emit_pv(idx - LOOK)
                        w_drain()
                        P.flush()

                if stop_after == "B%d" % l:
                    return nc
                with contextlib.ExitStack() as ph:
                    xc = [sb(ph, "xc%d" % i, [128, KC, 512], F32) for i in range(2)]
                    otb = [sb(ph, "otb%d" % i, [128, KC, 512], BF16) for i in range(2)]
                    sO_v = sO.rearrange("h p t -> p h t")
                    nb_ = 0
                    for tb in range(NB):
                        ts_ = slice(tb * 512, (tb + 1) * 512)
                        xb, xk = xc[tb % 2], "xc%d" % (tb % 2)
                        ob_, ok_ = otb[tb % 2], "otb%d" % (tb % 2)
                        if tb == 0:
                            b.dma(ob_[:], sO_v[:, :, ts_], r=[], w=[ok_])
                            b.dma(xb[:], xsrc_v[:, :, ts_], r=[("x", tb)], w=[xk])
                        if tb + 1 < NB:
                            b.dma(otb[(tb + 1) % 2][:], sO_v[:, :, (tb + 1) * 512:(tb + 2) * 512], r=[],
                                  w=["otb%d" % ((tb + 1) % 2)])
                            b.dma(xc[(tb + 1) % 2][:], xsrc_v[:, :, (tb + 1) * 512:(tb + 2) * 512],
                                  r=[("x", tb + 1)], w=["xc%d" % ((tb + 1) % 2)])
                        for dc in range(KC):
                            bi = nb_ % 4
                            nb_ += 1
                            pb, pk = banks[bi], "bank%d" % bi
                            for fc in range(KC):
                                b.mm(pb[:], wout[:, fc, dc * 128:(dc + 1) * 128], ob_[:, fc, :], fc == 0, fc == KC - 1,
                                     r=["wout", ok_], w=[pk])
                            b.stt(xb[:, dc, :], pb[:], modsb[:, l, 16 + dc:17 + dc], xb[:, dc, :], ALU.mult, ALU.add,
                                  r=[pk, "modsb", xk], w=[xk])
                        b.dma(y_v[:, :, ts_], xb[:], r=[xk], w=[("x", tb)], eng="pool")
                    P.flush()
    return nc


def _consts():
    s = np.arange(128)[:, None]
    t = np.arange(128)[None, :]
    maskbias = np.where(s > t, NEG, 0.0).astype(np.float32)
    mask01 = ((s // 32 == t // 32) & (s <= t)).astype(np.float32)
    mj = (s // 32 == np.arange(4)[None, :]).astype(np.float32)
    blk64 = (s // 64 == t // 64).astype(np.float32)
    return dict(c_maskbias=maskbias, c_mask01=mask01, c_mj=mj, c_blk64=blk64)


def _pcol(v, n):
    return np.ascontiguousarray(np.asarray(v, np.float32).reshape(n, 128).T)


def make_in_maps(inputs, n_cores, S):
    f = lambda a: np.ascontiguousarray(np.asarray(a, dtype=np.float32))
    x = f(inputs["x"])
    c = f(inputs["c"])
    shared = dict(
        gT=np.ascontiguousarray(np.stack([_pcol(g, 8) for g in f(inputs["norm_g"])], 1)),
        ada_w=f(inputs["ada_w"]),
        ada_bT=np.ascontiguousarray(np.stack([_pcol(v, 24) for v in f(inputs["ada_b"])], 1)),
        lbT=np.ascontiguousarray(np.stack([_pcol(v, 8) for v in f(inputs["hg_lb_logits"])], 1)),
        hg_w_in=f(inputs["hg_w_in"]),
        ogT=np.ascontiguousarray(np.stack([_pcol(v, 8) for v in f(inputs["hg_o_g"])], 1)),
        hg_w_out=f(inputs["hg_w_out"]),
        fx_w_in=f(inputs["fx_w_in"]),
        bfT=np.ascontiguousarray(f(inputs["fx_b_f"]).T),
        qgT=np.ascontiguousarray(np.tile(f(inputs["fx_q_g"]).T, (2, 1))),
        kgT=np.ascontiguousarray(np.tile(f(inputs["fx_k_g"]).T, (2, 1))),
        fx_w_out=f(inputs["fx_w_out"]),
    )
    shared.update(_consts())
    maps = []
    for i in range(n_cores):
        m = dict(shared)
        m["xT"] = np.ascontiguousarray(x[i].T)
        m["cT"] = _pcol(c[i], 8)
        maps.append(m)
    return maps


_NC_CACHE = {}


def run(inputs, n_layers=4, stop_after=None):
    x = np.asarray(inputs["x"])
    Bn, S, _ = x.shape
    key = (S, n_layers, stop_after)
    if key not in _NC_CACHE:
        _NC_CACHE[key] = build(S, n_layers, stop_after)
    nc = _NC_CACHE[key]
    maps = make_in_maps(inputs, Bn, S)
    res = run_bass_kernel_spmd(nc, maps, core_ids=list(range(Bn)))
    out = np.stack([np.ascontiguousarray(res.results[i]["yT"].T) for i in range(Bn)], 0)
    return out.astype(np.float32)


def kernel(**inputs):
    return run(inputs, n_layers=4)
```

```python
import contextlib
import numpy as np
import concourse.bass as bass
import concourse.mybir as mybir
from concourse.bass_utils import run_bass_kernel_spmd

F32 = mybir.dt.float32
BF16 = mybir.dt.bfloat16
ALU = mybir.AluOpType
AF = mybir.ActivationFunctionType

ENGS = ("pe", "act", "dve", "pool", "sp")
D = 1024
KC = 8
EPS = 1e-6
SAME_ENG_SYNC = False
NEG = -30000.0


class Prog:
    def __init__(self, nc, es, n_dma_sems=40):
        self.nc = nc
        self.ops = []
        self.buf = {}
        self.n_dma_sems = n_dma_sems
        self.dma_rr = 0
        self.dma_rr_e = {}
        self.slot_ord = [0] * n_dma_sems
        self.slot_last = {}
        self.flushed = 0
        self.cnt = {e: 0 for e in ENGS}
        self.esem = {e: es.enter_context(nc.semaphore("s_" + e)) for e in ENGS}
        self.dsem = [es.enter_context(nc.semaphore("d_%d" % k)) for k in range(n_dma_sems)]
        self.nflush = 0

    def op(self, eng, fn, r=(), w=(), dma=False):
        pk_ = [k for k in r if isinstance(k, str) and k.startswith("bank")]
        if pk_:
            w = list(w) + [k for k in pk_ if k not in w]
            r = [k for k in r if k not in pk_]
        deps = set()
        for b in r:
            st = self.buf.get(b)
            if st is not None and st[0] is not None:
                deps.add(st[0])
        for b in w:
            st = self.buf.get(b)
            if st is not None:
                if st[0] is not None:
                    deps.add(st[0])
                deps.update(st[1])
        i = len(self.ops)
        o = dict(eng=eng, fn=fn, deps=deps, dma=dma, slot=None, ordinal=None, count=None)
        if dma:
            lo_, hi_ = (0, 28) if eng == "sp" else (28, self.n_dma_sems)
            k_ = self.dma_rr_e.get(eng, 0)
            self.dma_rr_e[eng] = k_ + 1
            s = lo_ + k_ % (hi_ - lo_)
            prev = self.slot_last.get(s)
            if prev is not None:
                deps.add(prev)
            self.slot_ord[s] += 1
            o["ordinal"] = self.slot_ord[s]
            o["slot"] = s
            self.slot_last[s] = i
        self.ops.append(o)
        for b in r:
            st = self.buf.setdefault(b, [None, []])
            st[1].append(i)
        for b in w:
            self.buf[b] = [i, []]
        return i

    def flush(self):
        nc = self.nc
        ops = self.ops
        lo, hi = self.flushed, len(ops)
        if lo == hi:
            return
        base_cnt = dict(self.cnt)
        base_ord = list(self.slot_ord_flushed) if hasattr(self, "slot_ord_flushed") else [0] * self.n_dma_sems
        waited = {e: {f: -1 for f in ENGS} for e in ENGS}
        waited_dma = {e: {} for e in ENGS}
        needed = {}
        for i in range(lo, hi):
            o = ops[i]
            e = o["eng"]
            wl = []
            for d in sorted(o["deps"]):
                if d < lo:
                    continue
                od = ops[d]
                if od["dma"]:
                    if waited_dma[e].get(od["slot"], 0) >= od["ordinal"]:
                        continue
                    waited_dma[e][od["slot"]] = od["ordinal"]
                    wl.append(("dma", od["slot"], od["ordinal"]))
                else:
                    f = od["eng"]
                    if f == e and (e == "pe" or (not SAME_ENG_SYNC and e != "pool")):
                        continue
                    if waited[e][f] >= d:
                        continue
                    waited[e][f] = d
                    needed[d] = True
                    wl.append(("eng", f, d))
            o["waits"] = wl
        last_of = {}
        for i in range(lo, hi):
            if not ops[i]["dma"]:
                last_of[ops[i]["eng"]] = i
        for e, i in last_of.items():
            needed[i] = True
        for i in range(lo, hi):
            o = ops[i]
            if o["dma"]:
                continue
            if needed.get(i):
                self.cnt[o["eng"]] += 1
                o["count"] = self.cnt[o["eng"]]
        per_eng = {e: [i for i in range(lo, hi) if ops[i]["eng"] == e] for e in ENGS}
        esem, dsem = self.esem, self.dsem
        first = self.nflush == 0
        self.nflush += 1
        final_waits = {e: [] for e in ENGS}
        cur_last = {}
        for i in range(lo, hi):
            if ops[i]["dma"]:
                cur_last[ops[i]["slot"]] = i
        for s, i in cur_last.items():
            final_waits[ops[i]["eng"]].append((s, ops[i]["ordinal"]))

        def replay(ename, eng):
            if not first:
                for f in ENGS:
                    if f != ename and base_cnt[f] > 0:
                        eng.wait_ge(esem[f], base_cnt[f])
            for i in per_eng[ename]:
                o = ops[i]
                for wt in o["waits"]:
                    if wt[0] == "dma":
                        eng.wait_ge(dsem[wt[1]], 16 * wt[2])
                    else:
                        eng.wait_ge(esem[wt[1]], ops[wt[2]]["count"])
                ins = o["fn"](eng)
                if o["dma"]:
                    ins.then_inc(dsem[o["slot"]], 16)
                elif needed.get(i):
                    ins.then_inc(esem[ename], 1)
                o["fn"] = None
            for s, ordn in final_waits[ename]:
                eng.wait_ge(dsem[s], 16 * ordn)

        with nc.Block() as block:
            def wrap(ename):
                def f(eng):
                    replay(ename, eng)
                    if final_waits[ename]:
                        self.cnt[ename] += 1
                        eng.nop().then_inc(esem[ename], 1)
                return f
            block.tensor(wrap("pe"))
            block.scalar(wrap("act"))
            block.vector(wrap("dve"))
            block.gpsimd(wrap("pool"))
            block.sync(wrap("sp"))
        self.flushed = hi
        self.slot_ord_flushed = list(self.slot_ord)


class B:
    def __init__(self, P):
        self.P = P

    def dma(self, out, in_, r, w, eng="sp"):
        return self.P.op(eng, lambda e: e.dma_start(out=out, in_=in_), r=r, w=w, dma=True)

    def mm(self, out, lhsT, rhs, start, stop, r, w, **kw):
        return self.P.op("pe", lambda e: e.matmul(out, lhsT=lhsT, rhs=rhs, start=start, stop=stop,
                                                  skip_group_check=True, **kw), r=r, w=w)

    def tr(self, out, in_, ident, r, w):
        return self.P.op("pe", lambda e: e.transpose(out=out, in_=in_, identity=ident), r=r, w=w)

    def act(self, out, in_, func, r, w, scale=1.0, bias=0.0):
        return self.P.op("act", lambda e: e.activation(out=out, in_=in_, func=func, scale=scale, bias=bias), r=r, w=w)

    def tt(self, out, in0, in1, op, r, w, eng="dve"):
        return self.P.op(eng, lambda e: e.tensor_tensor(out=out, in0=in0, in1=in1, op=op), r=r, w=w)

    def ts(self, out, in0, s1, s2, op0, op1, r, w, eng="dve"):
        return self.P.op(eng, lambda e: e.tensor_scalar(out=out, in0=in0, scalar1=s1, scalar2=s2, op0=op0, op1=op1),
                         r=r, w=w)

    def ts1(self, out, in0, s1, op0, r, w, eng="dve"):
        return self.P.op(eng, lambda e: e.tensor_single_scalar(out=out, in_=in0, scalar=s1, op=op0), r=r, w=w)

    def stt(self, out, in0, scalar, in1, op0, op1, r, w, eng="dve"):
        return self.P.op(eng, lambda e: e.scalar_tensor_tensor(out=out, in0=in0, scalar=scalar, in1=in1,
                                                               op0=op0, op1=op1), r=r, w=w)

    def copy(self, out, in_, r, w, eng="dve"):
        return self.P.op(eng, lambda e: e.tensor_copy(out=out, in_=in_), r=r, w=w)

    def recip(self, out, in_, r, w):
        return self.P.op("dve", lambda e: e.reciprocal(out=out, in_=in_), r=r, w=w)

    def memset(self, ap, val, w, eng="pool"):
        return self.P.op(eng, lambda e: e.memset(ap, val), r=(), w=w)

    def scan(self, out, d0, d1, initial, r, w):
        return self.P.op("dve", lambda e: e.tensor_tensor_scan(out=out, data0=d0, data1=d1, initial=initial,
                                                               op0=ALU.mult, op1=ALU.add), r=r, w=w)


def build(S, n_layers, stop_after=None):
    NB = S // 512
    NT = S // 128
    nc = bass.Bass("TRN2", target_bir_lowering=False)

    def din(name, shape, dt=F32):
        return nc.dram_tensor(name, list(shape), dt, kind="ExternalInput").ap()

    def dscr(name, shape, dt):
        return nc.dram_tensor(name, list(shape), dt, kind="Internal").ap()

    xT = din("xT", [D, S])
    cT = din("cT", [128, KC])
    gT = din("gT", [128, 4, KC])
    ada_w = din("ada_w", [4, D, 3 * D])
    ada_bT = din("ada_bT", [128, 4, 24])
    lbT = din("lbT", [128, 2, 8])
    hg_w_in = din("hg_w_in", [2, D, 4096])
    ogT = din("ogT", [128, 2, 8])
    hg_w_out = din("hg_w_out", [2, D, D])
    fx_w_in = din("fx_w_in", [2, D, 4112])
    bfT = din("bfT", [16, 2])
    qgT = din("qgT", [128, 2])
    kgT = din("kgT", [128, 2])
    fx_w_out = din("fx_w_out", [2, D, D])
    c_maskbias = din("c_maskbias", [128, 128])
    c_mask01 = din("c_mask01", [128, 128])
    c_mj = din("c_mj", [128, 4])
    c_blk64 = din("c_blk64", [128, 128])
    yT = nc.dram_tensor("yT", [D, S], F32, kind="ExternalOutput").ap()

    sA = dscr("sA", [8, 128, S], BF16)
    sK = dscr("sK", [8, 128, S], BF16)
    sG = dscr("sG", [8, 128, S], F32)
    sV = dscr("sV", [NT, 128, 1024], BF16)
    sZ = dscr("sZ", [8, 128, S], BF16)
    sC = dscr("sC", [16, 6, S], BF16)

    with contextlib.ExitStack() as es:
        P = Prog(nc, es)
        b = B(P)

        uid = [0]

        def sb(stack, name, shape, dt):
            uid[0] += 1
            return stack.enter_context(nc.sbuf_tensor("%s_u%d" % (name, uid[0]), list(shape), dt))

        banks = [es.enter_context(nc.psum_tensor("bank%d" % i, [128, 512], F32)) for i in range(7)]
        bankT = es.enter_context(nc.psum_tensor("bankT", [128, 1024], BF16))
        ident = sb(es, "ident", [128, 128], BF16)
        ones_bf = sb(es, "ones_bf", [128, 128], BF16)
        blk64 = sb(es, "blk64", [128, 128], BF16)
        maskbias = sb(es, "maskbias", [128, 128], BF16)
        mask01 = sb(es, "mask01", [128, 128], F32)
        mj = sb(es, "mj", [128, 4], F32)
        ctmp = sb(es, "ctmp", [128, 128], F32)
        cact = sb(es, "cact", [128, KC], F32)
        gsb = sb(es, "gsb", [128, 4, KC], F32)
        modsb = sb(es, "modsb", [128, 4, 24], F32)
        asb = sb(es, "asb", [128, 4, KC], F32)
        lbsb = sb(es, "lbsb", [128, 2, 8], F32)
        omlsb = sb(es, "omlsb", [128, 2, 8], F32)
        ogsb = sb(es, "ogsb", [128, 2, 8], F32)
        nbf = sb(es, "nbf", [16, 2], F32)
        qg = sb(es, "qg", [128, 2], F32)
        kg = sb(es, "kg", [128, 2], F32)
        wout = sb(es, "wout", [128, KC, D], BF16)
        onesf = sb(es, "onesf", [128, 512], F32)
        win = sb(es, "win", [128, KC, 4112], BF16)
        wst = [sb(es, "wst%d" % i, [128, 2048], F32) for i in range(2)]
        sO = dscr("sO", [8, 128, S], BF16)
        wq = {"steps": [], "k": 0}

        def w_make(l_):
            is_hg_ = (l_ % 2 == 0)
            w_ = (hg_w_in[l_ // 2] if is_hg_ else fx_w_in[l_ // 2]).rearrange("(kc p) f -> p kc f", p=128)
            ncol = 4096 if is_hg_ else 4112
            for kc in range(KC):
                for c0 in range(0, ncol, 2048):
                    wq["steps"].append((w_, kc, c0, min(ncol, c0 + 2048)))

        def w_tick(n=1):
            for _ in range(n):
                pend = wq.get("pend")
                if pend is not None:
                    kc, c0, c1, st, key = pend
                    b.copy(win[:, kc, c0:c1], st[:, 0:c1 - c0], r=[key], w=["win"], eng="pool")
                    wq["pend"] = None
                if not wq["steps"]:
                    continue
                w_, kc, c0, c1 = wq["steps"].pop(0)
                k_ = wq["k"]
                wq["k"] += 1
                st, key = wst[k_ % 2], "wst%d" % (k_ % 2)
                b.dma(st[:, 0:c1 - c0], w_[:, kc, c0:c1], r=[], w=[key])
                wq["pend"] = (kc, c0, c1, st, key)

        def w_drain():
            while wq["steps"] or wq.get("pend") is not None:
                w_tick()

        with contextlib.ExitStack() as ph:
            adast = [sb(ph, "adast%d" % i, [128, KC, 512], F32) for i in range(2)]
            b.memset(ident[:], 0.0, w=["ident"])
            P.op("pool", lambda e: e.affine_select(out=ident[:], in_=ident[:], pattern=[[-1, 128]],
                                                   compare_op=ALU.not_equal, fill=1.0, base=0,
                                                   channel_multiplier=1), r=["ident"], w=["ident"])
            b.memset(ones_bf[:], 1.0, w=["ones_bf"])
            b.memset(onesf[:], 1.0, w=["onesf"])
            for (src, dst, nm) in ((c_maskbias, maskbias, "maskbias"), (c_blk64, blk64, "blk64")):
                b.dma(ctmp[:], src, r=[], w=["ctmp"])
                b.copy(dst[:], ctmp[:], r=["ctmp"], w=[nm])
            b.dma(mask01[:], c_mask01, r=[], w=["mask01"])
            b.dma(mj[:], c_mj, r=[], w=["mj"])
            b.dma(cact[:], cT, r=[], w=["cact"])
            b.dma(gsb[:], gT, r=[], w=["gsb"])
            b.dma(modsb[:], ada_bT, r=[], w=["modsb"])
            b.dma(lbsb[:], lbT, r=[], w=["lbsb"])
            b.dma(ogsb[:], ogT, r=[], w=["ogsb"])
            b.dma(nbf[:], bfT, r=[], w=["nbf"])
            b.dma(qg[:], qgT, r=[], w=["qg"])
            b.dma(kg[:], kgT, r=[], w=["kg"])
            b.act(cact[:], cact[:], AF.Silu, r=["cact"], w=["cact"])
            b.tt(lbsb[:, 1, :], lbsb[:, 1, :], lbsb[:, 0, :], ALU.subtract, r=["lbsb"], w=["lbsb"])
            b.act(lbsb[:, 1, :], lbsb[:, 1, :], AF.Sigmoid, r=["lbsb"], w=["lbsb"])
            b.memset(lbsb[:, 0, :], 0.0, w=["lbsb"], eng="dve")
            b.ts(omlsb[:], lbsb[:], -1.0, 1.0, ALU.mult, ALU.add, r=["lbsb"], w=["omlsb"])
            b.ts1(nbf[:], nbf[:], -1.0, ALU.mult, r=["nbf"], w=["nbf"])
            b.ts1(qg[:], qg[:], 0.125, ALU.mult, r=["qg"], w=["qg"])
            modps = banks[0]
            k = 0
            for l in range(n_layers):
                for jb in range(6):
                    st = adast[k % 2]
                    key = "adast%d" % (k % 2)
                    k += 1
                    b.dma(st[:], ada_w[l].rearrange("(kc p) j -> p kc j", p=128)[:, :, jb * 512:(jb + 1) * 512],
                          r=[], w=[key])
                    for jj in range(4):
                        col = l * 24 + jb * 4 + jj
                        for kc in range(KC):
                            b.mm(modps[:, col:col + 1], st[:, kc, jj * 128:(jj + 1) * 128], cact[:, kc:kc + 1],
                                 kc == 0, kc == KC - 1, r=[key, "cact"], w=["bank0"])
            nl = n_layers
            b.tt(modsb[:, 0:nl, :], modsb[:, 0:nl, :], modps[:, 0:nl * 24].rearrange("p (l j) -> p l j", j=24),
                 ALU.add, r=["bank0", "modsb"], w=["modsb"])
            b.stt(asb[:, 0:nl, :], modsb[:, 0:nl, 8:16], 1.0, gsb[:, 0:nl, :], ALU.add, ALU.mult,
                  r=["modsb", "gsb"], w=["asb"])
            P.flush()
        if stop_after == "p0":
            return nc

        for l in range(n_layers):
            is_hg = (l % 2 == 0)
            j = l // 2
            xsrc = xT if l == 0 else yT
            w_in = hg_w_in[j] if is_hg else fx_w_in[j]
            w_out = hg_w_out[j] if is_hg else fx_w_out[j]
            NCOL = 4096 if is_hg else 4112
            xsrc_v = xsrc.rearrange("(kc p) t -> p kc t", p=128)
            y_v = yT.rearrange("(kc p) t -> p kc t", p=128)

            with contextlib.ExitStack() as ph:
                xblk = [sb(ph, "xblk%d" % i, [128, KC, 512], F32) for i in range(1)]
                hT = [sb(ph, "hT%d" % i, [128, KC, 512], BF16) for i in range(2)]
                sq = [sb(ph, "sq%d" % i, [128, 512], BF16) for i in range(2)]
                rstd = sb(ph, "rstd", [128, 512], F32)
                tmpf = [sb(ph, "tmpf%d" % i, [128, 512], F32) for i in range(4)]
                stb = [sb(ph, "stb%d" % i, [128, 512], BF16) for i in range(6)]
                stf = [sb(ph, "stf%d" % i, [128, 512], F32) for i in range(3)]
                ncum = [sb(ph, "ncum%d" % i, [16, 512], F32) for i in range(2)]
                c6 = [sb(ph, "c6_%d" % i, [16, 6, 512], BF16) for i in range(2)]
                cnt = {"tmpf": 0, "stb": 0, "stf": 0, "bank": 0, "wst": 0, "ss2": 0}

                def rot(name, lst):
                    i = cnt[name] % len(lst)
                    cnt[name] += 1
                    return lst[i], "%s%d" % (name, i)

                pb_list = [1, 2, 3, 4, 5, 6] if is_hg else [1, 2, 3, 4]

                def pbank():
                    i = pb_list[cnt["bank"] % len(pb_list)]
                    cnt["bank"] += 1
                    return banks[i], "bank%d" % i

                if l == 0:
                    w_make(0)
                w_drain()
                w_out_v = w_out.rearrange("(kc p) f -> p kc f", p=128)
                for kc in range(0, KC, 2):
                    k_ = wq["k"]
                    wq["k"] += 1
                    st, key = wst[k_ % 2], "wst%d" % (k_ % 2)
                    b.dma(st[:].rearrange("p (a f) -> p a f", a=2), w_out_v[:, kc:kc + 2, :], r=[], w=[key])
                    b.copy(wout[:, kc:kc + 2, :], st[:].rearrange("p (a f) -> p a f", a=2), r=[key], w=["wout"],
                           eng="pool")

                def norm(tb):
                    xb, xk = xblk[0], "xblk0"
                    hb, hk = hT[tb % 2], "hT%d" % (tb % 2)
                    if tb == 0:
                        b.dma(xb[:], xsrc_v[:, :, 0:512], r=[("x", 0)], w=[xk])
                    ssp = banks[0]
                    for kc in range(KC):
                        b.act(sq[kc % 2][:], xb[:, kc, :], AF.Square, r=[xk], w=["sq%d" % (kc % 2)])
                        b.mm(ssp[:], ones_bf[:], sq[kc % 2][:], kc == 0, kc == KC - 1,
                             r=["sq%d" % (kc % 2), "ones_bf"], w=["bank0"])
                    b.act(rstd[:], ssp[:], AF.Sqrt, r=["bank0"], w=["rstd"], scale=1.0 / D, bias=EPS)
                    b.recip(rstd[:], rstd[:], r=["rstd"], w=["rstd"])
                    for kc in range(KC):
                        t, tk = rot("tmpf", tmpf)
                        b.stt(t[:], xb[:, kc, :], asb[:, l, kc:kc + 1], rstd[:], ALU.mult, ALU.mult,
                              r=[xk, "asb", "rstd"], w=[tk])
                        b.act(hb[:, kc, :], t[:], AF.Identity, r=[tk, "modsb"], w=[hk],
                              bias=modsb[:, l, kc:kc + 1])
                    if tb + 1 < NB:
                        b.dma(xb[:], xsrc_v[:, :, (tb + 1) * 512:(tb + 2) * 512], r=[("x", tb + 1)], w=[xk])

                norm(0)
                for tb in range(NB):
                    ts_ = slice(tb * 512, (tb + 1) * 512)
                    hb, hk = hT[tb % 2], "hT%d" % (tb % 2)

                    def proj_fm(c0, M=128):
                        pb, pk = pbank()
                        for kc in range(KC):
                            b.mm(pb[0:M, :], win[:, kc, c0:c0 + M], hb[:, kc, :], kc == 0, kc == KC - 1,
                                 r=["win", hk], w=[pk])
                        return pb, pk

                    if is_hg:
                        for h in range(8):
                            pb, pk = proj_fm(h * 128)
                            s, sk = rot("stb", stb)
                            b.act(s[:], pb[:], AF.Copy, r=[pk], w=[sk])
                            b.dma(sA[h][:, ts_], s[:], r=[sk], w=[("sA", h, tb)])
                            pb, pk = proj_fm(1024 + h * 128)
                            t, tk = rot("tmpf", tmpf)
                            b.act(t[:], pb[:], AF.Sigmoid, r=[pk], w=[tk])
                            b.ts(t[:], t[:], omlsb[:, j, h:h + 1], lbsb[:, j, h:h + 1], ALU.mult, ALU.add,
                                 r=[tk, "omlsb", "lbsb"], w=[tk])
                            g_, gk = rot("stf", stf)
                            b.act(g_[:], t[:], AF.Ln, r=[tk], w=[gk])
                            b.dma(sG[h][:, ts_], g_[:], r=[gk], w=[("sG", h, tb)])
                            s, sk = rot("stb", stb)
                            b.ts(s[:], t[:], -1.0, 1.0, ALU.mult, ALU.add, r=[tk], w=[sk])
                            b.dma(sK[h][:, ts_], s[:], r=[sk], w=[("sK", h, tb)])
                    else:
                        for hp in range(8):
                            for (c0, dst, gv, nm) in ((hp * 128, sA, qg, "sA"), (1024 + hp * 128, sK, kg, "sK")):
                                pb, pk = proj_fm(c0)
                                s2, s2k = rot("stb", stb)
                                b.act(s2[:], pb[:], AF.Square, r=[pk], w=[s2k])
                                sbi = 5 + cnt["ss2"] % 2
                                cnt["ss2"] += 1
                                b.mm(banks[sbi][:], blk64[:], s2[:], True, True, r=[s2k, "blk64"], w=["bank%d" % sbi])
                                r2, r2k = rot("tmpf", tmpf)
                                b.act(r2[:], banks[sbi][:], AF.Sqrt, r=["bank%d" % sbi], w=[r2k], scale=1.0 / 64, bias=EPS)
                                b.recip(r2[:], r2[:], r=[r2k], w=[r2k])
                                s, sk = rot("stb", stb)
                                b.stt(s[:], pb[:], gv[:, j:j + 1], r2[:], ALU.mult, ALU.mult,
                                      r=[pk, r2k, "qg", "kg"], w=[sk])
                                b.dma(dst[hp][:, ts_], s[:], r=[sk], w=[(nm, hp, tb)])
                        pb, pk = proj_fm(4096, M=16)
                        t, tk = rot("tmpf", tmpf)
                        b.act(t[0:16, :], pb[0:16, :], AF.Exp, r=[pk, "nbf"], w=[tk], scale=-1.0, bias=nbf[:, j:j + 1])
                        b.act(t[0:16, :], t[0:16, :], AF.Ln, r=[tk], w=[tk], scale=1.0, bias=1.0)
                        nc_, nk = ncum[tb % 2], "ncum%d" % (tb % 2)
                        pc_, pck = ncum[(tb + 1) % 2], "ncum%d" % ((tb + 1) % 2)
                        init = 0.0 if tb == 0 else pc_[:, 511:512]
                        b.scan(nc_[:], onesf[0:16, :], t[0:16, :], init, r=[tk, pck, "onesf"], w=[nk])
                        cc, ck = c6[tb % 2], "c6_%d" % (tb % 2)
                        r1, r1k = rot("tmpf", tmpf)
                        b.copy(cc[:, 3, :], nc_[:], r=[nk], w=[ck])
                        b.tt(r1[0:16, :], nc_[:], cc[:, 3, :], ALU.subtract, r=[nk, ck], w=[r1k])
                        b.copy(cc[:, 4, :], r1[0:16, :], r=[r1k], w=[ck])
                        b.tt(r1[0:16, :], r1[0:16, :], cc[:, 4, :], ALU.subtract, r=[r1k, ck], w=[r1k])
                        b.copy(cc[:, 5, :], r1[0:16, :], r=[r1k], w=[ck])
                        b.ts1(cc[:, 0:3, :], cc[:, 3:6, :], -1.0, ALU.mult, r=[ck], w=[ck])
                        b.dma(sC[:, :, ts_], cc[:], r=[ck], w=[("sC", tb)])

                    if tb + 1 < NB:
                        norm(tb + 1)
                    for tt_ in range(4):
                        for half in range(2):
                            pb, pk = pbank()
                            for kc in range(KC):
                                b.mm(pb[:], hb[:, kc, tt_ * 128:(tt_ + 1) * 128],
                                     win[:, kc, 2048 + half * 512:2048 + (half + 1) * 512], kc == 0, kc == KC - 1,
                                     r=["win", hk], w=[pk])
                            s, sk = rot("stb", stb)
                            b.act(s[:], pb[:], AF.Copy, r=[pk], w=[sk])
                            b.dma(sV[tb * 4 + tt_][:, half * 512:(half + 1) * 512], s[:], r=[sk],
                                  w=[("sV", tb * 4 + tt_, half)])
                    for h in range(8):
                        pb, pk = proj_fm(3072 + h * 128)
                        s, sk = rot("stb", stb)
                        b.act(s[:], pb[:], AF.Silu, r=[pk], w=[sk])
                        b.dma(sZ[h][:, ts_], s[:], r=[sk], w=[("sZ", h, tb)])
                P.flush()
            if stop_after == "A%d" % l:
                return nc

            with contextlib.ExitStack() as phOT:
                if l + 1 < n_layers:
                    w_make(l + 1)
                if is_hg:
                    with contextlib.ExitStack() as ph:
                        SEG = 256
                        NSEG = S // SEG
                        Ql = [sb(ph, "Ql%d" % i, [128, 8, SEG], BF16) for i in range(2)]
                        Kl = [sb(ph, "Kl%d" % i, [128, 8, SEG], BF16) for i in range(2)]
                        Gl = [sb(ph, "Gl%d" % i, [128, 8, SEG], F32) for i in range(2)]
                        Vl = [sb(ph, "Vl%d" % i, [128, 2, 1024], BF16) for i in range(2)]
                        Zl = [sb(ph, "Zl%d" % i, [128, 8, SEG], BF16) for i in range(2)]
                        rmask = sb(ph, "rmask", [128, SEG], F32)
                        bcum = sb(ph, "bcum", [128, 4 * SEG], F32)
                        tmpe = sb(ph, "tmpe", [128, 4 * SEG], F32)
                        ebtL = [sb(ph, "ebt%d" % i, [128, 4 * SEG], F32) for i in range(2)]
                        qdL = [sb(ph, "qd%d" % i, [128, 4 * SEG], BF16) for i in range(2)]
                        kinvL = [sb(ph, "kinv%d" % i, [128, 4 * SEG], BF16) for i in range(2)]
                        kdTL = [sb(ph, "kdT%d" % i, [128, 4 * SEG], BF16) for i in range(2)]
                        ATs = [sb(ph, "ATs%d" % i, [128, 128], BF16) for i in range(4)]
                        kdk = [sb(ph, "kdk%d" % i, [128, 128], BF16) for i in range(4)]
                        Sf = sb(ph, "Sf", [128, 8, 128], F32)
                        Sb = sb(ph, "Sb", [128, 8, 128], BF16)
                        of = [sb(ph, "of%d" % i, [128, SEG], F32) for i in range(2)]
                        osq = [sb(ph, "osq%d" % i, [128, SEG], BF16) for i in range(2)]
                        orst = [sb(ph, "orst%d" % i, [128, SEG], F32) for i in range(2)]
                        ost = [sb(ph, "ost%d" % i, [128, SEG], BF16) for i in range(4)]
                        b.memset(rmask[:], 1.0, w=["rmask"])
                        b.memset(rmask[:].rearrange("p (c t) -> p c t", t=32)[:, :, 0:1], 0.0, w=["rmask"])
                        b.memset(Sf[:], 0.0, w=["Sf"])
                        b.memset(Sb[:], 0.0, w=["Sb"])
                        rc = {"at": 0, "ats": 0, "kdk": 0, "u": 0, "tp": 0, "post": 0, "ost": 0}
                        v3 = lambda t_: t_[:].rearrange("p (h t) -> p h t", h=4)
                        c3 = lambda t_: t_[:].rearrange("p (c t) -> p c t", t=32)

                        def hg_loads(sg):
                            sl = slice(sg * SEG, (sg + 1) * SEG)
                            lk = "L%d" % (sg % 2)
                            tbk = sg * SEG // 512
                            i2 = sg % 2
                            b.dma(Gl[i2][:], sG.rearrange("h p t -> p h t")[:, :, sl],
                                  r=[("sG", h, tbk) for h in range(8)], w=[lk + "g"])
                            b.dma(Ql[i2][:], sA.rearrange("h p t -> p h t")[:, :, sl],
                                  r=[("sA", h, tbk) for h in range(8)], w=[lk + "q"])
                            b.dma(Kl[i2][:], sK.rearrange("h p t -> p h t")[:, :, sl],
                                  r=[("sK", h, tbk) for h in range(8)], w=[lk + "k"])
                            b.dma(Vl[i2][:], sV.rearrange("n p f -> p n f")[:, sg * 2:sg * 2 + 2, :],
                                  r=[("sV", sg * 2 + a, hh) for a in range(2) for hh in range(2)], w=[lk + "v"])
                            b.dma(Zl[i2][:], sZ.rearrange("h p t -> p h t")[:, :, sl],
                                  r=[("sZ", h, tbk) for h in range(8)], w=[lk + "z"])

                        def hg_prep(u):
                            sg, hg = u // 2, u % 2
                            lk = "L%d" % (sg % 2)
                            p2 = u % 2
                            hs = slice(hg * 4, hg * 4 + 4)
                            ebt, qd, kinv, kdT = ebtL[p2], qdL[p2], kinvL[p2], kdTL[p2]
                            ek, qk_, kk_, dk_ = "ebt%d" % p2, "qd%d" % p2, "kinv%d" % p2, "kdT%d" % p2
                            for hh in range(4):
                                b.scan(bcum[:, hh * SEG:(hh + 1) * SEG], rmask[:], Gl[sg % 2][:, hg * 4 + hh, :], 0.0,
                                       r=[lk + "g", "rmask"], w=["bcum"])
                            b.act(ebt[:], bcum[:], AF.Exp, r=["bcum"], w=[ek])
                            b.act(tmpe[:], bcum[:], AF.Exp, r=["bcum"], w=["tmpe"], scale=-1.0)
                            b.tt(v3(qd), Ql[sg % 2][:, hs, :], v3(ebt), ALU.mult, r=[lk + "q", ek], w=[qk_], eng="pool")
                            b.tt(v3(kinv), Kl[sg % 2][:, hs, :], v3(tmpe), ALU.mult, r=[lk + "k", "tmpe"], w=[kk_],
                                 eng="pool")
                            b.tt(c3(kdT), c3(kinv), c3(ebt)[:, :, 31:32].to_broadcast([128, 4 * SEG // 32, 32]), ALU.mult,
                                 r=[kk_, ek], w=[dk_], eng="pool")

                        def hg_steps(u):
                            sg, hg = u // 2, u % 2
                            sl = slice(sg * SEG, (sg + 1) * SEG)
                            lk = "L%d" % (sg % 2)
                            p2 = u % 2
                            v_, z_ = Vl[sg % 2], Zl[sg % 2]
                            ebt, qd, kinv, kdT = ebtL[p2], qdL[p2], kinvL[p2], kdTL[p2]
                            ek, qk_, kk_, dk_ = "ebt%d" % p2, "qd%d" % p2, "kinv%d" % p2, "kdT%d" % p2
                            eb3 = c3(ebt)
                            for tl in range(SEG // 128):
                                asis = []
                                for hh in range(4):
                                    h = hg * 4 + hh
                                    cs = slice(hh * SEG + tl * 128, hh * SEG + (tl + 1) * 128)
                                    ai = rc["at"] % 2
                                    rc["at"] += 1
                                    atp = banks[4][:, ai * 128:(ai + 1) * 128]
                                    b.mm(atp, kinv[:, cs], qd[:, cs], True, True, r=[kk_, qk_], w=["bank4"])
                                    asi = rc["ats"] % 4
                                    rc["ats"] += 1
                                    b.tt(ATs[asi][:], atp, mask01[:], ALU.mult, r=["bank4", "mask01"], w=[("ATs", asi)])
                                    ti = rc["tp"] % 8
                                    rc["tp"] += 1
                                    tpp = bankT[:, ti * 128:(ti + 1) * 128]
                                    b.tr(tpp, kdT[:, cs], ident[:], r=[dk_, "ident"], w=["bankT"])
                                    ki = rc["kdk"] % 4
                                    rc["kdk"] += 1
                                    b.act(kdk[ki][:], tpp, AF.Copy, r=["bankT"], w=[("kdk", ki)])
                                    ob = banks[hh]
                                    oc = slice(tl * 128, (tl + 1) * 128)
                                    b.mm(ob[:, oc], v_[:, tl, h * 128:(h + 1) * 128], ATs[asi][:], True, False,
                                         r=[lk + "v", ("ATs", asi)], w=["bank%d" % hh])
                                for jj in range(4):
                                    for hh in range(4):
                                        h = hg * 4 + hh
                                        ob = banks[hh]
                                        cj = hh * SEG + tl * 128 + jj * 32
                                        ki = (rc["kdk"] - 4 + hh) % 4
                                        b.mm(ob[:, tl * 128 + jj * 32: tl * 128 + jj * 32 + 32],
                                             Sb[:, h, :], qd[:, cj:cj + 32], False, jj == 3,
                                             r=[("Sb", h), qk_], w=["bank%d" % hh])
                                        ui = rc["u"] % 2
                                        rc["u"] += 1
                                        up = banks[5 + ui][:, 0:128]
                                        tkw = {"tile_position": (96, 0)} if jj == 3 else {}
                                        b.mm(up, kdk[ki][32 * jj:32 * jj + 32, :],
                                             v_[32 * jj:32 * jj + 32, tl, h * 128:(h + 1) * 128], True, True,
                                             r=[("kdk", ki), lk + "v"], w=["bank%d" % (5 + ui)], **tkw)
                                        chunk = (hh * SEG + tl * 128 + jj * 32) // 32
                                        b.stt(Sf[:, h, :], Sf[:, h, :], eb3[:, chunk, 31:32], up, ALU.mult, ALU.add,
                                              r=[("Sf", h), ek, "bank%d" % (5 + ui)], w=[("Sf", h)])
                                        b.act(Sb[:, h, :], Sf[:, h, :], AF.Copy, r=[("Sf", h)], w=[("Sb", h)])
                            for hh in range(4):
                                h = hg * 4 + hh
                                pi = rc["post"] % 2
                                rc["post"] += 1
                                ob = banks[hh]
                                b.act(osq[pi][:], ob[:, 0:SEG], AF.Square, r=["bank%d" % hh], w=[("osq", pi)])
                                b.copy(of[pi][:], ob[:, 0:SEG], r=["bank%d" % hh], w=[("of", pi)])
                                ssb = banks[4][:, 256:256 + SEG]
                                b.mm(ssb, ones_bf[:], osq[pi][:], True, True, r=[("osq", pi), "ones_bf"], w=["bank4"])
                                b.act(orst[pi][:], ssb, AF.Sqrt, r=["bank4"], w=[("orst", pi)], scale=1.0 / 128, bias=EPS)
                                b.recip(orst[pi][:], orst[pi][:], r=[("orst", pi)], w=[("orst", pi)])
                                b.stt(of[pi][:], of[pi][:], ogsb[:, j, h:h + 1], orst[pi][:], ALU.mult, ALU.mult,
                                      r=[("of", pi), ("orst", pi), "ogsb"], w=[("of", pi)])
                                oi_ = rc["ost"] % 4
                                rc["ost"] += 1
                                b.tt(ost[oi_][:], of[pi][:], z_[:, h, :], ALU.mult, r=[("of", pi), lk + "z"],
                                     w=[("ost", oi_)], eng="pool")
                                b.dma(sO[h][:, sl], ost[oi_][:], r=[("ost", oi_)], w=[("sO", h, sg)], eng="pool")

                        NU = NSEG * 2
                        hg_loads(0)
                        hg_prep(0)
                        for u in range(NU):
                            if u % 2 == 0 and u // 2 + 1 < NSEG:
                                hg_loads(u // 2 + 1)
                            if u + 1 < NU:
                                hg_prep(u + 1)
                            hg_steps(u)
                            w_tick()
                        w_drain()
                        P.flush()
                else:
                    with contextlib.ExitStack() as ph:
                        qa = [sb(ph, "qa%d" % i, [70, S], BF16) for i in range(2)]
                        ka = [sb(ph, "ka%d" % i, [70, S], BF16) for i in range(2)]
                        va = [sb(ph, "va%d" % i, [128, NT, 128], BF16) for i in range(2)]
                        zt = [sb(ph, "zt%d" % i, [64, S], BF16) for i in range(2)]
                        PT = [sb(ph, "PT%d" % i, [128, 512], BF16) for i in range(4)]
                        rl = [sb(ph, "rl%d" % i, [64, 512], F32) for i in range(2)]
                        t1 = [sb(ph, "t1%d" % i, [64, 512], F32) for i in range(2)]
                        ost = [sb(ph, "ost%d" % i, [64, 512], BF16) for i in range(2)]
                        for i in range(2):
                            b.memset(qa[i][64:70, :], 1.0, w=[("qa1", i), ("qac", i)])
                            b.memset(ka[i][64:70, :], 1.0, w=[("ka1", i), ("kac", i)])
                            b.memset(va[i][:, :, 64:128], 1.0, w=[("va1", i)])
                        rc = {"st": 0, "pt": 0, "oa": 0, "fin": 0}

                        def load_head(h):
                            hp, ho = h // 2, (h % 2) * 64
                            i = h % 2
                            allA = [("sA", hp, tb) for tb in range(NB)]
                            allK = [("sK", hp, tb) for tb in range(NB)]
                            allC = [("sC", tb) for tb in range(NB)]
                            b.dma(qa[i][0:64, :], sA[hp][ho:ho + 64, :], r=allA, w=[("qaq", i)])
                            b.dma(qa[i][64:67, :], sC[h, 0:3, :], r=allC, w=[("qac", i)])
                            b.dma(ka[i][0:64, :], sK[hp][ho:ho + 64, :], r=allK, w=[("kaq", i)])
                            b.dma(ka[i][67:70, :], sC[h, 3:6, :], r=allC, w=[("kac", i)])
                            for n0 in range(0, NT, 8):
                                n1 = min(NT, n0 + 8)
                                b.dma(va[i][:, n0:n1, 0:64],
                                      sV.rearrange("n p f -> p n f")[:, n0:n1, h * 64:(h + 1) * 64],
                                      r=[("sV", n, hh) for n in range(n0, n1) for hh in range(2)], w=[("vav", i, n0)])
                            b.dma(zt[i][:, :], sZ[hp][ho:ho + 64, :], r=[("sZ", hp, tb) for tb in range(NB)],
                                  w=["zt%d" % i])

                        steps = [(h, qs, kb) for h in range(16) for qs in range(NB) for kb in range(4 * qs + 4)]
                        info = {}
                        span_oa = {}
                        LOOK = 2

                        def emit_qk(idx):
                            h, qs, kb = steps[idx]
                            i = h % 2
                            jd = kb - 4 * qs
                            c0 = 128 * jd if jd > 0 else 0
                            si = rc["st"] % 3
                            rc["st"] += 1
                            ST = banks[si]
                            b.mm(ST[:, c0:512], ka[i][0:70, kb * 128:(kb + 1) * 128],
                                 qa[i][0:70, qs * 512 + c0:(qs + 1) * 512], True, jd < 0,
                                 r=[("kaq", i), ("kac", i), ("ka1", i), ("qaq", i), ("qac", i), ("qa1", i)],
                                 w=["bank%d" % si])
                            if jd >= 0:
                                b.mm(ST[:, c0:c0 + 128], ident[:], maskbias[:], False, True,
                                     r=["ident", "maskbias"], w=["bank%d" % si])
                            pi = rc["pt"] % 4
                            rc["pt"] += 1
                            b.act(PT[pi][:, c0:512], ST[:, c0:512], AF.Exp, r=["bank%d" % si], w=[("PT", pi)])
                            info[idx] = (c0, pi)

                        def emit_pv(idx):
                            h, qs, kb = steps[idx]
                            hp, ho = h // 2, (h % 2) * 64
                            i = h % 2
                            c0, pi = info.pop(idx)
                            nkb = 4 * qs + 4
                            if kb == 0:
                                span_oa[(h, qs)] = rc["oa"] % 2
                                rc["oa"] += 1
                            oi = span_oa[(h, qs)]
                            OA = banks[3 + oi]
                            b.mm(OA[:, c0:512], va[i][:, kb, :], PT[pi][:, c0:512], kb == 0, kb == nkb - 1,
                                 r=[("vav", i, (kb // 8) * 8), ("va1", i), ("PT", pi)], w=["bank%d" % (3 + oi)])
                            if kb == nkb - 1:
                                fi = rc["fin"] % 2
                                rc["fin"] += 1
                                b.recip(rl[fi][:], OA[64:128, :], r=["bank%d" % (3 + oi)], w=[("rl", fi)])
                                b.tt(t1[fi][:], OA[0:64, :], rl[fi][:], ALU.mult, r=["bank%d" % (3 + oi), ("rl", fi)],
                                     w=[("t1", fi)])
                                b.tt(ost[fi][:], t1[fi][:],
                                     zt[i][:, qs * 512:(qs + 1) * 512], ALU.mult, r=[("t1", fi), "zt%d" % i],
                                     w=[("ost", fi)], eng="pool")
                                b.dma(sO[hp][ho:ho + 64, qs * 512:(qs + 1) * 512], ost[fi][:], r=[("ost", fi)],
                                      w=[("sO", h, qs)], eng="pool")
                                if qs == NB - 1:
                                    w_tick()
                                    if h + 2 < 16:
                                        load_head(h + 2)

                        load_head(0)
                        load_head(1)
                        for idx in range(len(steps) + LOOK):
                            if idx < len(steps):
                                emit_qk(idx)
                            if idx - LOOK >= 0:
                                emit_pv(idx - LOOK)
                        w_drain()
                        P.flush()

                if stop_after == "B%d" % l:
                    return nc
                with contextlib.ExitStack() as ph:
                    xc = [sb(ph, "xc%d" % i, [128, KC, 512], F32) for i in range(2)]
                    otb = [sb(ph, "otb%d" % i, [128, KC, 512], BF16) for i in range(2)]
                    sO_v = sO.rearrange("h p t -> p h t")
                    nb_ = 0
                    for tb in range(NB):
                        ts_ = slice(tb * 512, (tb + 1) * 512)
                        xb, xk = xc[tb % 2], "xc%d" % (tb % 2)
                        ob_, ok_ = otb[tb % 2], "otb%d" % (tb % 2)
                        if tb == 0:
                            b.dma(ob_[:], sO_v[:, :, ts_], r=[], w=[ok_])
                            b.dma(xb[:], xsrc_v[:, :, ts_], r=[("x", tb)], w=[xk])
                        if tb + 1 < NB:
                            b.dma(otb[(tb + 1) % 2][:], sO_v[:, :, (tb + 1) * 512:(tb + 2) * 512], r=[],
                                  w=["otb%d" % ((tb + 1) % 2)])
                            b.dma(xc[(tb + 1) % 2][:], xsrc_v[:, :, (tb + 1) * 512:(tb + 2) * 512],
                                  r=[("x", tb + 1)], w=["xc%d" % ((tb + 1) % 2)])
                        for dc in range(KC):
                            bi = nb_ % 4
                            nb_ += 1
                            pb, pk = banks[bi], "bank%d" % bi
                            for fc in range(KC):
                                b.mm(pb[:], wout[:, fc, dc * 128:(dc + 1) * 128], ob_[:, fc, :], fc == 0, fc == KC - 1,
                                     r=["wout", ok_], w=[pk])
                            b.stt(xb[:, dc, :], pb[:], modsb[:, l, 16 + dc:17 + dc], xb[:, dc, :], ALU.mult, ALU.add,
                                  r=[pk, "modsb", xk], w=[xk])
                        b.dma(y_v[:, :, ts_], xb[:], r=[xk], w=[("x", tb)], eng="pool")
                    P.flush()
    return nc


def _consts():
    s = np.arange(128)[:, None]
    t = np.arange(128)[None, :]
    maskbias = np.where(s > t, NEG, 0.0).astype(np.float32)
    mask01 = ((s // 32 == t // 32) & (s <= t)).astype(np.float32)
    mj = (s // 32 == np.arange(4)[None, :]).astype(np.float32)
    blk64 = (s // 64 == t // 64).astype(np.float32)
    return dict(c_maskbias=maskbias, c_mask01=mask01, c_mj=mj, c_blk64=blk64)


def _pcol(v, n):
    return np.ascontiguousarray(np.asarray(v, np.float32).reshape(n, 128).T)


def make_in_maps(inputs, n_cores, S):
    f = lambda a: np.ascontiguousarray(np.asarray(a, dtype=np.float32))
    x = f(inputs["x"])
    c = f(inputs["c"])
    shared = dict(
        gT=np.ascontiguousarray(np.stack([_pcol(g, 8) for g in f(inputs["norm_g"])], 1)),
        ada_w=f(inputs["ada_w"]),
        ada_bT=np.ascontiguousarray(np.stack([_pcol(v, 24) for v in f(inputs["ada_b"])], 1)),
        lbT=np.ascontiguousarray(np.stack([_pcol(v, 8) for v in f(inputs["hg_lb_logits"])], 1)),
        hg_w_in=f(inputs["hg_w_in"]),
        ogT=np.ascontiguousarray(np.stack([_pcol(v, 8) for v in f(inputs["hg_o_g"])], 1)),
        hg_w_out=f(inputs["hg_w_out"]),
        fx_w_in=f(inputs["fx_w_in"]),
        bfT=np.ascontiguousarray(f(inputs["fx_b_f"]).T),
        qgT=np.ascontiguousarray(np.tile(f(inputs["fx_q_g"]).T, (2, 1))),
        kgT=np.ascontiguousarray(np.tile(f(inputs["fx_k_g"]).T, (2, 1))),
        fx_w_out=f(inputs["fx_w_out"]),
    )
    shared.update(_consts())
    maps = []
    for i in range(n_cores):
        m = dict(shared)
        m["xT"] = np.ascontiguousarray(x[i].T)
        m["cT"] = _pcol(c[i], 8)
        maps.append(m)
    return maps


_NC_CACHE = {}


def run(inputs, n_layers=4, stop_after=None):
    x = np.asarray(inputs["x"])
    Bn, S, _ = x.shape
    key = (S, n_layers, stop_after)
    if key not in _NC_CACHE:
        _NC_CACHE[key] = build(S, n_layers, stop_after)
    nc = _NC_CACHE[key]
    maps = make_in_maps(inputs, Bn, S)
    res = run_bass_kernel_spmd(nc, maps, core_ids=list(range(Bn)))
    out = np.stack([np.ascontiguousarray(res.results[i]["yT"].T) for i in range(Bn)], 0)
    return out.astype(np.float32)


def kernel(**inputs):
    return run(inputs, n_layers=4)
```
